# Optimizing a Trainium2 kernel written in Bass

```python
import jax, jax.numpy as jnp
from jax import lax
import numpy as np

D_MODEL = 1024
BATCH = 8
SEQ = 4096
DEPTH = 2

HEAD_DIM = 64
GRID_W = 64
NA_HEADS = D_MODEL // (4 * HEAD_DIM)
NA_WIN_H = 8
NA_WIN_W = 16
MLA_HEADS = 3 * D_MODEL // (8 * HEAD_DIM)
MLA_Q_LORA = D_MODEL // 4
MLA_KV_LORA = D_MODEL // 8
MLA_NOPE = 64
MLA_ROPE = 32
MLA_V = 64
MLA_BLOCK = 128
SWA_HEADS = 3 * D_MODEL // (8 * HEAD_DIM)
SWA_KV_HEADS = 2
SWA_WINDOW = 128
SWA_BLOCK = 128
ROPE_THETA = 10000.0
N_EXPERTS = 16
EC_CAPACITY = 2
D_EXPERT = D_MODEL
RMS_EPS = 1e-6
NEG_INF = -1e30

A_W = NA_HEADS * HEAD_DIM
B_W = MLA_HEADS * MLA_V
C_W = SWA_HEADS * HEAD_DIM
D_MIX = A_W + B_W + C_W
IN_WIDTHS = (A_W, A_W, A_W,
             MLA_Q_LORA, MLA_KV_LORA, MLA_ROPE,
             SWA_HEADS * HEAD_DIM, SWA_KV_HEADS * HEAD_DIM, SWA_KV_HEADS * HEAD_DIM)
IN_COLS = sum(IN_WIDTHS)
IN_OFFSETS = tuple(int(v) for v in np.cumsum(IN_WIDTHS)[:-1])

kernel_name = 'hybrid_na_mla_swa_ec_encoder'


def rms_norm(x, g):
    x32 = x.astype(jnp.float32)
    y = x32 * lax.rsqrt(jnp.mean(x32 * x32, axis=-1, keepdims=True) + RMS_EPS)
    return (y * g.astype(jnp.float32)).astype(x.dtype)


def rope_tables(seq, dim, dtype):
    inv = 1.0 / (ROPE_THETA ** (jnp.arange(0, dim, 2, dtype=jnp.float32) / dim))
    ang = jnp.arange(seq, dtype=jnp.float32)[:, None] * inv[None, :]
    return jnp.cos(ang).astype(dtype), jnp.sin(ang).astype(dtype)


def apply_rope(x, cos, sin):
    x1, x2 = jnp.split(x, 2, axis=-1)
    c = cos[None, :, None, :]
    s = sin[None, :, None, :]
    return jnp.concatenate([x1 * c - x2 * s, x2 * c + x1 * s], axis=-1)


def neighbourhood_attention(q, k, v, rpb):
    B, S, _ = q.shape
    rows = S // GRID_W
    wh = min(NA_WIN_H, rows)
    ww = NA_WIN_W
    shp = (B, rows, GRID_W, NA_HEADS, HEAD_DIM)
    qg, kg, vg = q.reshape(shp), k.reshape(shp), v.reshape(shp)
    cols = jnp.arange(GRID_W)
    col_start = jnp.clip(cols - ww // 2, 0, GRID_W - ww)
    col_idx = col_start[:, None] + jnp.arange(ww)[None, :]
    dc = col_idx - cols[:, None] + (NA_WIN_W - 1)
    scale = HEAD_DIM ** -0.5

    def one_row(args):
        r, q_row = args
        rs = jnp.clip(r - wh // 2, 0, rows - wh)
        k_rows = lax.dynamic_slice_in_dim(kg, rs, wh, axis=1)
        v_rows = lax.dynamic_slice_in_dim(vg, rs, wh, axis=1)
        k_win = k_rows[:, :, col_idx]
        v_win = v_rows[:, :, col_idx]
        dr = rs + jnp.arange(wh) - r + (NA_WIN_H - 1)
        bias = rpb[:, dr][:, :, dc].transpose(0, 2, 1, 3)
        s = jnp.einsum('bchd,brcjhd->bhcrj', q_row, k_win).astype(jnp.float32) * scale
        s = s + bias[None].astype(jnp.float32)
        p = jax.nn.softmax(s.reshape(B, NA_HEADS, GRID_W, wh * ww), axis=-1)
        p = p.reshape(s.shape).astype(v.dtype)
        return jnp.einsum('bhcrj,brcjhd->bchd', p, v_win)

    out = lax.map(one_row, (jnp.arange(rows), qg.swapaxes(0, 1)))
    return out.swapaxes(0, 1).reshape(B, S, NA_HEADS * HEAD_DIM)


def latent_attention(c_q, c_kv, k_rope, q_norm, w_uq, kv_norm, w_ukv, cos_r, sin_r):
    B, S, _ = c_q.shape
    q = (rms_norm(c_q, q_norm) @ w_uq).reshape(B, S, MLA_HEADS, MLA_NOPE + MLA_ROPE)
    q_nope = q[..., :MLA_NOPE]
    q_pe = apply_rope(q[..., MLA_NOPE:], cos_r, sin_r)
    kv = (rms_norm(c_kv, kv_norm) @ w_ukv).reshape(B, S, MLA_HEADS, MLA_NOPE + MLA_V)
    k_nope = kv[..., :MLA_NOPE]
    v = kv[..., MLA_NOPE:]
    k_pe = apply_rope(k_rope[:, :, None, :], cos_r, sin_r)[:, :, 0]
    nb = S // MLA_BLOCK
    scale = (MLA_NOPE + MLA_ROPE) ** -0.5

    def to_blocks(t):
        return t.reshape(B, nb, MLA_BLOCK, *t.shape[2:]).swapaxes(0, 1)

    def one_block(args):
        qn, qp = args
        s = (jnp.einsum('bqhd,bkhd->bhqk', qn, k_nope)
             + jnp.einsum('bqhd,bkd->bhqk', qp, k_pe)).astype(jnp.float32) * scale
        p = jax.nn.softmax(s, axis=-1).astype(v.dtype)
        return jnp.einsum('bhqk,bkhd->bqhd', p, v)

    o = lax.map(one_block, (to_blocks(q_nope), to_blocks(q_pe)))
    return o.swapaxes(0, 1).reshape(B, S, MLA_HEADS * MLA_V)


def window_gqa(q, k, v, sink, cos, sin):
    B, S, _ = q.shape
    G = SWA_HEADS // SWA_KV_HEADS
    q = apply_rope(q.reshape(B, S, SWA_HEADS, HEAD_DIM), cos, sin)
    k = apply_rope(k.reshape(B, S, SWA_KV_HEADS, HEAD_DIM), cos, sin)
    v = v.reshape(B, S, SWA_KV_HEADS, HEAD_DIM)
    nb = S // SWA_BLOCK
    n_side = -(-SWA_WINDOW // SWA_BLOCK)
    pad = n_side * SWA_BLOCK
    n_keys = (2 * n_side + 1) * SWA_BLOCK

    def band(t):
        tp = jnp.pad(t, ((0, 0), (pad, pad), (0, 0), (0, 0)))
        tp = tp.reshape(B, nb + 2 * n_side, SWA_BLOCK, SWA_KV_HEADS, HEAD_DIM)
        return jnp.concatenate([tp[:, i:i + nb] for i in range(2 * n_side + 1)], axis=2)

    kb, vb = band(k), band(v)
    qb = q.reshape(B, nb, SWA_BLOCK, SWA_KV_HEADS, G, HEAD_DIM)
    s = jnp.einsum('bnqkgd,bnjkd->bnkgqj', qb, kb).astype(jnp.float32) * (HEAD_DIM ** -0.5)
    blk = jnp.arange(nb)[:, None, None] * SWA_BLOCK
    qpos = blk + jnp.arange(SWA_BLOCK)[None, :, None]
    kpos = blk - pad + jnp.arange(n_keys)[None, None, :]
    valid = (jnp.abs(qpos - kpos) <= SWA_WINDOW) & (kpos >= 0) & (kpos < S)
    s = jnp.where(valid[None, :, None, None], s, NEG_INF)
    sink_logit = jnp.broadcast_to(sink.astype(jnp.float32).reshape(1, 1, SWA_KV_HEADS, G, 1, 1),
                                  s.shape[:-1] + (1,))
    p = jax.nn.softmax(jnp.concatenate([s, sink_logit], axis=-1), axis=-1)[..., :-1].astype(v.dtype)
    o = jnp.einsum('bnkgqj,bnjkd->bnqkgd', p, vb)
    return o.reshape(B, S, SWA_HEADS * HEAD_DIM)


def expert_choice_ffn(xn, w_router, w_gate, w_up, w_down):
    B, S, D = xn.shape
    cap = EC_CAPACITY * S // N_EXPERTS
    aff = jax.nn.softmax((xn @ w_router).astype(jnp.float32), axis=-1)
    g, idx = lax.top_k(aff.swapaxes(1, 2), cap)
    bidx = jnp.arange(B)[:, None, None]
    xs = xn[bidx, idx]
    hid = jax.nn.silu(jnp.einsum('becd,edf->becf', xs, w_gate)) * jnp.einsum('becd,edf->becf', xs, w_up)
    y = jnp.einsum('becf,efd->becd', hid, w_down) * g[..., None].astype(xn.dtype)
    return jnp.zeros_like(xn).at[bidx, idx].add(y)


def setup_inputs(seed: int = 0) -> dict:
    key = jax.random.key(seed)
    ks = jax.random.split(key, 17)
    f32 = jnp.float32

    def nrm(k, shape, scale):
        return jax.random.normal(k, shape, f32) * scale

    def gain(k, shape):
        return 1.0 + 0.05 * jax.random.normal(k, shape, f32)

    return {
        'x': nrm(ks[0], (BATCH, SEQ, D_MODEL), 1.0),
        'attn_norm': gain(ks[1], (DEPTH, D_MODEL)),
        'w_in': nrm(ks[2], (DEPTH, D_MODEL, IN_COLS), D_MODEL ** -0.5),
        'na_rpb': nrm(ks[3], (DEPTH, NA_HEADS, 2 * NA_WIN_H - 1, 2 * NA_WIN_W - 1), 0.1),
        'mla_q_norm': gain(ks[4], (DEPTH, MLA_Q_LORA)),
        'mla_w_uq': nrm(ks[5], (DEPTH, MLA_Q_LORA, MLA_HEADS * (MLA_NOPE + MLA_ROPE)), MLA_Q_LORA ** -0.5),
        'mla_kv_norm': gain(ks[6], (DEPTH, MLA_KV_LORA)),
        'mla_w_ukv': nrm(ks[7], (DEPTH, MLA_KV_LORA, MLA_HEADS * (MLA_NOPE + MLA_V)), MLA_KV_LORA ** -0.5),
        'swa_sink': nrm(ks[8], (DEPTH, SWA_HEADS), 1.0),
        'group_norm': gain(ks[9], (DEPTH, D_MIX)),
        'w_out': nrm(ks[10], (DEPTH, D_MIX, D_MODEL), D_MIX ** -0.5),
        'ffn_norm': gain(ks[11], (DEPTH, D_MODEL)),
        'w_router': nrm(ks[12], (DEPTH, D_MODEL, N_EXPERTS), D_MODEL ** -0.5),
        'w_gate': nrm(ks[13], (DEPTH, N_EXPERTS, D_MODEL, D_EXPERT), D_MODEL ** -0.5),
        'w_up': nrm(ks[14], (DEPTH, N_EXPERTS, D_MODEL, D_EXPERT), D_MODEL ** -0.5),
        'w_down': nrm(ks[15], (DEPTH, N_EXPERTS, D_EXPERT, D_MODEL), D_EXPERT ** -0.5),
        'final_norm': gain(ks[16], (D_MODEL,)),
    }


def reference(x, attn_norm, w_in, na_rpb, mla_q_norm, mla_w_uq, mla_kv_norm, mla_w_ukv, swa_sink,
              group_norm, w_out, ffn_norm, w_router, w_gate, w_up, w_down, final_norm):
    B, S, _ = x.shape
    cos, sin = rope_tables(S, HEAD_DIM, x.dtype)
    cos_r, sin_r = rope_tables(S, MLA_ROPE, x.dtype)
    h = x
    for l in range(DEPTH):
        xn = rms_norm(h, attn_norm[l])
        proj = xn @ w_in[l]
        a_q, a_k, a_v, b_cq, b_ckv, b_kr, c_q, c_k, c_v = jnp.split(proj, IN_OFFSETS, axis=-1)
        o_a = neighbourhood_attention(a_q, a_k, a_v, na_rpb[l])
        o_b = latent_attention(b_cq, b_ckv, b_kr, mla_q_norm[l], mla_w_uq[l],
                               mla_kv_norm[l], mla_w_ukv[l], cos_r, sin_r)
        o_c = window_gqa(c_q, c_k, c_v, swa_sink[l], cos, sin)
        gn = group_norm[l]
        o = jnp.concatenate([rms_norm(o_a, gn[:A_W]),
                             rms_norm(o_b, gn[A_W:A_W + B_W]),
                             rms_norm(o_c, gn[A_W + B_W:])], axis=-1)
        h = h + o @ w_out[l]
        h = h + expert_choice_ffn(rms_norm(h, ffn_norm[l]), w_router[l], w_gate[l], w_up[l], w_down[l])
    return rms_norm(h, final_norm)
```

```python
import numpy as np
import ml_dtypes
from contextlib import ExitStack
import concourse.bass as bass
import concourse.mybir as mybir
from concourse.bass_utils import run_bass_kernel_spmd

F32 = mybir.dt.float32
BF16 = mybir.dt.bfloat16
U32 = mybir.dt.uint32
AF = mybir.ActivationFunctionType
ALU = mybir.AluOpType

S = 4096
D = 1024
NTB = 32
NQB = 8
L = 2
NE = 16
EPS = 1e-6
NEG = -30000.0
NCORES = 8
ROUNDS = 32
ROWS_PER_E = 4
SLOT_CHUNKS = 1
NBIS = 27


class Buf:
    __slots__ = ("name", "writer", "readers")

    def __init__(self, name=""):
        self.name = name
        self.writer = None
        self.readers = []


class Sched:
    ENG = ("pe", "act", "dve", "pool", "sp")

    def __init__(self, nc, es, same_engine_sync=True):
        self.nc = nc
        self.e = {"pe": nc.tensor, "act": nc.scalar, "dve": nc.vector,
                  "pool": nc.gpsimd, "sp": nc.sync}
        self.sem = {k: es.enter_context(nc.semaphore("s_" + k)) for k in self.ENG}
        self.seq = {k: 0 for k in self.ENG}
        self.waited = {a: {} for a in self.ENG}
        self.same_engine_sync = same_engine_sync
        self.lanes = {}
        self.lane_rr = {}
        for q, n in (("sp", 16), ("pool", 16)):
            self.lanes[q] = [[es.enter_context(nc.semaphore(f"d_{q}{i}")), 0] for i in range(n)]
            self.lane_rr[q] = 0
        self.pending_reads = {k: [] for k in self.ENG}

    def _wait(self, on, dep):
        if dep is None:
            return
        if dep[0] == "eng":
            _, eng, seq = dep
            if eng == on and (not self.same_engine_sync or on == "pe"):
                return
            if self.waited[on].get(eng, 0) >= seq:
                return
            self.e[on].wait_ge(self.sem[eng], seq)
            self.waited[on][eng] = seq
        else:
            _, q, li, cnt = dep
            key = ("dma", q, li)
            if self.waited[on].get(key, 0) >= cnt:
                return
            self.e[on].wait_ge(self.lanes[q][li][0], 16 * cnt)
            self.waited[on][key] = cnt

    def _deps(self, on, reads, writes):
        for r in reads:
            self._wait(on, r.writer)
        for w in writes:
            self._wait(on, w.writer)
            for d in w.readers:
                self._wait(on, d)

    def _commit(self, dep, reads, writes):
        for w in writes:
            w.writer = dep
            w.readers = []
        for r in reads:
            if r not in writes:
                r.readers.append(dep)
                if len(r.readers) > 48:
                    last = {}
                    for d in r.readers:
                        k = d[:2] if d[0] == "eng" else d[:3]
                        if k not in last or d[-1] > last[k][-1]:
                            last[k] = d
                    r.readers = list(last.values())

    def op(self, on, fn, reads=(), writes=(), inc=True):
        reads = list(reads)
        writes = list(writes)
        self._deps(on, reads, writes)
        inst = fn(self.e[on])
        if inc:
            self.seq[on] += 1
            inst.then_inc(self.sem[on], 1)
            dep = ("eng", on, self.seq[on])
            self._commit(dep, reads + self.pending_reads[on], writes)
            self.pending_reads[on] = []
        else:
            self.pending_reads[on].extend(reads)
            for w in writes:
                w.writer = ("eng", on, self.seq[on] + 1)
                w.readers = []
        return inst

    def dma(self, q, fn, reads=(), writes=()):
        reads = list(reads)
        writes = list(writes)
        lanes = self.lanes[q]
        li = self.lane_rr[q]
        self.lane_rr[q] = (li + 1) % len(lanes)
        sem, cnt = lanes[li]
        if cnt > 0:
            self._wait(q, ("dma", q, li, cnt))
        self._deps(q, reads, writes)
        inst = fn(self.e[q])
        inst.then_inc(sem, 16)
        lanes[li][1] = cnt + 1
        dep = ("dma", q, li, cnt + 1)
        self._commit(dep, reads, writes)
        return inst

    def barrier(self):
        for a in self.ENG:
            for b in self.ENG:
                if a != b and self.seq[b] > 0:
                    self._wait(a, ("eng", b, self.seq[b]))
            for q in self.lanes:
                for li, (sem, cnt) in enumerate(self.lanes[q]):
                    if cnt > 0:
                        self._wait(a, ("dma", q, li, cnt))


class Rot:
    def __init__(self, items):
        self.items = items
        self.i = 0

    def next(self):
        it = self.items[self.i]
        self.i = (self.i + 1) % len(self.items)
        return it


def _na_row_info(r):
    rs = min(max(r - 4, 0), 56)
    if rs % 2 == 0:
        start, nb = rs, 4
    else:
        start, nb = rs - 1, 5
    return rs, start, nb


def _na_variants():
    keys = []
    vmap = {}
    for r in range(64):
        rs, start, nb = _na_row_info(r)
        k = (start - r, rs - start, nb)
        if k not in keys:
            keys.append(k)
        vmap[r] = keys.index(k)
    return keys, vmap


NA_KEYS, NA_VMAP = _na_variants()
NV = len(NA_KEYS)


def build(debug=False, nlayers=L, stop_after=None):
    nc = bass.Bass("TRN2", target_bir_lowering=False)
    es = ExitStack()

    def din(name, shape, dt=F32):
        return nc.dram_tensor(name, list(shape), dt, kind="ExternalInput").ap()

    dbg_kind = "ExternalOutput" if debug else "Internal"

    def dscr(name, shape, dt=F32, dbg=True):
        return nc.dram_tensor(name, list(shape), dt, kind=(dbg_kind if dbg else "Internal")).ap()

    x_d = din("x", [S, D])
    attn_norm_d = din("attn_norm", [L, D])
    ffn_norm_d = din("ffn_norm", [L, D])
    final_norm_d = din("final_norm", [1, D])
    w_na_d = din("w_na", [L, D, 768])
    w_lat_d = din("w_lat", [L, D, 384])
    w_kr_d = din("w_kr", [L, D, 96])
    w_krp_d = din("w_krp", [L, D, 96])
    w_cq_d = din("w_cq", [L, D, 384])
    w_cqp_d = din("w_cqp", [L, D, 384])
    w_ck_d = din("w_ck", [L, D, 128])
    w_ckp_d = din("w_ckp", [L, D, 128])
    w_cv_d = din("w_cv", [L, D, 128])
    rpb_d = din("rpb_tiles", [L, NV, 4, 128, 320])
    qn_d = din("mla_q_norm", [L, 256])
    kvn_d = din("mla_kv_norm", [L, 128])
    w_uq_d = din("w_uq", [L, 256, 576])
    w_uqp_d = din("w_uqp", [L, 256, 576])
    w_uk_d = din("w_uk", [L, 128, 384])
    w_uv_d = din("w_uv", [L, 128, 384])
    sink_d = din("swa_sink", [L, 6])
    gn_d = din("group_norm", [L, D])
    w_out_d = din("w_out", [L, D, D])
    w_router_d = din("w_router", [L, D, NE])
    w_gate_d = din("w_gate", [L, NE, D, D])
    w_up_d = din("w_up", [L, NE, D, D])
    w_down_d = din("w_down", [L, NE, D, D])
    cs_c_d = din("rope_c_cos", [128, S])
    sn_c_d = din("rope_c_sin", [128, S])
    cs_b_d = din("rope_b_cos", [32, S])
    sn_b_d = din("rope_b_sin", [32, S])
    swa_mask_d = din("swa_mask", [6, 128, 512], BF16)
    ident_bf_d = din("ident_bf", [128, 128], BF16)
    ident_f_d = din("ident_f", [128, 128])
    gmat_d = din("gmat", [128, 128])
    rowoff_d = din("rowoff", [128, 1])
    invw_d = din("invw", [128, 3])
    trimat_d = din("trimat", [128, 128])
    dmat_d = din("dmat", [128, ROUNDS * 8 // 128, 512], mybir.dt.int16)

    out_d = nc.dram_tensor("out", [S, D], F32, kind="ExternalOutput").ap()
    h_d = dscr("h_scr", [S, D])
    xn2_d = dscr("xn2_scr", [S, D], BF16)
    oc_d = dscr("oc_scr", [16, 64, S], BF16)
    aff_dbg = dscr("aff_dbg", [128, 512]) if debug else None
    sel_dbg = dscr("sel_dbg", [128, 128]) if debug else None

    s = Sched(nc, es)
    T = lambda name, shape, dt, st=es: st.enter_context(nc.sbuf_tensor("t_" + name, list(shape), dt))

    pp = [es.enter_context(nc.psum_tensor(f"pp{i}", [128, 1024], F32)) for i in range(4)]
    banks = [pp[i // 2][:, (i % 2) * 512:(i % 2 + 1) * 512] for i in range(8)]
    bbuf = [Buf(f"bank{i}") for i in range(8)]

    ident_bf = T("ident_bf", [128, 128], BF16)
    ident_f = T("ident_f", [128, 128], F32)
    ones_bf = T("ones_bf", [128, 128], BF16)
    ones_f = T("ones_f", [128, 128], F32)
    eps_t = T("eps_t", [128, 1], F32)
    cbuf = Buf("consts")
    s.dma("sp", lambda e: e.dma_start(out=ident_bf[:], in_=ident_bf_d), writes=[cbuf])
    s.dma("sp", lambda e: e.dma_start(out=ident_f[:], in_=ident_f_d), writes=[cbuf])
    s.op("dve", lambda e: e.memset(ones_bf[:], 1.0), writes=[cbuf])
    s.op("dve", lambda e: e.memset(ones_f[:], 1.0), writes=[cbuf])
    s.op("dve", lambda e: e.memset(eps_t[:], EPS), writes=[cbuf])
    s.barrier()

    def rstd_big(ss_ap, out_ap, scratch_ap, inv_n, rd, wr):
        s.op("dve", lambda e: e.tensor_scalar(out=out_ap, in0=ss_ap, scalar1=inv_n, scalar2=EPS,
                                              op0=ALU.mult, op1=ALU.add), reads=rd, writes=wr)
        s.op("act", lambda e: e.activation(out=out_ap, in_=out_ap, func=AF.Ln), reads=wr, writes=wr)
        s.op("act", lambda e: e.activation(out=out_ap, in_=out_ap, func=AF.Exp, scale=-0.5), reads=wr, writes=wr)

    def rstd_from_ss(ss_ap, out_ap, inv_n, rd, wr):
        s.op("dve", lambda e: e.tensor_scalar(out=out_ap, in0=ss_ap, scalar1=inv_n, scalar2=EPS,
                                              op0=ALU.mult, op1=ALU.add), reads=rd, writes=wr)
        s.op("act", lambda e: e.activation(out=out_ap, in_=out_ap, func=AF.Sqrt), reads=wr, writes=wr)
        s.op("dve", lambda e: e.reciprocal(out=out_ap, in_=out_ap), reads=wr, writes=wr)

    evac_rr = [0]

    def evac_copy(out_ap, in_ap, rd, wr, scale=None):
        evac_rr[0] ^= 1
        if evac_rr[0]:
            if scale is None:
                s.op("act", lambda e: e.copy(out=out_ap, in_=in_ap), reads=rd, writes=wr)
            else:
                s.op("act", lambda e: e.mul(out=out_ap, in_=in_ap, mul=scale), reads=rd, writes=wr)
        else:
            if scale is None:
                s.op("dve", lambda e: e.tensor_copy(out=out_ap, in_=in_ap), reads=rd, writes=wr)
            else:
                s.op("dve", lambda e: e.tensor_scalar(out=out_ap, in0=in_ap, scalar1=scale, scalar2=None,
                                                      op0=ALU.mult), reads=rd, writes=wr)

    fin_queue = []
    fin_age = [3]

    def flush_fin(force=False):
        for ent in fin_queue:
            ent[0] += 1
        while fin_queue and (force or fin_queue[0][0] > fin_age[0]):
            fin_queue.pop(0)[1]()

    def finalize_heads(O_bank, O_buf, j_oc, col0, st_tiles, extra_den=None, bank_src=None, use_act=False):
        rden, b_rden, bc_bank, b_bc, bc_sb, b_bcsb, o_bf, b_obf = st_tiles
        if use_act:
            if extra_den is not None:
                s.op("act", lambda e: e.activation(out=rden[64:65, 0:512], in_=O_bank[64:65, :], func=AF.Ln, bias=extra_den),
                     reads=[O_buf], writes=[b_rden])
            else:
                s.op("act", lambda e: e.activation(out=rden[64:65, 0:512], in_=O_bank[64:65, :], func=AF.Ln),
                     reads=[O_buf], writes=[b_rden])
            s.op("act", lambda e: e.activation(out=rden[64:65, 0:512], in_=rden[64:65, 0:512], func=AF.Exp, scale=-1.0),
                 reads=[b_rden], writes=[b_rden])
        elif extra_den is not None:
            s.op("dve", lambda e: e.tensor_scalar(out=rden[64:65, 0:512], in0=O_bank[64:65, :], scalar1=extra_den,
                                                  scalar2=None, op0=ALU.add), reads=[O_buf], writes=[b_rden])
            s.op("dve", lambda e: e.reciprocal(out=rden[64:65, 0:512], in_=rden[64:65, 0:512]), reads=[b_rden], writes=[b_rden])
        else:
            s.op("dve", lambda e: e.reciprocal(out=rden[64:65, 0:512], in_=O_bank[64:65, :]), reads=[O_buf], writes=[b_rden])

        def fin_b(bc_bank=bc_bank, b_bc=b_bc):
            if bank_src is not None:
                Spb, b_Spb = bank_src.next()
                bc_bank, b_bc = Spb[:, 0:512], b_Spb[0]
            s.op("pe", lambda e: e.matmul(bc_bank[0:64, :], lhsT=ones_f[64:65, 0:64], rhs=rden[64:65, 0:512],
                                          start=True, stop=True), reads=[b_rden], writes=[b_bc])
            s.op("act" if use_act else "dve", (lambda e: e.copy(out=bc_sb[0:64, :], in_=bc_bank[0:64, :])) if use_act else
                 (lambda e: e.tensor_copy(out=bc_sb[0:64, :], in_=bc_bank[0:64, :])), reads=[b_bc], writes=[b_bcsb])
            s.op("dve", lambda e: e.tensor_tensor(out=o_bf[0:64, :], in0=O_bank[0:64, :], in1=bc_sb[0:64, :], op=ALU.mult),
                 reads=[O_buf, b_bcsb], writes=[b_obf])
            s.dma("sp", lambda e: e.dma_start(out=oc_d[j_oc, :, col0:col0 + 512], in_=o_bf[0:64, :]), reads=[b_obf])
        fin_queue.append([0, fin_b])

    for l in range(nlayers):
        src_d = x_d if l == 0 else h_d
        lat = ExitStack()
        cqn = T(f"cqn{l}", [128, 2, S], BF16, lat)
        ckvn = T(f"ckvn{l}", [128, S], BF16, lat)
        kpe = T(f"kpe{l}", [96, S], BF16, lat)
        lst = ExitStack()
        xnT = T(f"xnT{l}", [128, 8, S], BF16, lst)
        nst = ExitStack()
        rpb_sb = T(f"rpb{l}", [128, NV * 4, 320], BF16, nst)
        b_rpb = Buf()
        for v in range(NV):
            s.dma("pool", lambda e: e.dma_start(out=rpb_sb[:, v * 4:(v + 1) * 4, :], in_=rpb_d[l, v].rearrange("h p c -> p h c")), writes=[b_rpb])
        with ExitStack() as ps:
            g_bc = T(f"g_bc{l}", [128, D], F32, ps)
            b_g = Buf()
            s.dma("sp", lambda e: e.dma_start(out=g_bc[:], in_=attn_norm_d[l:l + 1, :].partition_broadcast(128)), writes=[b_g])
            hts = Rot([(T(f"ht{l}_{i}", [128, D], F32, ps), Buf()) for i in range(4)])
            xbs = Rot([(T(f"xb{l}_{i}", [128, D], BF16, ps), Buf()) for i in range(2)])
            junk = T(f"junk{l}", [128, D], BF16, ps)
            b_junk = Buf()
            sss = Rot([(T(f"ss{l}_{i}", [128, 1], F32, ps), Buf()) for i in range(4)])
            pts = Rot([(banks[i], bbuf[i]) for i in range(2)])
            def p1_a(tb):
                ht, b_ht = hts.next()
                ss, b_ss = sss.next()
                s.dma("sp", lambda e: e.dma_start(out=ht[:], in_=src_d[tb * 128:(tb + 1) * 128, :]), writes=[b_ht])
                s.op("dve", lambda e: e.scalar_tensor_tensor(out=junk[:], in0=ht[:], scalar=1.0, in1=ht[:],
                                                             op0=ALU.mult, op1=ALU.mult, accum_out=ss[:]),
                     reads=[b_ht], writes=[b_junk, b_ss])
                s.op("dve", lambda e: e.tensor_scalar(out=ss[:], in0=ss[:], scalar1=1.0 / D, scalar2=EPS, op0=ALU.mult, op1=ALU.add),
                     reads=[b_ss], writes=[b_ss])
                s.op("act", lambda e: e.activation(out=ss[:], in_=ss[:], func=AF.Sqrt), reads=[b_ss], writes=[b_ss])
                return (tb, ht, b_ht, ss, b_ss)

            def p1_b(ctx):
                tb, ht, b_ht, ss, b_ss = ctx
                xb, b_xb = xbs.next()
                pt, b_pt = pts.next()
                s.op("dve", lambda e: e.reciprocal(out=ss[:], in_=ss[:]), reads=[b_ss], writes=[b_ss])
                s.op("dve", lambda e: e.scalar_tensor_tensor(out=xb[:], in0=ht[:], scalar=ss[:, 0:1], in1=g_bc[:],
                                                             op0=ALU.mult, op1=ALU.mult),
                     reads=[b_ht, b_ss, b_g], writes=[b_xb])
                ptv = pt[:].bitcast(BF16)
                for c in range(8):
                    s.op("pe", lambda e: e.transpose(out=ptv[:, c * 128:(c + 1) * 128], in_=xb[:, c * 128:(c + 1) * 128],
                                                     identity=ident_bf[:]),
                         reads=[b_xb], writes=[b_pt], inc=(c == 7))
                xv = xnT[:, :, tb * 128:(tb + 1) * 128]
                pv = ptv.rearrange("p (c t) -> p c t", c=8)
                s.op("act", lambda e: e.copy(out=xv, in_=pv), reads=[b_pt])

            p1_pend = []
            for tb in range(NTB):
                p1_pend.append(p1_a(tb))
                if len(p1_pend) > 1:
                    p1_b(p1_pend.pop(0))
            while p1_pend:
                p1_b(p1_pend.pop(0))
            s.barrier()

        def proj_fm(w_sb, col0, ncol, nchunk, src, qb, bank, bbank, src_cols=None):
            for c in range(nchunk):
                lw = w_sb[:, c, col0:col0 + ncol]
                rr = src[:, c, qb * 512:(qb + 1) * 512]
                s.op("pe", lambda e: e.matmul(bank[0:ncol, :], lhsT=lw, rhs=rr, start=(c == 0), stop=(c == nchunk - 1)),
                     writes=[bbank], inc=(c == nchunk - 1))

        def load_w_cast(dst, src_ap, wbuf):
            s.dma("pool", lambda e: e.dma_start(out=dst, in_=src_ap), writes=[wbuf])

        with ExitStack() as ps:
            b_w = Buf()
            QT = T(f"QTa{l}", [128, 2, S], BF16, ps)
            KT = T(f"KTa{l}", [128, 4, S], BF16, ps)
            Va = T(f"Va{l}", [128, NTB, 4, 65], BF16, ps)
            s.op("pool", lambda e: e.memset(Va[:, :, :, 64:65], 1.0), writes=[b_w])
            s.op("pool", lambda e: e.memset(KT[:], 0.0), writes=[b_w])
            pw = ExitStack()
            w_sb = T(f"w_na{l}", [128, 8, 768], BF16, pw)
            for c in range(8):
                load_w_cast(w_sb[:, c, :], w_na_d[l, c * 128:(c + 1) * 128, :], b_w)
            s.barrier()
            pbk = Rot([(banks[i], bbuf[i]) for i in range(4)])
            for g in range(4):
                for qb in range(NQB):
                    bank, bb = pbk.next()
                    proj_fm(w_sb, g * 128, 128, 8, xnT, qb, bank, bb)
                    if g < 2:
                        evac_copy(QT[:, g, qb * 512:(qb + 1) * 512], bank[:, :], [bb], [], scale=0.125)
                    else:
                        for hf in range(2):
                            rr = slice(hf * 64, (hf + 1) * 64)
                            evac_copy(KT[rr, (g - 2) * 2 + hf, qb * 512:(qb + 1) * 512], bank[rr, :], [bb], [])
            for tb in range(NTB):
                bank, bb = pbk.next()
                for c in range(8):
                    s.op("pe", lambda e: e.matmul(bank[:, 0:256], lhsT=xnT[:, c, tb * 128:(tb + 1) * 128],
                                                  rhs=w_sb[:, c, 512:768], start=(c == 0), stop=(c == 7)),
                         writes=[bb], inc=(c == 7))
                evac_copy(Va[:, tb, :, 0:64], bank[:, 0:256].rearrange("p (h d) -> p h d", h=4), [bb], [])
            s.barrier()
            pw.close()
            sbk = Rot([(banks[i], bbuf[i]) for i in range(4)])
            obk = Rot([(banks[4 + i], bbuf[4 + i]) for i in range(2)])
            pTs = Rot([(T(f"pTa{l}_{i}", [128, 320], BF16, ps), Buf()) for i in range(4)])
            fin = Rot([(T(f"rdenA{l}_{i}", [65, 1024], F32, ps), Buf(), banks[6 + i], bbuf[6 + i],
                        T(f"bcsA{l}_{i}", [64, 512], F32, ps), Buf(),
                        T(f"obfA{l}_{i}", [64, 512], BF16, ps), Buf()) for i in range(2)])
            pend = []

            def na_back(ctx):
                hh_, ri_, r8_, nb_, start_, O_bank, O_buf, pT, b_pT, fin_t = ctx
                flush_fin()
                for b in range(nb_):
                    kb = (start_ * 64 + b * 128) // 128
                    s.op("pe", lambda e: e.matmul(O_bank[0:65, ri_ * 64:(ri_ + 1) * 64],
                                                  lhsT=Va[:, kb, hh_, :], rhs=pT[:, b * 64:(b + 1) * 64],
                                                  start=(b == 0), stop=(b == nb_ - 1)),
                         reads=[b_pT], writes=[O_buf], inc=(b == nb_ - 1))
                if ri_ == 7:
                    finalize_heads(O_bank, O_buf, hh_, r8_ * 512, fin_t, use_act=True)

            for hh in range(4):
                t, pb = hh // 2, (hh % 2) * 64
                for r8 in range(8):
                    O_bank, O_buf = obk.next()
                    fin_t = fin.next()
                    for ri in range(8):
                        r = r8 * 8 + ri
                        rs, start, nb = _na_row_info(r)
                        v = NA_VMAP[r]
                        w = nb * 64
                        S_bank, S_buf = sbk.next()
                        pT, b_pT = pTs.next()
                        s.op("pe", lambda e: e.matmul(S_bank[:, 0:w], lhsT=ident_bf[:], rhs=rpb_sb[:, v * 4 + hh, 0:w],
                                                      start=True, stop=False), writes=[S_buf], inc=False)
                        for b in range(nb):
                            k0 = start * 64 + b * 128
                            s.op("pe", lambda e: e.matmul(S_bank[:, b * 64:(b + 1) * 64],
                                                          lhsT=KT[:, hh, k0:k0 + 128],
                                                          rhs=QT[:, t, r * 64:(r + 1) * 64],
                                                          start=False, stop=(b == nb - 1)),
                                 writes=[S_buf], inc=(b == nb - 1))
                        s.op("act", lambda e: e.activation(out=pT[:, 0:w], in_=S_bank[:, 0:w], func=AF.Exp),
                             reads=[S_buf], writes=[b_pT])
                        pend.append((hh, ri, r8, nb, start, O_bank, O_buf, pT, b_pT, fin_t))
                        if len(pend) > 3:
                            na_back(pend.pop(0))
            while pend:
                na_back(pend.pop(0))
            flush_fin(force=True)
            s.barrier()
        nst.close()
        if stop_after == "na":
            lst.close(); lat.close()
            break

        with ExitStack() as ps:
            wq = T(f"wq{l}", [128, 8, 384], BF16, ps)
            wqp = T(f"wqp{l}", [128, 8, 384], BF16, ps)
            wk = T(f"wk{l}", [128, 8, 128], BF16, ps)
            wkp = T(f"wkp{l}", [128, 8, 128], BF16, ps)
            wv = T(f"wv{l}", [128, 8, 128], BF16, ps)
            b_w = Buf()
            for dst, srcw in ((wq, w_cq_d), (wqp, w_cqp_d), (wk, w_ck_d), (wkp, w_ckp_d), (wv, w_cv_d)):
                load_w_cast(dst[:], srcw[l].rearrange("(c p) n -> p c n", p=128), b_w)
            tabs = Rot([(T(f"cosc{l}_{i}", [128, 512], F32, ps), T(f"sinc{l}_{i}", [128, 512], F32, ps), Buf()) for i in range(2)])
            msk = T(f"msk{l}", [128, 6, 512], BF16, ps)
            s.dma("sp", lambda e: e.dma_start(out=msk[:], in_=swa_mask_d.rearrange("j p c -> p j c")), writes=[b_w])
            esink = T(f"esink{l}", [65, 6], F32, ps)
            s.dma("sp", lambda e: e.dma_start(out=esink[64:65, :], in_=sink_d[l:l + 1, :]), writes=[b_w])
            QT = T(f"QTc{l}", [128, 3, S], BF16, ps)
            KT = T(f"KTc{l}", [128, 2, S], BF16, ps)
            s.op("pool", lambda e: e.memset(KT[:], 0.0), writes=[b_w])
            Vc = T(f"Vc{l}", [128, NTB, 2, 65], BF16, ps)
            s.op("pool", lambda e: e.memset(Vc[:, :, :, 64:65], 1.0), writes=[b_w])
            s.barrier()
            s.op("act", lambda e: e.activation(out=esink[64:65, :], in_=esink[64:65, :], func=AF.Exp))
            pbk = Rot([(banks[2 * i], bbuf[2 * i], banks[2 * i + 1], bbuf[2 * i + 1]) for i in range(4)])
            t1s = Rot([(T(f"t1c{l}_{i}", [128, 512], F32, ps), Buf()) for i in range(2)])
            t2s = Rot([(T(f"t2c{l}_{i}", [128, 512], F32, ps), Buf()) for i in range(2)])
            for g in range(4):
                for qb in range(NQB):
                    bq, bbq, bp, bbp = pbk.next()
                    if g < 3:
                        proj_fm(wq, g * 128, 128, 8, xnT, qb, bq, bbq)
                        proj_fm(wqp, g * 128, 128, 8, xnT, qb, bp, bbp)
                        dst = QT[:, g, qb * 512:(qb + 1) * 512]
                    else:
                        proj_fm(wk, 0, 128, 8, xnT, qb, bq, bbq)
                        proj_fm(wkp, 0, 128, 8, xnT, qb, bp, bbp)
                        dst = None
                    t1, b_t1 = t1s.next()
                    t2, b_t2 = t2s.next()
                    ctab, stab, b_tab = tabs.next()
                    s.dma("sp", lambda e: e.dma_start(out=ctab[:], in_=cs_c_d[:, qb * 512:(qb + 1) * 512]), writes=[b_tab])
                    s.dma("sp", lambda e: e.dma_start(out=stab[:], in_=sn_c_d[:, qb * 512:(qb + 1) * 512]), writes=[b_tab])
                    s.op("dve", lambda e: e.tensor_tensor(out=t1[:], in0=bq[:, :], in1=ctab[:], op=ALU.mult), reads=[bbq, b_tab], writes=[b_t1])
                    s.op("dve", lambda e: e.tensor_tensor(out=t2[:], in0=bp[:, :], in1=stab[:], op=ALU.mult), reads=[bbp, b_tab], writes=[b_t2])
                    if g < 3:
                        s.op("pool", lambda e: e.tensor_tensor(out=t1[:], in0=t1[:], in1=t2[:], op=ALU.add), reads=[b_t1, b_t2], writes=[b_t1])
                        s.op("act", lambda e: e.mul(out=dst, in_=t1[:], mul=0.125), reads=[b_t1])
                    else:
                        for kv_ in range(2):
                            rr = slice(kv_ * 64, (kv_ + 1) * 64)
                            s.op("pool", lambda e: e.tensor_tensor(out=KT[rr, kv_, qb * 512:(qb + 1) * 512], in0=t1[rr, :], in1=t2[rr, :], op=ALU.add),
                                 reads=[b_t1, b_t2])
            pb2 = Rot([(banks[i], bbuf[i]) for i in range(4)])
            for tb in range(NTB):
                bank, bb = pb2.next()
                for c in range(8):
                    s.op("pe", lambda e: e.matmul(bank[:, 0:128], lhsT=xnT[:, c, tb * 128:(tb + 1) * 128],
                                                  rhs=wv[:, c, :], start=(c == 0), stop=(c == 7)),
                         writes=[bb], inc=(c == 7))
                evac_copy(Vc[:, tb, :, 0:64], bank[:, 0:128].rearrange("p (h d) -> p h d", h=2), [bb], [])
            s.barrier()
            spairs = Rot([(pp[0], [bbuf[0], bbuf[1]]), (pp[1], [bbuf[2], bbuf[3]]), (pp[2], [bbuf[4], bbuf[5]])])
            obk = Rot([(banks[6 + i], bbuf[6 + i]) for i in range(2)])
            pTs = Rot([(T(f"pTc{l}_{i}", [128, 1024], BF16, ps), Buf()) for i in range(4)])
            fin = Rot([(T(f"rdenC{l}_{i}", [65, 1024], F32, ps), Buf(),
                        T(f"bcsC{l}_{i}", [64, 512], F32, ps), Buf(),
                        T(f"obfC{l}_{i}", [64, 512], BF16, ps), Buf()) for i in range(2)])
            pend = []
            fin_age[0] = 1

            def swa_back(ctx):
                hh_, qb_, grp, first, last, O_bank, O_buf, pT, b_pT, fin_t = ctx
                flush_fin()
                kvh_ = hh_ // 3
                for u, kb in enumerate(grp):
                    s.op("pe", lambda e: e.matmul(O_bank[0:65, :], lhsT=Vc[:, kb, kvh_, :], rhs=pT[:, u * 512:(u + 1) * 512],
                                                  start=(first and u == 0), stop=(last and u == len(grp) - 1)),
                         reads=[b_pT], writes=[O_buf], inc=(u == len(grp) - 1))
                if last:
                    f_ = fin_t
                    finalize_heads(O_bank, O_buf, 10 + hh_, qb_ * 512, (f_[0], f_[1], None, None, f_[2], f_[3], f_[4], f_[5]),
                                   extra_den=esink[64:65, hh_:hh_ + 1], bank_src=spairs, use_act=True)

            for hh in range(6):
                kvh = hh // 3
                t = hh % 3
                pb = kvh * 64
                for qb in range(NQB):
                    O_bank, O_buf = obk.next()
                    fin_t = fin.next()
                    kbs = [kb for kb in range(4 * qb - 1, 4 * qb + 5) if 0 <= kb < NTB]
                    grps = [kbs[i:i + 2] for i in range(0, len(kbs), 2)]
                    for gi, grp in enumerate(grps):
                        Sp, b_Sp = spairs.next()
                        pT, b_pT = pTs.next()
                        for u, kb in enumerate(grp):
                            j = kb - (4 * qb - 1)
                            s.op("pe", lambda e: e.matmul(Sp[:, u * 512:(u + 1) * 512], lhsT=KT[:, kvh, kb * 128:(kb + 1) * 128],
                                                          rhs=QT[:, t, qb * 512:(qb + 1) * 512],
                                                          start=True, stop=True), writes=b_Sp, inc=(u == len(grp) - 1))
                        wdt = len(grp) * 512
                        j0 = grp[0] - (4 * qb - 1)
                        s.op("act", lambda e: e.activation(out=pT[:, 0:wdt], in_=Sp[:, 0:wdt], func=AF.Exp),
                             reads=b_Sp, writes=[b_pT])
                        s.op("dve", lambda e: e.tensor_tensor(out=pT[:, 0:wdt], in0=pT[:, 0:wdt],
                                                              in1=msk[:, j0:j0 + len(grp), :].rearrange("p j c -> p (j c)"), op=ALU.mult),
                             reads=[b_pT], writes=[b_pT])
                        pend.append((hh, qb, grp, gi == 0, gi == len(grps) - 1, O_bank, O_buf, pT, b_pT, fin_t))
                        if len(pend) > 2:
                            swa_back(pend.pop(0))
            while pend:
                swa_back(pend.pop(0))
            flush_fin(force=True)
            fin_age[0] = 3
            s.barrier()
        if stop_after == "swa":
            lst.close(); lat.close()
            break

        with ExitStack() as ps:
            cosB = T(f"cosb{l}", [96, S], F32, ps)
            sinB = T(f"sinb{l}", [96, S], F32, ps)
            wl = T(f"wl{l}", [128, 8, 384], BF16, ps)
            wkr = T(f"wkr{l}", [128, 8, 96], BF16, ps)
            wkrp = T(f"wkrp{l}", [128, 8, 96], BF16, ps)
            qn_t = T(f"qn{l}", [128, 2], F32, ps)
            kvn_t = T(f"kvn{l}", [128, 1], F32, ps)
            b_w = Buf()
            load_w_cast(wl[:], w_lat_d[l].rearrange("(c p) n -> p c n", p=128), b_w)
            load_w_cast(wkr[:], w_kr_d[l].rearrange("(c p) n -> p c n", p=128), b_w)
            load_w_cast(wkrp[:], w_krp_d[l].rearrange("(c p) n -> p c n", p=128), b_w)
            s.dma("sp", lambda e: e.dma_start(out=qn_t[:], in_=qn_d[l].rearrange("(c p) -> p c", p=128), allow_slow_non_contiguous=True), writes=[b_w])
            s.dma("sp", lambda e: e.dma_start(out=kvn_t[:], in_=kvn_d[l].rearrange("(c p) -> p c", p=128), allow_slow_non_contiguous=True), writes=[b_w])
            s.dma("sp", lambda e: e.dma_start(out=cosB[64:96, :], in_=cs_b_d), writes=[b_w])
            s.dma("sp", lambda e: e.dma_start(out=sinB[64:96, :], in_=sn_b_d), writes=[b_w])
            s.barrier()
            rb = Rot([(banks[i], bbuf[i]) for i in range(3)])
            sqs = Rot([(T(f"sq{l}_{i}", [128, 512], BF16, ps), Buf()) for i in range(3)])
            rsb = Rot([(T(f"rsb{l}_{i}", [128, 512], F32, ps), Buf()) for i in range(2)])
            rscr = T(f"rscr{l}", [128, 512], F32, ps)
            ssb = Rot([(banks[3 + i], bbuf[3 + i]) for i in range(2)])
            for qb in range(NQB):
                cols = slice(qb * 512, (qb + 1) * 512)
                raws = []
                for c2 in range(2):
                    bank, bb = rb.next()
                    proj_fm(wl, c2 * 128, 128, 8, xnT, qb, bank, bb)
                    sq, b_sq = sqs.next()
                    s.op("act", lambda e: e.activation(out=sq[:], in_=bank[:, :], func=AF.Square), reads=[bb], writes=[b_sq])
                    raws.append((bank, bb, sq, b_sq))
                ss_bank, ss_buf = ssb.next()
                for c2 in range(2):
                    s.op("pe", lambda e: e.matmul(ss_bank[:, :], lhsT=ones_bf[:], rhs=raws[c2][2][:], start=(c2 == 0), stop=(c2 == 1)),
                         reads=[raws[c2][3]], writes=[ss_buf], inc=(c2 == 1))
                rs_t, b_rs = rsb.next()
                rstd_big(ss_bank[:, :], rs_t[:], rscr[:], 1.0 / 256, [ss_buf], [b_rs])
                for c2 in range(2):
                    bank, bb = raws[c2][0], raws[c2][1]
                    s.op("dve", lambda e: e.scalar_tensor_tensor(out=cqn[:, c2, cols], in0=bank[:, :], scalar=qn_t[:, c2:c2 + 1],
                                                                 in1=rs_t[:], op0=ALU.mult, op1=ALU.mult), reads=[bb, b_rs])
                bank, bb = rb.next()
                proj_fm(wl, 256, 128, 8, xnT, qb, bank, bb)
                sq, b_sq = sqs.next()
                s.op("act", lambda e: e.activation(out=sq[:], in_=bank[:, :], func=AF.Square), reads=[bb], writes=[b_sq])
                ss_bank, ss_buf = ssb.next()
                s.op("pe", lambda e: e.matmul(ss_bank[:, :], lhsT=ones_bf[:], rhs=sq[:], start=True, stop=True), reads=[b_sq], writes=[ss_buf])
                rs_t, b_rs = rsb.next()
                rstd_big(ss_bank[:, :], rs_t[:], rscr[:], 1.0 / 128, [ss_buf], [b_rs])
                s.op("dve", lambda e: e.scalar_tensor_tensor(out=ckvn[:, cols], in0=bank[:, :], scalar=kvn_t[:, 0:1],
                                                             in1=rs_t[:], op0=ALU.mult, op1=ALU.mult), reads=[bb, b_rs])
                bank, bb = rb.next()
                proj_fm(wkr, 0, 96, 8, xnT, qb, bank, bb)
                bank2, bb2 = rb.next()
                proj_fm(wkrp, 0, 96, 8, xnT, qb, bank2, bb2)
                t1, b_t1 = rsb.next()
                t2, b_t2 = rsb.next()
                s.op("dve", lambda e: e.tensor_tensor(out=t1[64:96, :], in0=bank[64:96, :], in1=cosB[64:96, cols], op=ALU.mult), reads=[bb], writes=[b_t1])
                s.op("dve", lambda e: e.tensor_tensor(out=t2[64:96, :], in0=bank2[64:96, :], in1=sinB[64:96, cols], op=ALU.mult), reads=[bb2], writes=[b_t2])
                s.op("pool", lambda e: e.tensor_tensor(out=kpe[64:96, cols], in0=t1[64:96, :], in1=t2[64:96, :], op=ALU.add), reads=[b_t1, b_t2])
            s.barrier()
        lst.close()
        with ExitStack() as ps:
            Vb = T(f"Vb{l}", [128, NTB, 6, 65], BF16, ps)
            cosB = T(f"cosb2{l}", [96, S], F32, ps)
            sinB = T(f"sinb2{l}", [96, S], F32, ps)
            wuq = T(f"wuq{l}", [128, 2, 576], BF16, ps)
            wuqp = T(f"wuqp{l}", [128, 2, 576], BF16, ps)
            wuk = T(f"wuk{l}", [128, 384], BF16, ps)
            wuv = T(f"wuv{l}", [128, 384], BF16, ps)
            b_w = Buf()
            load_w_cast(wuq[:], w_uq_d[l].rearrange("(c p) n -> p c n", p=128), b_w)
            load_w_cast(wuqp[:], w_uqp_d[l].rearrange("(c p) n -> p c n", p=128), b_w)
            load_w_cast(wuk[:], w_uk_d[l], b_w)
            load_w_cast(wuv[:], w_uv_d[l], b_w)
            s.dma("sp", lambda e: e.dma_start(out=cosB[64:96, :], in_=cs_b_d), writes=[b_w])
            s.dma("sp", lambda e: e.dma_start(out=sinB[64:96, :], in_=sn_b_d), writes=[b_w])
            s.op("pool", lambda e: e.memset(Vb[:, :, :, 64:65], 1.0), writes=[b_w])
            s.barrier()
            pb2 = Rot([(banks[i], bbuf[i]) for i in range(4)])
            for tb in range(NTB):
                bank, bb = pb2.next()
                s.op("pe", lambda e: e.matmul(bank[:, 0:384], lhsT=ckvn[:, tb * 128:(tb + 1) * 128], rhs=wuv[:, :], start=True, stop=True), writes=[bb])
                evac_copy(Vb[:, tb, :, 0:64], bank[:, 0:384].rearrange("p (h d) -> p h d", h=6), [bb], [])
            s.barrier()
            scale_b = float(96 ** -0.5)
            QTs = [(T(f"QTb{l}_{i}", [96, S], BF16, ps), Buf()) for i in range(2)]
            KTs = [(T(f"KTb{l}_{i}", [96, S], BF16, ps), Buf()) for i in range(2)]
            t1s = Rot([(T(f"t1b{l}_{i}", [96, 512], F32, ps), Buf()) for i in range(2)])
            t2s = Rot([(T(f"t2b{l}_{i}", [96, 512], F32, ps), Buf()) for i in range(2)])
            pbk = Rot([(banks[i], bbuf[i]) for i in range(3)])
            spairs = Rot([(pp[0], [bbuf[0], bbuf[1]]), (pp[1], [bbuf[2], bbuf[3]]), (pp[2], [bbuf[4], bbuf[5]])])
            obk = Rot([(banks[6 + i], bbuf[6 + i]) for i in range(2)])
            pTs = Rot([(T(f"pTb{l}_{i}", [128, 1024], BF16, ps), Buf()) for i in range(4)])
            finB = Rot([(T(f"rdenB{l}_{i}", [65, 1024], F32, ps), Buf(),
                         T(f"bcsB{l}_{i}", [64, 512], F32, ps), Buf(),
                         T(f"obfB{l}_{i}", [64, 512], BF16, ps), Buf()) for i in range(2)])
            pend = []

            def mla_back(ctx):
                hh_, qb_, kp_, O_bank, O_buf, pT, b_pT, fin_t = ctx
                flush_fin()
                for u in range(2):
                    kb = 2 * kp_ + u
                    s.op("pe", lambda e: e.matmul(O_bank[0:65, :], lhsT=Vb[:, kb, hh_, :], rhs=pT[:, u * 512:(u + 1) * 512],
                                                  start=(kb == 0), stop=(kb == NTB - 1)),
                         reads=[b_pT], writes=[O_buf], inc=(u == 1))
                if kp_ == NTB // 2 - 1:
                    f_ = fin_t
                    finalize_heads(O_bank, O_buf, 4 + hh_, qb_ * 512, (f_[0], f_[1], None, None, f_[2], f_[3], f_[4], f_[5]), bank_src=spairs)

            for hh in range(6):
                QTh, b_Q = QTs[hh % 2]
                KTh, b_K = KTs[hh % 2]
                for qb in range(NQB):
                    cols = slice(qb * 512, (qb + 1) * 512)
                    bq, bbq = pbk.next()
                    proj_fm(wuq, hh * 96, 96, 2, cqn, qb, bq, bbq)
                    bp, bbp = pbk.next()
                    proj_fm(wuqp, hh * 96, 96, 2, cqn, qb, bp, bbp)
                    s.op("dve", lambda e: e.tensor_copy(out=QTh[0:64, cols], in_=bq[0:64, :]), reads=[bbq], writes=[b_Q])
                    t1, b_t1 = t1s.next()
                    t2, b_t2 = t2s.next()
                    s.op("dve", lambda e: e.tensor_tensor(out=t1[64:96, :], in0=bq[64:96, :], in1=cosB[64:96, cols], op=ALU.mult), reads=[bbq], writes=[b_t1])
                    s.op("dve", lambda e: e.tensor_tensor(out=t2[64:96, :], in0=bp[64:96, :], in1=sinB[64:96, cols], op=ALU.mult), reads=[bbp], writes=[b_t2])
                    s.op("pool", lambda e: e.tensor_tensor(out=QTh[64:96, cols], in0=t1[64:96, :], in1=t2[64:96, :], op=ALU.add),
                         reads=[b_t1, b_t2], writes=[b_Q])
                    bk, bbk = pbk.next()
                    s.op("pe", lambda e: e.matmul(bk[0:64, :], lhsT=wuk[:, hh * 64:(hh + 1) * 64], rhs=ckvn[:, cols], start=True, stop=True), writes=[bbk])
                    s.op("dve", lambda e: e.tensor_copy(out=KTh[0:64, cols], in_=bk[0:64, :]), reads=[bbk], writes=[b_K])
                    s.op("pool", lambda e: e.tensor_copy(out=KTh[64:96, cols], in_=kpe[64:96, cols]), writes=[b_K])
                for qb in range(NQB):
                    O_bank, O_buf = obk.next()
                    fin_t = finB.next()
                    for kp in range(NTB // 2):
                        Sp, b_Sp = spairs.next()
                        pT, b_pT = pTs.next()
                        for u in range(2):
                            kb = 2 * kp + u
                            s.op("pe", lambda e: e.matmul(Sp[:, u * 512:(u + 1) * 512], lhsT=KTh[0:96, kb * 128:(kb + 1) * 128],
                                                          rhs=QTh[0:96, qb * 512:(qb + 1) * 512], start=True, stop=True),
                                 reads=[b_Q, b_K], writes=b_Sp, inc=(u == 1))
                        s.op("act", lambda e: e.activation(out=pT[:], in_=Sp[:, :], func=AF.Exp, scale=scale_b),
                             reads=b_Sp, writes=[b_pT])
                        pend.append((hh, qb, kp, O_bank, O_buf, pT, b_pT, fin_t))
                        if len(pend) > 2:
                            mla_back(pend.pop(0))
            while pend:
                mla_back(pend.pop(0))
            flush_fin(force=True)
            s.barrier()
        lat.close()
        if stop_after == "mla":
            break

        sel = ExitStack()
        aff_all = T(f"aff{l}", [128, NTB, NE], F32, sel)
        affT = T(f"affT{l}", [128, 512], F32, sel)
        idsT = T(f"idsT{l}", [128, 64], U32, sel)
        gT = T(f"gT{l}", [128, 64], F32, sel)
        with ExitStack() as ps:
            Wo = T(f"Wo{l}", [128, 8, D], BF16, ps)
            gn_t = T(f"gn{l}", [128, 8], F32, ps)
            g2_bc = T(f"g2{l}", [128, D], F32, ps)
            wr_sb = T(f"wr{l}", [128, 8, NE], F32, ps)
            invw = T(f"invw{l}", [128, 3], F32, ps)
            b_w = Buf()
            s.dma("sp", lambda e: e.dma_start(out=gn_t[:], in_=gn_d[l].rearrange("(j p) -> p j", p=128), allow_slow_non_contiguous=True), writes=[b_w])
            s.dma("sp", lambda e: e.dma_start(out=g2_bc[:], in_=ffn_norm_d[l:l + 1, :].partition_broadcast(128)), writes=[b_w])
            s.dma("sp", lambda e: e.dma_start(out=wr_sb[:], in_=w_router_d[l].rearrange("(c p) n -> p c n", p=128)), writes=[b_w])
            s.dma("sp", lambda e: e.dma_start(out=invw[:], in_=invw_d), writes=[b_w])
            stg = Rot([(T(f"wstg{l}_{i}", [128, D], F32, ps), Buf()) for i in range(2)])
            for j in range(8):
                st_, b_st = stg.next()
                s.dma("sp", lambda e: e.dma_start(out=st_[:], in_=w_out_d[l, j * 128:(j + 1) * 128, :]), writes=[b_st])
                s.op("dve", lambda e: e.tensor_scalar(out=Wo[:, j, :], in0=st_[:], scalar1=gn_t[:, j:j + 1], scalar2=None, op0=ALU.mult),
                     reads=[b_st, b_w])
            Zs = [(T(f"Z{l}_{i}", [128, 128], F32, ps), Buf()) for i in range(8)]
            for z, bz in Zs:
                s.op("pool", lambda e: e.memset(z[:], 0.0), writes=[bz])
            s.barrier()
            ocs = Rot([(T(f"oc{l}_{i}", [128, 8, 512], BF16, ps), Buf()) for i in range(2)])
            osq = T(f"osq{l}", [128, 8, 512], BF16, ps)
            b_osq = Buf()
            hts = Rot([(T(f"h3_{l}_{i}", [128, D], F32, ps), Buf()) for i in range(3)])
            h1s = Rot([(T(f"h1_{l}_{i}", [128, D], F32, ps), Buf()) for i in range(4)])
            xfs = Rot([(T(f"xf_{l}_{i}", [128, D], F32, ps), Buf()) for i in range(3)])
            xbs = Rot([(T(f"xb3_{l}_{i}", [128, D], BF16, ps), Buf()) for i in range(2)])
            xTs = Rot([(T(f"xT3_{l}_{i}", [128, 8, 128], F32, ps), Buf()) for i in range(2)])
            junk = T(f"junk3_{l}", [128, D], BF16, ps)
            b_junk = Buf()
            smalls = Rot([(T(f"sm{l}_{i}", [128, 8], F32, ps), Buf()) for i in range(6)])
            lgs = Rot([(T(f"lg{l}_{i}", [128, NE], F32, ps), Buf()) for i in range(2)])
            mix_heads = [[0, 1], [2, 3, 4], [5, 6, 7]]
            obk = Rot([(banks[0], bbuf[0], banks[1], bbuf[1]), (banks[2], bbuf[2], banks[3], bbuf[3])])
            ssb = Rot([(banks[4], bbuf[4])])
            tpk = Rot([(banks[5], bbuf[5], banks[6], bbuf[6])])
            lgb = Rot([(banks[7], bbuf[7])])
            p3_xf = {}

            def p3_stage_b1(tb, h1, b_h1, sm, b_sm):
                s.op("dve", lambda e: e.scalar_tensor_tensor(out=junk[:], in0=h1[:], scalar=1.0, in1=h1[:],
                                                             op0=ALU.mult, op1=ALU.mult, accum_out=sm[:, 3:4]),
                     reads=[b_h1], writes=[b_junk, b_sm])
                s.op("dve", lambda e: e.tensor_scalar(out=sm[:, 3:4], in0=sm[:, 3:4], scalar1=1.0 / D, scalar2=EPS, op0=ALU.mult, op1=ALU.add),
                     reads=[b_sm], writes=[b_sm])
                s.op("act", lambda e: e.activation(out=sm[:, 3:4], in_=sm[:, 3:4], func=AF.Ln), reads=[b_sm], writes=[b_sm])
                s.op("act", lambda e: e.activation(out=sm[:, 3:4], in_=sm[:, 3:4], func=AF.Exp, scale=-0.5), reads=[b_sm], writes=[b_sm])
                xf, b_xf = xfs.next()
                xb, b_xb = xbs.next()
                s.op("dve", lambda e: e.scalar_tensor_tensor(out=xf[:], in0=h1[:], scalar=sm[:, 3:4], in1=g2_bc[:],
                                                             op0=ALU.mult, op1=ALU.mult), reads=[b_h1, b_sm], writes=[b_xf])
                s.op("act", lambda e: e.copy(out=xb[:], in_=xf[:]), reads=[b_xf], writes=[b_xb])
                s.dma("pool", lambda e: e.dma_start(out=xn2_d[tb * 128:(tb + 1) * 128, :], in_=xb[:]), reads=[b_xb])
                p3_xf[tb] = (xf, b_xf)

            def p3_stage_b2(tb, h1, b_h1, sm, b_sm):
                xf, b_xf = p3_xf.pop(tb)
                t0, bt0, t1_, bt1 = tpk.next()
                for c in range(8):
                    bk_, bbk_ = (t0, bt0) if c < 4 else (t1_, bt1)
                    s.op("pe", lambda e: e.transpose(out=bk_[:, (c % 4) * 128:(c % 4 + 1) * 128], in_=xf[:, c * 128:(c + 1) * 128], identity=ident_f[:]),
                         reads=[b_xf], writes=[bbk_], inc=(c % 4 == 3))
                xT, b_xT = xTs.next()
                s.op("act", lambda e: e.copy(out=xT[:, 0:4, :], in_=t0[:, :].rearrange("p (c t) -> p c t", c=4)), reads=[bt0], writes=[b_xT])
                s.op("act", lambda e: e.copy(out=xT[:, 4:8, :], in_=t1_[:, :].rearrange("p (c t) -> p c t", c=4)), reads=[bt1], writes=[b_xT])
                lb, blb = lgb.next()
                for c in range(8):
                    s.op("pe", lambda e: e.matmul(lb[:, 0:NE], lhsT=xT[:, c, :], rhs=wr_sb[:, c, :], start=(c == 0), stop=(c == 7)),
                         reads=[b_xT], writes=[blb], inc=(c == 7))
                lg, b_lg = lgs.next()
                s.op("dve", lambda e: e.reduce_max(out=sm[:, 4:5], in_=lb[:, 0:NE], axis=mybir.AxisListType.X), reads=[blb], writes=[b_sm])
                s.op("dve", lambda e: e.tensor_scalar(out=sm[:, 4:5], in0=sm[:, 4:5], scalar1=-1.0, scalar2=None, op0=ALU.mult), reads=[b_sm], writes=[b_sm])
                s.op("act", lambda e: e.activation(out=lg[:], in_=lb[:, 0:NE], func=AF.Exp, bias=sm[:, 4:5], accum_out=sm[:, 5:6]),
                     reads=[blb, b_sm], writes=[b_lg, b_sm])
                s.op("dve", lambda e: e.reciprocal(out=sm[:, 5:6], in_=sm[:, 5:6]), reads=[b_sm], writes=[b_sm])
                s.op("dve", lambda e: e.tensor_scalar(out=aff_all[:, tb, :], in0=lg[:], scalar1=sm[:, 5:6], scalar2=None, op0=ALU.mult),
                     reads=[b_lg, b_sm])

            p3_pend = []
            for qb in range(NQB):
                oc, b_oc = ocs.next()
                ocv = oc_d.rearrange("(jj two) p t -> two p jj t", two=2)
                for two in range(2):
                    s.dma("sp", lambda e: e.dma_start(out=oc[two * 64:(two + 1) * 64, :, :], in_=ocv[two][:, :, qb * 512:(qb + 1) * 512]), writes=[b_oc])
                s.op("pool", lambda e: e.tensor_tensor(out=osq[:], in0=oc[:], in1=oc[:], op=ALU.mult), reads=[b_oc], writes=[b_osq])
                for sub in range(4):
                    tb = qb * 4 + sub
                    tc_ = slice(sub * 128, (sub + 1) * 128)
                    ht, b_ht = hts.next()
                    s.dma("sp", lambda e: e.dma_start(out=ht[:], in_=src_d[tb * 128:(tb + 1) * 128, :]), writes=[b_ht])
                    ss_bank, ss_buf = ssb.next()
                    for m in range(3):
                        hs = mix_heads[m]
                        for i, j in enumerate(hs):
                            s.op("pe", lambda e: e.matmul(ss_bank[:, m:m + 1], lhsT=osq[:, j, tc_], rhs=ones_bf[:, 0:1],
                                                          start=(i == 0), stop=(i == len(hs) - 1)),
                                 reads=[b_osq], writes=[ss_buf], inc=(i == len(hs) - 1))
                    sm, b_sm = smalls.next()
                    s.op("dve", lambda e: e.tensor_tensor(out=sm[:, 0:3], in0=ss_bank[:, 0:3], in1=invw[:], op=ALU.mult), reads=[ss_buf], writes=[b_sm])
                    s.op("dve", lambda e: e.tensor_scalar(out=sm[:, 0:3], in0=sm[:, 0:3], scalar1=EPS, scalar2=None, op0=ALU.add), reads=[b_sm], writes=[b_sm])
                    s.op("act", lambda e: e.activation(out=sm[:, 0:3], in_=sm[:, 0:3], func=AF.Ln), reads=[b_sm], writes=[b_sm])
                    s.op("act", lambda e: e.activation(out=sm[:, 0:3], in_=sm[:, 0:3], func=AF.Exp, scale=-0.5), reads=[b_sm], writes=[b_sm])
                    if p3_pend:
                        p3_stage_b1(*p3_pend[0])
                    h1, b_h1 = h1s.next()
                    prev, b_prev = ht, b_ht
                    for m in range(3):
                        hs = mix_heads[m]
                        b0, bb0, b1, bb1 = obk.next()
                        for half, (bk_, bbk_) in enumerate(((b0, bb0), (b1, bb1))):
                            for i, j in enumerate(hs):
                                s.op("pe", lambda e: e.matmul(bk_[:, :], lhsT=oc[:, j, tc_], rhs=Wo[:, j, half * 512:(half + 1) * 512],
                                                              start=(i == 0), stop=(i == len(hs) - 1)),
                                     reads=[b_oc], writes=[bbk_], inc=(i == len(hs) - 1))
                            hc = slice(half * 512, (half + 1) * 512)
                            s.op("dve", lambda e: e.scalar_tensor_tensor(out=h1[:, hc], in0=bk_[:, :], scalar=sm[:, m:m + 1], in1=prev[:, hc],
                                                                         op0=ALU.mult, op1=ALU.add),
                                 reads=[bbk_, b_sm, b_prev], writes=[b_h1])
                        prev, b_prev = h1, b_h1
                    s.dma("pool", lambda e: e.dma_start(out=h_d[tb * 128:(tb + 1) * 128, :], in_=h1[:]), reads=[b_h1])
                    if p3_pend:
                        p3_stage_b2(*p3_pend.pop(0))
                    p3_pend.append((tb, h1, b_h1, sm, b_sm))
            while p3_pend:
                p3_stage_b1(*p3_pend[0])
                p3_stage_b2(*p3_pend.pop(0))
            s.barrier()
            for jc in range(4):
                for part in range(8):
                    tb = part * 4 + jc
                    z, bz = Zs[part]
                    zv = z[:].rearrange("p (e q) -> p e q", q=8)[:, :, part:part + 1]
                    s.op("dve", lambda e: e.tensor_copy(out=zv, in_=aff_all[:, tb, :].rearrange("p (e o) -> p e o", o=1)), writes=[bz])
                    s.op("pe", lambda e: e.matmul(banks[0][:, jc * 128:(jc + 1) * 128], lhsT=z[:], rhs=ident_f[:],
                                                  start=(part == 0), stop=(part == 7)), reads=[bz], writes=[bbuf[0]])
            s.op("act", lambda e: e.copy(out=affT[:], in_=banks[0][:, :]), reads=[bbuf[0]])
            s.barrier()
        if stop_after == "p3":
            sel.close()
            break

        wst = ExitStack()
        Ws = [(T(f"Wg{l}_{i}", [128, 8, D], BF16, wst), T(f"Wu{l}_{i}", [128, 8, D], BF16, wst),
               T(f"Wd{l}_{i}", [128, 8, D], BF16, wst), Buf()) for i in range(2)]
        stg = Rot([(T(f"wst{l}_{i}", [128, D], F32, wst), Buf()) for i in range(8)])

        def load_expert_steps(e_):
            Wg, Wu, Wd, b_W = Ws[e_ % 2]
            steps = []
            for dst, srcw in ((Wg, w_gate_d), (Wu, w_up_d), (Wd, w_down_d)):
                for c in range(8):
                    def step(dst=dst, srcw=srcw, c=c):
                        st_, b_st = stg.next()
                        s.dma("sp", lambda e: e.dma_start(out=st_[:], in_=srcw[l, e_, c * 128:(c + 1) * 128, :]), writes=[b_st])
                        s.op("act", lambda e: e.copy(out=dst[:, c, :], in_=st_[:]), reads=[b_st], writes=[b_W])
                    steps.append(step)
            return steps

        def load_expert(e_):
            for st in load_expert_steps(e_):
                st()

        load_expert(0)
        with ExitStack() as ps:
            gmat = T(f"gmat{l}", [128, 128], F32, ps)
            rowoff = T(f"rowoff{l}", [128, 1], F32, ps)
            b_c = Buf()
            s.dma("sp", lambda e: e.dma_start(out=gmat[:], in_=gmat_d), writes=[b_c])
            s.dma("sp", lambda e: e.dma_start(out=rowoff[:], in_=rowoff_d), writes=[b_c])
            mid = T(f"mid{l}", [128, 1], F32, ps)
            lo = T(f"lo{l}", [128, 1], F32, ps)
            cnt = T(f"cnt{l}", [128, 1], F32, ps)
            gef = T(f"gef{l}", [128, 1], F32, ps)
            tt = T(f"tt{l}", [128, 1], F32, ps)
            cmpj = T(f"cmpj{l}", [128, 512], F32, ps)
            am = T(f"am{l}", [128, 512], F32, ps)
            vals = T(f"vals{l}", [128, ROUNDS * 8], F32, ps)
            idxs = T(f"idxs{l}", [128, ROUNDS * 8], mybir.dt.uint16, ps)
            idf = T(f"idf{l}", [128, ROUNDS * 8], F32, ps)
            vmask = T(f"vmask{l}", [128, ROUNDS * 8], F32, ps)
            nrow = T(f"nrow{l}", [128, 1], F32, ps)
            off_sb = T(f"off{l}", [128, 1], F32, ps)
            diag = T(f"diag{l}", [128, 128], F32, ps)
            offB = T(f"offB{l}", [128, 128], F32, ps)
            RT = T(f"RT{l}", [128, ROUNDS * 8 // 128, 128, 4], BF16, ps)
            trimat = T(f"trimat{l}", [128, 128], F32, ps)
            dmat = T(f"dmat{l}", [128, ROUNDS * 8 // 128, 512], mybir.dt.int16, ps)
            s.dma("sp", lambda e: e.dma_start(out=trimat[:], in_=trimat_d), writes=[b_c])
            s.dma("sp", lambda e: e.dma_start(out=dmat[:], in_=dmat_d), writes=[b_c])
            b_s = Buf()
            s.op("dve", lambda e: e.memset(mid[:], 0.5), writes=[b_s])
            s.op("dve", lambda e: e.memset(lo[:], 0.0), writes=[b_s])
            s.barrier()
            step = 0.5
            cb = banks[1]
            b_cb = bbuf[1]
            for it in range(NBIS):
                s.op("dve", lambda e: e.tensor_scalar(out=cmpj[:], in0=affT[:], scalar1=mid[:, 0:1], scalar2=None,
                                                      op0=ALU.is_ge, op1=ALU.add, accum_out=cnt[:]), reads=[b_s], writes=[b_s])
                s.op("pe", lambda e: e.matmul(cb[:, 0:1], lhsT=gmat[:], rhs=cnt[:], start=True, stop=True), reads=[b_s], writes=[b_cb])
                s.op("dve", lambda e: e.tensor_scalar(out=gef[:], in0=cb[:, 0:1], scalar1=511.5, scalar2=None, op0=ALU.is_ge), reads=[b_cb], writes=[b_s])
                s.op("dve", lambda e: e.scalar_tensor_tensor(out=lo[:], in0=gef[:], scalar=mid[:, 0:1], in1=lo[:], op0=ALU.mult, op1=ALU.max),
                     reads=[b_s], writes=[b_s])
                step *= 0.5
                st2 = step
                s.op("dve", lambda e: e.tensor_scalar(out=tt[:], in0=gef[:], scalar1=2.0 * st2, scalar2=-st2, op0=ALU.mult, op1=ALU.add),
                     reads=[b_s], writes=[b_s])
                s.op("dve", lambda e: e.tensor_tensor(out=mid[:], in0=mid[:], in1=tt[:], op=ALU.add), reads=[b_s], writes=[b_s])
            s.op("dve", lambda e: e.scalar_tensor_tensor(out=am[:], in0=affT[:], scalar=lo[:, 0:1], in1=affT[:], op0=ALU.is_ge, op1=ALU.mult),
                 reads=[b_s], writes=[b_s])
            for r in range(ROUNDS):
                sl = slice(r * 8, (r + 1) * 8)
                s.op("dve", lambda e: e.max(out=vals[:, sl], in_=am[:]), reads=[b_s], writes=[b_s])
                s.op("dve", lambda e: e.max_index(out=idxs[:, sl], in_max=vals[:, sl], in_values=am[:]), reads=[b_s], writes=[b_s])
                s.op("dve", lambda e: e.match_replace(out=am[:], in_to_replace=vals[:, sl], in_values=am[:], imm_value=-1.0), reads=[b_s], writes=[b_s])
            NSL = ROUNDS * 8
            NIC = NSL // 128
            U8 = mybir.dt.uint8
            s.op("dve", lambda e: e.tensor_scalar(out=vmask[:], in0=vals[:], scalar1=0.0, scalar2=None, op0=ALU.is_gt, op1=ALU.add, accum_out=nrow[:]),
                 reads=[b_s], writes=[b_s])
            s.op("dve", lambda e: e.tensor_tensor(out=vals[:], in0=vals[:], in1=vmask[:], op=ALU.mult), reads=[b_s], writes=[b_s])
            idx8 = idxs[:].bitcast(U8).rearrange("p (n two) -> p n two", two=2)
            dig = T(f"dig{l}", [128, 4, NSL], BF16, ps)
            tmpf = T(f"tmpf{l}", [128, NSL], F32, ps)
            s.op("dve", lambda e: e.tensor_copy(out=tmpf[:].rearrange("p (n o) -> p n o", o=1), in_=idx8[:, :, 1:2]), reads=[b_s], writes=[b_s])
            s.op("dve", lambda e: e.scalar_tensor_tensor(out=dig[:, 0, :], in0=tmpf[:], scalar=rowoff[:, 0:1], in1=vmask[:], op0=ALU.add, op1=ALU.mult),
                 reads=[b_s, b_c], writes=[b_s])
            s.op("dve", lambda e: e.tensor_copy(out=tmpf[:].rearrange("p (n o) -> p n o", o=1), in_=idx8[:, :, 0:1]), reads=[b_s], writes=[b_s])
            s.op("dve", lambda e: e.tensor_tensor(out=dig[:, 1, :], in0=tmpf[:], in1=vmask[:], op=ALU.mult), reads=[b_s], writes=[b_s])
            s.op("dve", lambda e: e.tensor_copy(out=dig[:, 2, :], in_=vals[:]), reads=[b_s], writes=[b_s])
            s.op("dve", lambda e: e.tensor_tensor(out=dig[:, 3, :], in0=vals[:], in1=dig[:, 2, :], op=ALU.subtract), reads=[b_s], writes=[b_s])
            s.op("pe", lambda e: e.matmul(banks[2][:, 0:1], lhsT=trimat[:], rhs=nrow[:], start=True, stop=True), reads=[b_s, b_c], writes=[bbuf[2]])
            s.op("dve", lambda e: e.tensor_copy(out=off_sb[:], in_=banks[2][:, 0:1]), reads=[bbuf[2]], writes=[b_s])
            s.op("dve", lambda e: e.tensor_scalar(out=diag[:], in0=ident_f[:], scalar1=off_sb[:, 0:1], scalar2=None, op0=ALU.mult), reads=[b_s], writes=[b_s])
            s.op("pe", lambda e: e.matmul(banks[3][:, 0:128], lhsT=ones_f[:], rhs=diag[:], start=True, stop=True), reads=[b_s], writes=[bbuf[3]])
            s.op("act", lambda e: e.copy(out=offB[:], in_=banks[3][:, 0:128]), reads=[bbuf[3]], writes=[b_s])
            tb2 = banks[0][:].bitcast(BF16)
            for ic in range(NIC):
                for k in range(4):
                    col = (ic * 4 + k) * 128
                    s.op("pe", lambda e: e.transpose(out=tb2[:, col:col + 128], in_=dig[:, k, ic * 128:(ic + 1) * 128], identity=ident_bf[:]),
                         reads=[b_s], writes=[bbuf[0]])
                s.op("dve", lambda e: e.tensor_copy(out=RT[:, ic, :, :].rearrange("p r k -> p k r"),
                                                    in_=tb2[:, ic * 512:(ic + 1) * 512].rearrange("p (k r) -> p k r", k=4)), reads=[bbuf[0]], writes=[b_s])
            s.barrier()
            sels = Rot([(T(f"selt{l}_{i}", [128, 512], BF16, ps), Buf()) for i in range(6)])
            for row in range(128):
                e_ = row // 8
                for ic in range(NIC):
                    sel_t, b_sel = sels.next()
                    s.op("pool" if (row * NIC + ic) % 3 == 2 else "dve",
                         lambda e: e.tensor_scalar(out=sel_t[:], in0=dmat[:, ic, :], scalar1=offB[:, row:row + 1], scalar2=None, op0=ALU.is_equal),
                         writes=[b_sel])
                    first = (row % 8 == 0 and ic == 0)
                    last = (row % 8 == 7 and ic == NIC - 1)
                    for jc in range(4):
                        s.op("pe", lambda e: e.matmul(banks[4 + jc][:, e_ * 4:e_ * 4 + 4], lhsT=sel_t[:, jc * 128:(jc + 1) * 128], rhs=RT[:, ic, row, :],
                                                      start=first, stop=last), reads=[b_sel], writes=[bbuf[4 + jc]], inc=(jc == 3))
            rs_sb = T(f"rs_sb{l}", [128, 4, NE, 4], F32, ps)
            b_rs = Buf()
            for jc in range(4):
                s.op("act", lambda e: e.copy(out=rs_sb[:, jc, :, :], in_=banks[4 + jc][:, 0:4 * NE].rearrange("p (e t) -> p e t", t=4)),
                     reads=[bbuf[4 + jc]], writes=[b_rs])
            dg = lambda k: rs_sb[:, :, :, k:k + 1].rearrange("p j e o -> p j (e o)")
            s.op("dve", lambda e: e.scalar_tensor_tensor(out=idsT[:].rearrange("p (e j) -> p j e", j=4), in0=dg(0), scalar=256.0, in1=dg(1),
                                                         op0=ALU.mult, op1=ALU.add), reads=[b_rs])
            s.op("dve", lambda e: e.tensor_tensor(out=gT[:].rearrange("p (e j) -> p j e", j=4), in0=dg(2), in1=dg(3), op=ALU.add), reads=[b_rs])
            if debug:
                s.barrier()
                s.dma("sp", lambda e: e.dma_start(out=aff_dbg, in_=affT[:]))
                s.dma("sp", lambda e: e.dma_start(out=sel_dbg[:, 0:64], in_=idsT[:].bitcast(F32)))
                s.dma("sp", lambda e: e.dma_start(out=sel_dbg[:, 64:128], in_=gT[:]))
            s.barrier()
        if stop_after == "p4":
            s.barrier()
            wst.close()
            sel.close()
            break

        with ExitStack() as ps:
            xss = Rot([(T(f"xs{l}_{i}", [128, D], BF16, ps), Buf()) for i in range(8)])
            xsTs = [(T(f"xsT{l}_{i}", [128, 8, 512], BF16, ps), Buf()) for i in range(2)]
            hidTs = Rot([(T(f"hidT{l}_{i}", [128, 8, 512], BF16, ps), Buf()) for i in range(2)])
            sgs = Rot([(T(f"sg{l}_{i}", [128, 512], F32, ps), Buf()) for i in range(2)])
            ys = Rot([(T(f"y{l}_{i}", [128, D], F32, ps), Buf()) for i in range(2)])
            tpk = Rot([(banks[0], bbuf[0]), (banks[1], bbuf[1])])
            gub = Rot([(banks[2], bbuf[2], banks[3], bbuf[3]), (banks[4], bbuf[4], banks[5], bbuf[5])])
            ybk = Rot([(banks[6], bbuf[6]), (banks[7], bbuf[7])])
            hreg = [[Buf(f"hreg{q}_{p}") for p in range(ROWS_PER_E)] for q in range(2)]
            NCH = NE * SLOT_CHUNKS

            def rows_of(ch):
                e_, half = ch // SLOT_CHUNKS, ch % SLOT_CHUNKS
                return [e_ * ROWS_PER_E + half * 4 + i for i in range(4)]

            gathered = {}

            def issue_gathers(ch):
                lst_ = []
                for r in rows_of(ch):
                    xs, b_xs = xss.next()
                    s.dma("pool", lambda e: e.indirect_dma_start(out=xs[:], out_offset=None, in_=xn2_d,
                                                                 in_offset=bass.IndirectOffsetOnAxis(ap=idsT[:, r:r + 1], axis=0)),
                          writes=[b_xs])
                    lst_.append((xs, b_xs))
                gathered[ch] = lst_

            def do_transposes(ch):
                xsT, b_xsT = xsTs[ch % 2]
                for i, (xs, b_xs) in enumerate(gathered.pop(ch)):
                    pt, b_pt = tpk.next()
                    ptv = pt[:].bitcast(BF16)
                    for c in range(8):
                        s.op("pe", lambda e: e.transpose(out=ptv[:, c * 128:(c + 1) * 128], in_=xs[:, c * 128:(c + 1) * 128], identity=ident_bf[:]),
                             reads=[b_xs], writes=[b_pt], inc=(c == 7))
                    s.op("dve", lambda e: e.tensor_copy(out=xsT[:, :, i * 128:(i + 1) * 128], in_=ptv.rearrange("p (c t) -> p c t", c=8)),
                         reads=[b_pt], writes=[b_xsT])

            issue_gathers(0)
            do_transposes(0)
            for ch in range(NCH):
                e_ = ch // SLOT_CHUNKS
                Wg, Wu, Wd, b_W = Ws[e_ % 2]
                wsteps = load_expert_steps(e_ + 1) if (ch % SLOT_CHUNKS == 0 and e_ + 1 < NE) else []
                if ch + 1 < NCH:
                    issue_gathers(ch + 1)
                xsT, b_xsT = xsTs[ch % 2]
                hidT, b_hid = hidTs.next()
                for f in range(8):
                    for _ in range(3):
                        if wsteps:
                            wsteps.pop(0)()
                    gb, bgb, ub, bub = gub.next()
                    for c in range(8):
                        s.op("pe", lambda e: e.matmul(gb[:, :], lhsT=Wg[:, c, f * 128:(f + 1) * 128], rhs=xsT[:, c, :], start=(c == 0), stop=(c == 7)),
                             reads=[b_W, b_xsT], writes=[bgb], inc=(c == 7))
                    for c in range(8):
                        s.op("pe", lambda e: e.matmul(ub[:, :], lhsT=Wu[:, c, f * 128:(f + 1) * 128], rhs=xsT[:, c, :], start=(c == 0), stop=(c == 7)),
                             reads=[b_W, b_xsT], writes=[bub], inc=(c == 7))
                    sg, b_sg = sgs.next()
                    s.op("act", lambda e: e.activation(out=sg[:], in_=gb[:, :], func=AF.Silu), reads=[bgb], writes=[b_sg])
                    s.op("dve", lambda e: e.tensor_tensor(out=hidT[:, f, :], in0=ub[:, :], in1=sg[:], op=ALU.mult), reads=[bub, b_sg], writes=[b_hid])
                if ch + 1 < NCH:
                    do_transposes(ch + 1)
                for i, r in enumerate(rows_of(ch)):
                    part = r % ROWS_PER_E
                    y, b_y = ys.next()
                    for hc in range(2):
                        yb, byb = ybk.next()
                        for f in range(8):
                            s.op("pe", lambda e: e.matmul(yb[:, :], lhsT=hidT[:, f, i * 128:(i + 1) * 128], rhs=Wd[:, f, hc * 512:(hc + 1) * 512],
                                                          start=(f == 0), stop=(f == 7)),
                                 reads=[b_W, b_hid], writes=[byb], inc=(f == 7))
                        s.op("dve", lambda e: e.tensor_scalar(out=y[:, hc * 512:(hc + 1) * 512], in0=yb[:, :], scalar1=gT[:, r:r + 1], scalar2=None,
                                                              op0=ALU.mult), reads=[byb], writes=[b_y])
                    s.dma("pool", lambda e: e.indirect_dma_start(out=h_d, out_offset=bass.IndirectOffsetOnAxis(ap=idsT[:, r:r + 1], axis=0),
                                                                 in_=y[:], in_offset=None, compute_op=ALU.add),
                          reads=[b_y] + hreg[(e_ + 1) % 2], writes=[hreg[e_ % 2][part]])
            s.barrier()
        wst.close()
        sel.close()

    if stop_after is None:
        with ExitStack() as ps:
            g_bc = T("gfin", [128, D], F32, ps)
            b_g = Buf()
            s.dma("sp", lambda e: e.dma_start(out=g_bc[:], in_=final_norm_d[0:1, :].partition_broadcast(128)), writes=[b_g])
            hts = Rot([(T(f"htf_{i}", [128, D], F32, ps), Buf()) for i in range(3)])
            ots = Rot([(T(f"otf_{i}", [128, D], F32, ps), Buf()) for i in range(3)])
            junk = T("junkf", [128, D], BF16, ps)
            b_junk = Buf()
            sss = Rot([(T(f"ssf_{i}", [128, 1], F32, ps), Buf()) for i in range(2)])
            for tb in range(NTB):
                ht, b_ht = hts.next()
                ot, b_ot = ots.next()
                ss, b_ss = sss.next()
                s.dma("sp", lambda e: e.dma_start(out=ht[:], in_=h_d[tb * 128:(tb + 1) * 128, :]), writes=[b_ht])
                s.op("dve", lambda e: e.scalar_tensor_tensor(out=junk[:], in0=ht[:], scalar=1.0, in1=ht[:],
                                                             op0=ALU.mult, op1=ALU.mult, accum_out=ss[:]),
                     reads=[b_ht], writes=[b_junk, b_ss])
                rstd_from_ss(ss[:], ss[:], 1.0 / D, [b_ss], [b_ss])
                s.op("dve", lambda e: e.scalar_tensor_tensor(out=ot[:], in0=ht[:], scalar=ss[:, 0:1], in1=g_bc[:],
                                                             op0=ALU.mult, op1=ALU.mult), reads=[b_ht, b_ss, b_g], writes=[b_ot])
                s.dma("sp", lambda e: e.dma_start(out=out_d[tb * 128:(tb + 1) * 128, :], in_=ot[:]), reads=[b_ot])
    s.barrier()
    es.close()
    return nc


def _swap_halves(w, hd):
    sh = w.shape
    w4 = w.reshape(sh[:-1] + (sh[-1] // hd, 2, hd // 2))
    return np.ascontiguousarray(w4[..., ::-1, :]).reshape(sh)


def _rope_tables(dim):
    inv = (1.0 / (np.float32(10000.0) ** (np.arange(0, dim, 2, dtype=np.float32) / np.float32(dim)))).astype(np.float32)
    ang = np.arange(S, dtype=np.float32)[:, None] * inv[None, :]
    return np.cos(ang).astype(np.float32), np.sin(ang).astype(np.float32)


def _host_prep(inp):
    f = lambda a: np.ascontiguousarray(a, dtype=np.float32)
    w_in = np.asarray(inp["w_in"])
    shared = {}
    shared["attn_norm"] = f(inp["attn_norm"])
    shared["ffn_norm"] = f(inp["ffn_norm"])
    shared["final_norm"] = f(np.asarray(inp["final_norm"]).reshape(1, D))
    shared["w_na"] = f(w_in[:, :, 0:768])
    shared["w_lat"] = f(w_in[:, :, 768:1152])
    kr96 = w_in[:, :, 1088:1184]
    shared["w_kr"] = f(kr96)
    krp = np.array(kr96, copy=True)
    krp[:, :, 64:96] = _swap_halves(kr96[:, :, 64:96], 32)
    shared["w_krp"] = f(krp)
    cq = w_in[:, :, 1184:1568].reshape(L, D, 6, 64)
    order = [0, 3, 1, 4, 2, 5]
    cq_r = cq[:, :, order, :].reshape(L, D, 384)
    shared["w_cq"] = f(cq_r)
    shared["w_cqp"] = f(_swap_halves(cq_r, 64))
    ck = w_in[:, :, 1568:1696]
    shared["w_ck"] = f(ck)
    shared["w_ckp"] = f(_swap_halves(ck, 64))
    shared["w_cv"] = f(w_in[:, :, 1696:1824])
    rpb = np.asarray(inp["na_rpb"], dtype=np.float32)
    tiles = np.full((L, NV, 4, 128, 320), NEG, np.float32)
    p = np.arange(128)
    kc = p % 64
    krl = p // 64
    c = np.arange(64)
    cs_ = np.clip(c - 8, 0, 48)
    colvalid = (kc[:, None] >= cs_[None, :]) & (kc[:, None] <= cs_[None, :] + 15)
    dc = np.clip(kc[:, None] - c[None, :] + 15, 0, 30)
    for v, (d0, roff, nb) in enumerate(NA_KEYS):
        for b in range(nb):
            kr_rel = 2 * b + krl
            rowvalid = (kr_rel >= roff) & (kr_rel <= roff + 7)
            dr = np.clip(d0 + kr_rel + 7, 0, 14)
            vals = rpb[:, :, dr[:, None], dc]
            ok = (rowvalid[:, None] & colvalid)[None, None]
            tiles[:, v, :, :, b * 64:(b + 1) * 64] = np.where(ok, vals, np.float32(NEG))
    shared["rpb_tiles"] = tiles
    shared["mla_q_norm"] = f(inp["mla_q_norm"])
    shared["mla_kv_norm"] = f(inp["mla_kv_norm"])
    w_uq = np.asarray(inp["mla_w_uq"])
    shared["w_uq"] = f(w_uq)
    uq4 = np.array(w_uq.reshape(L, 256, 6, 96), copy=True)
    uq4[..., 64:96] = _swap_halves(uq4[..., 64:96], 32)
    shared["w_uqp"] = f(uq4.reshape(L, 256, 576))
    ukv = np.asarray(inp["mla_w_ukv"]).reshape(L, 128, 6, 128)
    shared["w_uk"] = f(ukv[..., 0:64].reshape(L, 128, 384))
    shared["w_uv"] = f(ukv[..., 64:128].reshape(L, 128, 384))
    shared["swa_sink"] = f(inp["swa_sink"])
    shared["group_norm"] = f(inp["group_norm"])
    shared["w_out"] = f(inp["w_out"])
    shared["w_router"] = f(inp["w_router"])
    shared["w_gate"] = f(inp["w_gate"])
    shared["w_up"] = f(inp["w_up"])
    shared["w_down"] = f(inp["w_down"])
    cos, sin = _rope_tables(64)
    cT = np.concatenate([cos.T, cos.T], 0)
    sT = np.concatenate([-sin.T, sin.T], 0)
    shared["rope_c_cos"] = f(np.concatenate([cT, cT], 0))
    shared["rope_c_sin"] = f(np.concatenate([sT, sT], 0))
    cos, sin = _rope_tables(32)
    shared["rope_b_cos"] = f(np.concatenate([cos.T, cos.T], 0))
    shared["rope_b_sin"] = f(np.concatenate([-sin.T, sin.T], 0))
    k = np.arange(128)[:, None]
    q = np.arange(512)[None, :]
    m = np.zeros((6, 128, 512), np.float32)
    for j in range(6):
        diff = q - k - (j - 1) * 128
        m[j] = np.where(np.abs(diff) <= 128, 1.0, 0.0)
    shared["swa_mask"] = m.astype(ml_dtypes.bfloat16)
    shared["ident_bf"] = np.eye(128, dtype=np.float32).astype(ml_dtypes.bfloat16)
    shared["ident_f"] = np.eye(128, dtype=np.float32)
    pp = np.arange(128)
    shared["gmat"] = (pp[:, None] // 8 == pp[None, :] // 8).astype(np.float32)
    shared["rowoff"] = ((pp % 8) * 2).astype(np.float32).reshape(128, 1)
    shared["trimat"] = ((pp[:, None] // 8 == pp[None, :] // 8) & (pp[:, None] < pp[None, :])).astype(np.float32)
    nic = ROUNDS * 8 // 128
    ii = np.arange(128)[:, None, None]
    icc = np.arange(nic)[None, :, None]
    jj = np.arange(512)[None, None, :]
    shared["dmat"] = (jj - ii - icc * 128).astype(np.int16)
    shared["invw"] = np.tile(np.array([[1 / 256, 1 / 384, 1 / 384]], np.float32), (128, 1))
    return shared


def kernel(**inputs):
    shared = _host_prep(inputs)
    x = np.asarray(inputs["x"], dtype=np.float32)
    nc = build()
    in_maps = []
    for c in range(NCORES):
        m = dict(shared)
        m["x"] = np.ascontiguousarray(x[c])
        in_maps.append(m)
    res = run_bass_kernel_spmd(nc, in_maps, core_ids=list(range(NCORES)))
    return np.stack([np.asarray(res.results[c]["out"], dtype=np.float32) for c in range(NCORES)], axis=0)
```

```python
import numpy as np
import ml_dtypes
from contextlib import ExitStack
import concourse.bass as bass
import concourse.mybir as mybir
from concourse.bass_utils import run_bass_kernel_spmd

F32 = mybir.dt.float32
BF16 = mybir.dt.bfloat16
U32 = mybir.dt.uint32
AF = mybir.ActivationFunctionType
ALU = mybir.AluOpType

S = 4096
D = 1024
NTB = 32
NQB = 8
L = 2
NE = 16
EPS = 1e-6
NEG = -30000.0
NCORES = 8
ROUNDS = 32
ROWS_PER_E = 4
SLOT_CHUNKS = 1
NBIS = 27


class Buf:
    __slots__ = ("name", "writer", "readers")

    def __init__(self, name=""):
        self.name = name
        self.writer = None
        self.readers = []


class Sched:
    ENG = ("pe", "act", "dve", "pool", "sp")

    def __init__(self, nc, es, same_engine_sync=True):
        self.nc = nc
        self.e = {"pe": nc.tensor, "act": nc.scalar, "dve": nc.vector,
                  "pool": nc.gpsimd, "sp": nc.sync}
        self.sem = {k: es.enter_context(nc.semaphore("s_" + k)) for k in self.ENG}
        self.seq = {k: 0 for k in self.ENG}
        self.waited = {a: {} for a in self.ENG}
        self.same_engine_sync = same_engine_sync
        self.lanes = {}
        self.lane_rr = {}
        for q, n in (("sp", 16), ("pool", 16)):
            self.lanes[q] = [[es.enter_context(nc.semaphore(f"d_{q}{i}")), 0] for i in range(n)]
            self.lane_rr[q] = 0
        self.pending_reads = {k: [] for k in self.ENG}

    def _wait(self, on, dep):
        if dep is None:
            return
        if dep[0] == "eng":
            _, eng, seq = dep
            if eng == on and (not self.same_engine_sync or on == "pe"):
                return
            if self.waited[on].get(eng, 0) >= seq:
                return
            self.e[on].wait_ge(self.sem[eng], seq)
            self.waited[on][eng] = seq
        else:
            _, q, li, cnt = dep
            key = ("dma", q, li)
            if self.waited[on].get(key, 0) >= cnt:
                return
            self.e[on].wait_ge(self.lanes[q][li][0], 16 * cnt)
            self.waited[on][key] = cnt

    def _deps(self, on, reads, writes):
        for r in reads:
            self._wait(on, r.writer)
        for w in writes:
            self._wait(on, w.writer)
            for d in w.readers:
                self._wait(on, d)

    def _commit(self, dep, reads, writes):
        for w in writes:
            w.writer = dep
            w.readers = []
        for r in reads:
            if r not in writes:
                r.readers.append(dep)
                if len(r.readers) > 48:
                    last = {}
                    for d in r.readers:
                        k = d[:2] if d[0] == "eng" else d[:3]
                        if k not in last or d[-1] > last[k][-1]:
                            last[k] = d
                    r.readers = list(last.values())

    def op(self, on, fn, reads=(), writes=(), inc=True):
        reads = list(reads)
        writes = list(writes)
        self._deps(on, reads, writes)
        inst = fn(self.e[on])
        if inc:
            self.seq[on] += 1
            inst.then_inc(self.sem[on], 1)
            dep = ("eng", on, self.seq[on])
            self._commit(dep, reads + self.pending_reads[on], writes)
            self.pending_reads[on] = []
        else:
            self.pending_reads[on].extend(reads)
            for w in writes:
                w.writer = ("eng", on, self.seq[on] + 1)
                w.readers = []
        return inst

    def dma(self, q, fn, reads=(), writes=()):
        reads = list(reads)
        writes = list(writes)
        lanes = self.lanes[q]
        li = self.lane_rr[q]
        self.lane_rr[q] = (li + 1) % len(lanes)
        sem, cnt = lanes[li]
        if cnt > 0:
            self._wait(q, ("dma", q, li, cnt))
        self._deps(q, reads, writes)
        inst = fn(self.e[q])
        inst.then_inc(sem, 16)
        lanes[li][1] = cnt + 1
        dep = ("dma", q, li, cnt + 1)
        self._commit(dep, reads, writes)
        return inst

    def barrier(self):
        for a in self.ENG:
            for b in self.ENG:
                if a != b and self.seq[b] > 0:
                    self._wait(a, ("eng", b, self.seq[b]))
            for q in self.lanes:
                for li, (sem, cnt) in enumerate(self.lanes[q]):
                    if cnt > 0:
                        self._wait(a, ("dma", q, li, cnt))


class Rot:
    def __init__(self, items):
        self.items = items
        self.i = 0

    def next(self):
        it = self.items[self.i]
        self.i = (self.i + 1) % len(self.items)
        return it


def _na_row_info(r):
    rs = min(max(r - 4, 0), 56)
    if rs % 2 == 0:
        start, nb = rs, 4
    else:
        start, nb = rs - 1, 5
    return rs, start, nb


def _na_variants():
    keys = []
    vmap = {}
    for r in range(64):
        rs, start, nb = _na_row_info(r)
        k = (start - r, rs - start, nb)
        if k not in keys:
            keys.append(k)
        vmap[r] = keys.index(k)
    return keys, vmap


NA_KEYS, NA_VMAP = _na_variants()
NV = len(NA_KEYS)


def build(debug=False, nlayers=L, stop_after=None):
    nc = bass.Bass("TRN2", target_bir_lowering=False)
    es = ExitStack()

    def din(name, shape, dt=F32):
        return nc.dram_tensor(name, list(shape), dt, kind="ExternalInput").ap()

    dbg_kind = "ExternalOutput" if debug else "Internal"

    def dscr(name, shape, dt=F32, dbg=True):
        return nc.dram_tensor(name, list(shape), dt, kind=(dbg_kind if dbg else "Internal")).ap()

    x_d = din("x", [S, D])
    attn_norm_d = din("attn_norm", [L, D])
    ffn_norm_d = din("ffn_norm", [L, D])
    final_norm_d = din("final_norm", [1, D])
    w_na_d = din("w_na", [L, D, 768])
    w_lat_d = din("w_lat", [L, D, 384])
    w_kr_d = din("w_kr", [L, D, 96])
    w_krp_d = din("w_krp", [L, D, 96])
    w_cq_d = din("w_cq", [L, D, 384])
    w_cqp_d = din("w_cqp", [L, D, 384])
    w_ck_d = din("w_ck", [L, D, 128])
    w_ckp_d = din("w_ckp", [L, D, 128])
    w_cv_d = din("w_cv", [L, D, 128])
    rpb_d = din("rpb_tiles", [L, NV, 4, 128, 320])
    qn_d = din("mla_q_norm", [L, 256])
    kvn_d = din("mla_kv_norm", [L, 128])
    w_uq_d = din("w_uq", [L, 256, 576])
    w_uqp_d = din("w_uqp", [L, 256, 576])
    w_uk_d = din("w_uk", [L, 128, 384])
    w_uv_d = din("w_uv", [L, 128, 384])
    sink_d = din("swa_sink", [L, 6])
    gn_d = din("group_norm", [L, D])
    w_out_d = din("w_out", [L, D, D])
    w_router_d = din("w_router", [L, D, NE])
    w_gate_d = din("w_gate", [L, NE, D, D])
    w_up_d = din("w_up", [L, NE, D, D])
    w_down_d = din("w_down", [L, NE, D, D])
    cs_c_d = din("rope_c_cos", [128, S])
    sn_c_d = din("rope_c_sin", [128, S])
    cs_b_d = din("rope_b_cos", [32, S])
    sn_b_d = din("rope_b_sin", [32, S])
    swa_mask_d = din("swa_mask", [6, 128, 512], BF16)
    ident_bf_d = din("ident_bf", [128, 128], BF16)
    ident_f_d = din("ident_f", [128, 128])
    gmat_d = din("gmat", [128, 128])
    rowoff_d = din("rowoff", [128, 1])
    invw_d = din("invw", [128, 3])
    trimat_d = din("trimat", [128, 128])
    dmat_d = din("dmat", [128, ROUNDS * 8 // 128, 512], mybir.dt.int16)

    out_d = nc.dram_tensor("out", [S, D], F32, kind="ExternalOutput").ap()
    h_d = dscr("h_scr", [S, D])
    xn2_d = dscr("xn2_scr", [S, D], BF16)
    oc_d = dscr("oc_scr", [16, 64, S], BF16)
    aff_dbg = dscr("aff_dbg", [128, 512]) if debug else None
    sel_dbg = dscr("sel_dbg", [128, 128]) if debug else None

    s = Sched(nc, es)
    T = lambda name, shape, dt, st=es: st.enter_context(nc.sbuf_tensor("t_" + name, list(shape), dt))

    pp = [es.enter_context(nc.psum_tensor(f"pp{i}", [128, 1024], F32)) for i in range(4)]
    banks = [pp[i // 2][:, (i % 2) * 512:(i % 2 + 1) * 512] for i in range(8)]
    bbuf = [Buf(f"bank{i}") for i in range(8)]

    ident_bf = T("ident_bf", [128, 128], BF16)
    ident_f = T("ident_f", [128, 128], F32)
    ones_bf = T("ones_bf", [128, 128], BF16)
    ones_f = T("ones_f", [128, 128], F32)
    eps_t = T("eps_t", [128, 1], F32)
    cbuf = Buf("consts")
    s.dma("sp", lambda e: e.dma_start(out=ident_bf[:], in_=ident_bf_d), writes=[cbuf])
    s.dma("sp", lambda e: e.dma_start(out=ident_f[:], in_=ident_f_d), writes=[cbuf])
    s.op("dve", lambda e: e.memset(ones_bf[:], 1.0), writes=[cbuf])
    s.op("dve", lambda e: e.memset(ones_f[:], 1.0), writes=[cbuf])
    s.op("dve", lambda e: e.memset(eps_t[:], EPS), writes=[cbuf])
    s.barrier()

    def rstd_big(ss_ap, out_ap, scratch_ap, inv_n, rd, wr):
        s.op("dve", lambda e: e.tensor_scalar(out=out_ap, in0=ss_ap, scalar1=inv_n, scalar2=EPS,
                                              op0=ALU.mult, op1=ALU.add), reads=rd, writes=wr)
        s.op("act", lambda e: e.activation(out=out_ap, in_=out_ap, func=AF.Ln), reads=wr, writes=wr)
        s.op("act", lambda e: e.activation(out=out_ap, in_=out_ap, func=AF.Exp, scale=-0.5), reads=wr, writes=wr)

    def rstd_from_ss(ss_ap, out_ap, inv_n, rd, wr):
        s.op("dve", lambda e: e.tensor_scalar(out=out_ap, in0=ss_ap, scalar1=inv_n, scalar2=EPS,
                                              op0=ALU.mult, op1=ALU.add), reads=rd, writes=wr)
        s.op("act", lambda e: e.activation(out=out_ap, in_=out_ap, func=AF.Sqrt), reads=wr, writes=wr)
        s.op("dve", lambda e: e.reciprocal(out=out_ap, in_=out_ap), reads=wr, writes=wr)

    evac_rr = [0]

    def evac_copy(out_ap, in_ap, rd, wr, scale=None):
        evac_rr[0] ^= 1
        if evac_rr[0]:
            if scale is None:
                s.op("act", lambda e: e.copy(out=out_ap, in_=in_ap), reads=rd, writes=wr)
            else:
                s.op("act", lambda e: e.mul(out=out_ap, in_=in_ap, mul=scale), reads=rd, writes=wr)
        else:
            if scale is None:
                s.op("dve", lambda e: e.tensor_copy(out=out_ap, in_=in_ap), reads=rd, writes=wr)
            else:
                s.op("dve", lambda e: e.tensor_scalar(out=out_ap, in0=in_ap, scalar1=scale, scalar2=None,
                                                      op0=ALU.mult), reads=rd, writes=wr)

    fin_queue = []
    fin_age = [3]

    def flush_fin(force=False):
        for ent in fin_queue:
            ent[0] += 1
        while fin_queue and (force or fin_queue[0][0] > fin_age[0]):
            fin_queue.pop(0)[1]()

    def finalize_heads(O_bank, O_buf, j_oc, col0, st_tiles, extra_den=None, bank_src=None, use_act=False):
        rden, b_rden, bc_bank, b_bc, bc_sb, b_bcsb, o_bf, b_obf = st_tiles
        if use_act:
            if extra_den is not None:
                s.op("act", lambda e: e.activation(out=rden[64:65, 0:512], in_=O_bank[64:65, :], func=AF.Ln, bias=extra_den),
                     reads=[O_buf], writes=[b_rden])
            else:
                s.op("act", lambda e: e.activation(out=rden[64:65, 0:512], in_=O_bank[64:65, :], func=AF.Ln),
                     reads=[O_buf], writes=[b_rden])
            s.op("act", lambda e: e.activation(out=rden[64:65, 0:512], in_=rden[64:65, 0:512], func=AF.Exp, scale=-1.0),
                 reads=[b_rden], writes=[b_rden])
        elif extra_den is not None:
            s.op("dve", lambda e: e.tensor_scalar(out=rden[64:65, 0:512], in0=O_bank[64:65, :], scalar1=extra_den,
                                                  scalar2=None, op0=ALU.add), reads=[O_buf], writes=[b_rden])
            s.op("dve", lambda e: e.reciprocal(out=rden[64:65, 0:512], in_=rden[64:65, 0:512]), reads=[b_rden], writes=[b_rden])
        else:
            s.op("dve", lambda e: e.reciprocal(out=rden[64:65, 0:512], in_=O_bank[64:65, :]), reads=[O_buf], writes=[b_rden])

        def fin_b(bc_bank=bc_bank, b_bc=b_bc):
            if bank_src is not None:
                Spb, b_Spb = bank_src.next()
                bc_bank, b_bc = Spb[:, 0:512], b_Spb[0]
            s.op("pe", lambda e: e.matmul(bc_bank[0:64, :], lhsT=ones_f[64:65, 0:64], rhs=rden[64:65, 0:512],
                                          start=True, stop=True), reads=[b_rden], writes=[b_bc])
            s.op("act" if use_act else "dve", (lambda e: e.copy(out=bc_sb[0:64, :], in_=bc_bank[0:64, :])) if use_act else
                 (lambda e: e.tensor_copy(out=bc_sb[0:64, :], in_=bc_bank[0:64, :])), reads=[b_bc], writes=[b_bcsb])
            s.op("dve", lambda e: e.tensor_tensor(out=o_bf[0:64, :], in0=O_bank[0:64, :], in1=bc_sb[0:64, :], op=ALU.mult),
                 reads=[O_buf, b_bcsb], writes=[b_obf])
            s.dma("sp", lambda e: e.dma_start(out=oc_d[j_oc, :, col0:col0 + 512], in_=o_bf[0:64, :]), reads=[b_obf])
        fin_queue.append([0, fin_b])

    for l in range(nlayers):
        src_d = x_d if l == 0 else h_d
        lat = ExitStack()
        cqn = T(f"cqn{l}", [128, 2, S], BF16, lat)
        ckvn = T(f"ckvn{l}", [128, S], BF16, lat)
        kpe = T(f"kpe{l}", [96, S], BF16, lat)
        lst = ExitStack()
        xnT = T(f"xnT{l}", [128, 8, S], BF16, lst)
        nst = ExitStack()
        rpb_sb = T(f"rpb{l}", [128, NV * 4, 320], BF16, nst)
        b_rpb = Buf()
        for v in range(NV):
            s.dma("pool", lambda e: e.dma_start(out=rpb_sb[:, v * 4:(v + 1) * 4, :], in_=rpb_d[l, v].rearrange("h p c -> p h c")), writes=[b_rpb])
        with ExitStack() as ps:
            g_bc = T(f"g_bc{l}", [128, D], F32, ps)
            b_g = Buf()
            s.dma("sp", lambda e: e.dma_start(out=g_bc[:], in_=attn_norm_d[l:l + 1, :].partition_broadcast(128)), writes=[b_g])
            hts = Rot([(T(f"ht{l}_{i}", [128, D], F32, ps), Buf()) for i in range(4)])
            xbs = Rot([(T(f"xb{l}_{i}", [128, D], BF16, ps), Buf()) for i in range(2)])
            junk = T(f"junk{l}", [128, D], BF16, ps)
            b_junk = Buf()
            sss = Rot([(T(f"ss{l}_{i}", [128, 1], F32, ps), Buf()) for i in range(4)])
            pts = Rot([(banks[i], bbuf[i]) for i in range(2)])
            def p1_a(tb):
                ht, b_ht = hts.next()
                ss, b_ss = sss.next()
                s.dma("sp", lambda e: e.dma_start(out=ht[:], in_=src_d[tb * 128:(tb + 1) * 128, :]), writes=[b_ht])
                s.op("dve", lambda e: e.scalar_tensor_tensor(out=junk[:], in0=ht[:], scalar=1.0, in1=ht[:],
                                                             op0=ALU.mult, op1=ALU.mult, accum_out=ss[:]),
                     reads=[b_ht], writes=[b_junk, b_ss])
                s.op("dve", lambda e: e.tensor_scalar(out=ss[:], in0=ss[:], scalar1=1.0 / D, scalar2=EPS, op0=ALU.mult, op1=ALU.add),
                     reads=[b_ss], writes=[b_ss])
                s.op("act", lambda e: e.activation(out=ss[:], in_=ss[:], func=AF.Sqrt), reads=[b_ss], writes=[b_ss])
                return (tb, ht, b_ht, ss, b_ss)

            def p1_b(ctx):
                tb, ht, b_ht, ss, b_ss = ctx
                xb, b_xb = xbs.next()
                pt, b_pt = pts.next()
                s.op("dve", lambda e: e.reciprocal(out=ss[:], in_=ss[:]), reads=[b_ss], writes=[b_ss])
                s.op("dve", lambda e: e.scalar_tensor_tensor(out=xb[:], in0=ht[:], scalar=ss[:, 0:1], in1=g_bc[:],
                                                             op0=ALU.mult, op1=ALU.mult),
                     reads=[b_ht, b_ss, b_g], writes=[b_xb])
                ptv = pt[:].bitcast(BF16)
                for c in range(8):
                    s.op("pe", lambda e: e.transpose(out=ptv[:, c * 128:(c + 1) * 128], in_=xb[:, c * 128:(c + 1) * 128],
                                                     identity=ident_bf[:]),
                         reads=[b_xb], writes=[b_pt], inc=(c == 7))
                xv = xnT[:, :, tb * 128:(tb + 1) * 128]
                pv = ptv.rearrange("p (c t) -> p c t", c=8)
                s.op("act", lambda e: e.copy(out=xv, in_=pv), reads=[b_pt])

            p1_pend = []
            for tb in range(NTB):
                p1_pend.append(p1_a(tb))
                if len(p1_pend) > 1:
                    p1_b(p1_pend.pop(0))
            while p1_pend:
                p1_b(p1_pend.pop(0))
            s.barrier()

        def proj_fm(w_sb, col0, ncol, nchunk, src, qb, bank, bbank, src_cols=None):
            for c in range(nchunk):
                lw = w_sb[:, c, col0:col0 + ncol]
                rr = src[:, c, qb * 512:(qb + 1) * 512]
                s.op("pe", lambda e: e.matmul(bank[0:ncol, :], lhsT=lw, rhs=rr, start=(c == 0), stop=(c == nchunk - 1)),
                     writes=[bbank], inc=(c == nchunk - 1))

        def load_w_cast(dst, src_ap, wbuf):
            s.dma("pool", lambda e: e.dma_start(out=dst, in_=src_ap), writes=[wbuf])

        with ExitStack() as ps:
            b_w = Buf()
            QT = T(f"QTa{l}", [128, 2, S], BF16, ps)
            KT = T(f"KTa{l}", [128, 4, S], BF16, ps)
            Va = T(f"Va{l}", [128, NTB, 4, 65], BF16, ps)
            s.op("pool", lambda e: e.memset(Va[:, :, :, 64:65], 1.0), writes=[b_w])
            s.op("pool", lambda e: e.memset(KT[:], 0.0), writes=[b_w])
            pw = ExitStack()
            w_sb = T(f"w_na{l}", [128, 8, 768], BF16, pw)
            for c in range(8):
                load_w_cast(w_sb[:, c, :], w_na_d[l, c * 128:(c + 1) * 128, :], b_w)
            s.barrier()
            pbk = Rot([(banks[i], bbuf[i]) for i in range(4)])
            for g in range(4):
                for qb in range(NQB):
                    bank, bb = pbk.next()
                    proj_fm(w_sb, g * 128, 128, 8, xnT, qb, bank, bb)
                    if g < 2:
                        evac_copy(QT[:, g, qb * 512:(qb + 1) * 512], bank[:, :], [bb], [], scale=0.125)
                    else:
                        for hf in range(2):
                            rr = slice(hf * 64, (hf + 1) * 64)
                            evac_copy(KT[rr, (g - 2) * 2 + hf, qb * 512:(qb + 1) * 512], bank[rr, :], [bb], [])
            for tb in range(NTB):
                bank, bb = pbk.next()
                for c in range(8):
                    s.op("pe", lambda e: e.matmul(bank[:, 0:256], lhsT=xnT[:, c, tb * 128:(tb + 1) * 128],
                                                  rhs=w_sb[:, c, 512:768], start=(c == 0), stop=(c == 7)),
                         writes=[bb], inc=(c == 7))
                evac_copy(Va[:, tb, :, 0:64], bank[:, 0:256].rearrange("p (h d) -> p h d", h=4), [bb], [])
            s.barrier()
            pw.close()
            sbk = Rot([(banks[i], bbuf[i]) for i in range(4)])
            obk = Rot([(banks[4 + i], bbuf[4 + i]) for i in range(2)])
            pTs = Rot([(T(f"pTa{l}_{i}", [128, 320], BF16, ps), Buf()) for i in range(4)])
            fin = Rot([(T(f"rdenA{l}_{i}", [65, 1024], F32, ps), Buf(), banks[6 + i], bbuf[6 + i],
                        T(f"bcsA{l}_{i}", [64, 512], F32, ps), Buf(),
                        T(f"obfA{l}_{i}", [64, 512], BF16, ps), Buf()) for i in range(2)])
            pend = []

            def na_back(ctx):
                hh_, ri_, r8_, nb_, start_, O_bank, O_buf, pT, b_pT, fin_t = ctx
                flush_fin()
                for b in range(nb_):
                    kb = (start_ * 64 + b * 128) // 128
                    s.op("pe", lambda e: e.matmul(O_bank[0:65, ri_ * 64:(ri_ + 1) * 64],
                                                  lhsT=Va[:, kb, hh_, :], rhs=pT[:, b * 64:(b + 1) * 64],
                                                  start=(b == 0), stop=(b == nb_ - 1)),
                         reads=[b_pT], writes=[O_buf], inc=(b == nb_ - 1))
                if ri_ == 7:
                    finalize_heads(O_bank, O_buf, hh_, r8_ * 512, fin_t, use_act=True)

            for hh in range(4):
                t, pb = hh // 2, (hh % 2) * 64
                for r8 in range(8):
                    O_bank, O_buf = obk.next()
                    fin_t = fin.next()
                    for ri in range(8):
                        r = r8 * 8 + ri
                        rs, start, nb = _na_row_info(r)
                        v = NA_VMAP[r]
                        w = nb * 64
                        S_bank, S_buf = sbk.next()
                        pT, b_pT = pTs.next()
                        s.op("pe", lambda e: e.matmul(S_bank[:, 0:w], lhsT=ident_bf[:], rhs=rpb_sb[:, v * 4 + hh, 0:w],
                                                      start=True, stop=False), writes=[S_buf], inc=False)
                        for b in range(nb):
                            k0 = start * 64 + b * 128
                            s.op("pe", lambda e: e.matmul(S_bank[:, b * 64:(b + 1) * 64],
                                                          lhsT=KT[:, hh, k0:k0 + 128],
                                                          rhs=QT[:, t, r * 64:(r + 1) * 64],
                                                          start=False, stop=(b == nb - 1)),
                                 writes=[S_buf], inc=(b == nb - 1))
                        s.op("act", lambda e: e.activation(out=pT[:, 0:w], in_=S_bank[:, 0:w], func=AF.Exp),
                             reads=[S_buf], writes=[b_pT])
                        pend.append((hh, ri, r8, nb, start, O_bank, O_buf, pT, b_pT, fin_t))
                        if len(pend) > 3:
                            na_back(pend.pop(0))
            while pend:
                na_back(pend.pop(0))
            flush_fin(force=True)
            s.barrier()
        nst.close()
        if stop_after == "na":
            lst.close(); lat.close()
            break

        with ExitStack() as ps:
            wq = T(f"wq{l}", [128, 8, 384], BF16, ps)
            wqp = T(f"wqp{l}", [128, 8, 384], BF16, ps)
            wk = T(f"wk{l}", [128, 8, 128], BF16, ps)
            wkp = T(f"wkp{l}", [128, 8, 128], BF16, ps)
            wv = T(f"wv{l}", [128, 8, 128], BF16, ps)
            b_w = Buf()
            for dst, srcw in ((wq, w_cq_d), (wqp, w_cqp_d), (wk, w_ck_d), (wkp, w_ckp_d), (wv, w_cv_d)):
                load_w_cast(dst[:], srcw[l].rearrange("(c p) n -> p c n", p=128), b_w)
            tabs = Rot([(T(f"cosc{l}_{i}", [128, 512], F32, ps), T(f"sinc{l}_{i}", [128, 512], F32, ps), Buf()) for i in range(2)])
            msk = T(f"msk{l}", [128, 6, 512], BF16, ps)
            s.dma("sp", lambda e: e.dma_start(out=msk[:], in_=swa_mask_d.rearrange("j p c -> p j c")), writes=[b_w])
            esink = T(f"esink{l}", [65, 6], F32, ps)
            s.dma("sp", lambda e: e.dma_start(out=esink[64:65, :], in_=sink_d[l:l + 1, :]), writes=[b_w])
            QT = T(f"QTc{l}", [128, 3, S], BF16, ps)
            KT = T(f"KTc{l}", [128, 2, S], BF16, ps)
            s.op("pool", lambda e: e.memset(KT[:], 0.0), writes=[b_w])
            Vc = T(f"Vc{l}", [128, NTB, 2, 65], BF16, ps)
            s.op("pool", lambda e: e.memset(Vc[:, :, :, 64:65], 1.0), writes=[b_w])
            s.barrier()
            s.op("act", lambda e: e.activation(out=esink[64:65, :], in_=esink[64:65, :], func=AF.Exp))
            pbk = Rot([(banks[2 * i], bbuf[2 * i], banks[2 * i + 1], bbuf[2 * i + 1]) for i in range(4)])
            t1s = Rot([(T(f"t1c{l}_{i}", [128, 512], F32, ps), Buf()) for i in range(2)])
            t2s = Rot([(T(f"t2c{l}_{i}", [128, 512], F32, ps), Buf()) for i in range(2)])
            for g in range(4):
                for qb in range(NQB):
                    bq, bbq, bp, bbp = pbk.next()
                    if g < 3:
                        proj_fm(wq, g * 128, 128, 8, xnT, qb, bq, bbq)
                        proj_fm(wqp, g * 128, 128, 8, xnT, qb, bp, bbp)
                        dst = QT[:, g, qb * 512:(qb + 1) * 512]
                    else:
                        proj_fm(wk, 0, 128, 8, xnT, qb, bq, bbq)
                        proj_fm(wkp, 0, 128, 8, xnT, qb, bp, bbp)
                        dst = None
                    t1, b_t1 = t1s.next()
                    t2, b_t2 = t2s.next()
                    ctab, stab, b_tab = tabs.next()
                    s.dma("sp", lambda e: e.dma_start(out=ctab[:], in_=cs_c_d[:, qb * 512:(qb + 1) * 512]), writes=[b_tab])
                    s.dma("sp", lambda e: e.dma_start(out=stab[:], in_=sn_c_d[:, qb * 512:(qb + 1) * 512]), writes=[b_tab])
                    s.op("dve", lambda e: e.tensor_tensor(out=t1[:], in0=bq[:, :], in1=ctab[:], op=ALU.mult), reads=[bbq, b_tab], writes=[b_t1])
                    s.op("dve", lambda e: e.tensor_tensor(out=t2[:], in0=bp[:, :], in1=stab[:], op=ALU.mult), reads=[bbp, b_tab], writes=[b_t2])
                    if g < 3:
                        s.op("pool", lambda e: e.tensor_tensor(out=t1[:], in0=t1[:], in1=t2[:], op=ALU.add), reads=[b_t1, b_t2], writes=[b_t1])
                        s.op("act", lambda e: e.mul(out=dst, in_=t1[:], mul=0.125), reads=[b_t1])
                    else:
                        for kv_ in range(2):
                            rr = slice(kv_ * 64, (kv_ + 1) * 64)
                            s.op("pool", lambda e: e.tensor_tensor(out=KT[rr, kv_, qb * 512:(qb + 1) * 512], in0=t1[rr, :], in1=t2[rr, :], op=ALU.add),
                                 reads=[b_t1, b_t2])
            pb2 = Rot([(banks[i], bbuf[i]) for i in range(4)])
            for tb in range(NTB):
                bank, bb = pb2.next()
                for c in range(8):
                    s.op("pe", lambda e: e.matmul(bank[:, 0:128], lhsT=xnT[:, c, tb * 128:(tb + 1) * 128],
                                                  rhs=wv[:, c, :], start=(c == 0), stop=(c == 7)),
                         writes=[bb], inc=(c == 7))
                evac_copy(Vc[:, tb, :, 0:64], bank[:, 0:128].rearrange("p (h d) -> p h d", h=2), [bb], [])
            s.barrier()
            spairs = Rot([(pp[0], [bbuf[0], bbuf[1]]), (pp[1], [bbuf[2], bbuf[3]]), (pp[2], [bbuf[4], bbuf[5]])])
            obk = Rot([(banks[6 + i], bbuf[6 + i]) for i in range(2)])
            pTs = Rot([(T(f"pTc{l}_{i}", [128, 1024], BF16, ps), Buf()) for i in range(4)])
            fin = Rot([(T(f"rdenC{l}_{i}", [65, 1024], F32, ps), Buf(),
                        T(f"bcsC{l}_{i}", [64, 512], F32, ps), Buf(),
                        T(f"obfC{l}_{i}", [64, 512], BF16, ps), Buf()) for i in range(2)])
            pend = []
            fin_age[0] = 1

            def swa_back(ctx):
                hh_, qb_, grp, first, last, O_bank, O_buf, pT, b_pT, fin_t = ctx
                flush_fin()
                kvh_ = hh_ // 3
                for u, kb in enumerate(grp):
                    s.op("pe", lambda e: e.matmul(O_bank[0:65, :], lhsT=Vc[:, kb, kvh_, :], rhs=pT[:, u * 512:(u + 1) * 512],
                                                  start=(first and u == 0), stop=(last and u == len(grp) - 1)),
                         reads=[b_pT], writes=[O_buf], inc=(u == len(grp) - 1))
                if last:
                    f_ = fin_t
                    finalize_heads(O_bank, O_buf, 10 + hh_, qb_ * 512, (f_[0], f_[1], None, None, f_[2], f_[3], f_[4], f_[5]),
                                   extra_den=esink[64:65, hh_:hh_ + 1], bank_src=spairs, use_act=True)

            for hh in range(6):
                kvh = hh // 3
                t = hh % 3
                pb = kvh * 64
                for qb in range(NQB):
                    O_bank, O_buf = obk.next()
                    fin_t = fin.next()
                    kbs = [kb for kb in range(4 * qb - 1, 4 * qb + 5) if 0 <= kb < NTB]
                    grps = [kbs[i:i + 2] for i in range(0, len(kbs), 2)]
                    for gi, grp in enumerate(grps):
                        Sp, b_Sp = spairs.next()
                        pT, b_pT = pTs.next()
                        for u, kb in enumerate(grp):
                            j = kb - (4 * qb - 1)
                            s.op("pe", lambda e: e.matmul(Sp[:, u * 512:(u + 1) * 512], lhsT=KT[:, kvh, kb * 128:(kb + 1) * 128],
                                                          rhs=QT[:, t, qb * 512:(qb + 1) * 512],
                                                          start=True, stop=True), writes=b_Sp, inc=(u == len(grp) - 1))
                        wdt = len(grp) * 512
                        j0 = grp[0] - (4 * qb - 1)
                        s.op("act", lambda e: e.activation(out=pT[:, 0:wdt], in_=Sp[:, 0:wdt], func=AF.Exp),
                             reads=b_Sp, writes=[b_pT])
                        s.op("dve", lambda e: e.tensor_tensor(out=pT[:, 0:wdt], in0=pT[:, 0:wdt],
                                                              in1=msk[:, j0:j0 + len(grp), :].rearrange("p j c -> p (j c)"), op=ALU.mult),
                             reads=[b_pT], writes=[b_pT])
                        pend.append((hh, qb, grp, gi == 0, gi == len(grps) - 1, O_bank, O_buf, pT, b_pT, fin_t))
                        if len(pend) > 2:
                            swa_back(pend.pop(0))
            while pend:
                swa_back(pend.pop(0))
            flush_fin(force=True)
            fin_age[0] = 3
            s.barrier()
        if stop_after == "swa":
            lst.close(); lat.close()
            break

        with ExitStack() as ps:
            cosB = T(f"cosb{l}", [96, S], F32, ps)
            sinB = T(f"sinb{l}", [96, S], F32, ps)
            wl = T(f"wl{l}", [128, 8, 384], BF16, ps)
            wkr = T(f"wkr{l}", [128, 8, 96], BF16, ps)
            wkrp = T(f"wkrp{l}", [128, 8, 96], BF16, ps)
            qn_t = T(f"qn{l}", [128, 2], F32, ps)
            kvn_t = T(f"kvn{l}", [128, 1], F32, ps)
            b_w = Buf()
            load_w_cast(wl[:], w_lat_d[l].rearrange("(c p) n -> p c n", p=128), b_w)
            load_w_cast(wkr[:], w_kr_d[l].rearrange("(c p) n -> p c n", p=128), b_w)
            load_w_cast(wkrp[:], w_krp_d[l].rearrange("(c p) n -> p c n", p=128), b_w)
            s.dma("sp", lambda e: e.dma_start(out=qn_t[:], in_=qn_d[l].rearrange("(c p) -> p c", p=128), allow_slow_non_contiguous=True), writes=[b_w])
            s.dma("sp", lambda e: e.dma_start(out=kvn_t[:], in_=kvn_d[l].rearrange("(c p) -> p c", p=128), allow_slow_non_contiguous=True), writes=[b_w])
            s.dma("sp", lambda e: e.dma_start(out=cosB[64:96, :], in_=cs_b_d), writes=[b_w])
            s.dma("sp", lambda e: e.dma_start(out=sinB[64:96, :], in_=sn_b_d), writes=[b_w])
            s.barrier()
            rb = Rot([(banks[i], bbuf[i]) for i in range(3)])
            sqs = Rot([(T(f"sq{l}_{i}", [128, 512], BF16, ps), Buf()) for i in range(3)])
            rsb = Rot([(T(f"rsb{l}_{i}", [128, 512], F32, ps), Buf()) for i in range(2)])
            rscr = T(f"rscr{l}", [128, 512], F32, ps)
            ssb = Rot([(banks[3 + i], bbuf[3 + i]) for i in range(2)])
            for qb in range(NQB):
                cols = slice(qb * 512, (qb + 1) * 512)
                raws = []
                for c2 in range(2):
                    bank, bb = rb.next()
                    proj_fm(wl, c2 * 128, 128, 8, xnT, qb, bank, bb)
                    sq, b_sq = sqs.next()
                    s.op("act", lambda e: e.activation(out=sq[:], in_=bank[:, :], func=AF.Square), reads=[bb], writes=[b_sq])
                    raws.append((bank, bb, sq, b_sq))
                ss_bank, ss_buf = ssb.next()
                for c2 in range(2):
                    s.op("pe", lambda e: e.matmul(ss_bank[:, :], lhsT=ones_bf[:], rhs=raws[c2][2][:], start=(c2 == 0), stop=(c2 == 1)),
                         reads=[raws[c2][3]], writes=[ss_buf], inc=(c2 == 1))
                rs_t, b_rs = rsb.next()
                rstd_big(ss_bank[:, :], rs_t[:], rscr[:], 1.0 / 256, [ss_buf], [b_rs])
                for c2 in range(2):
                    bank, bb = raws[c2][0], raws[c2][1]
                    s.op("dve", lambda e: e.scalar_tensor_tensor(out=cqn[:, c2, cols], in0=bank[:, :], scalar=qn_t[:, c2:c2 + 1],
                                                                 in1=rs_t[:], op0=ALU.mult, op1=ALU.mult), reads=[bb, b_rs])
                bank, bb = rb.next()
                proj_fm(wl, 256, 128, 8, xnT, qb, bank, bb)
                sq, b_sq = sqs.next()
                s.op("act", lambda e: e.activation(out=sq[:], in_=bank[:, :], func=AF.Square), reads=[bb], writes=[b_sq])
                ss_bank, ss_buf = ssb.next()
                s.op("pe", lambda e: e.matmul(ss_bank[:, :], lhsT=ones_bf[:], rhs=sq[:], start=True, stop=True), reads=[b_sq], writes=[ss_buf])
                rs_t, b_rs = rsb.next()
                rstd_big(ss_bank[:, :], rs_t[:], rscr[:], 1.0 / 128, [ss_buf], [b_rs])
                s.op("dve", lambda e: e.scalar_tensor_tensor(out=ckvn[:, cols], in0=bank[:, :], scalar=kvn_t[:, 0:1],
                                                             in1=rs_t[:], op0=ALU.mult, op1=ALU.mult), reads=[bb, b_rs])
                bank, bb = rb.next()
                proj_fm(wkr, 0, 96, 8, xnT, qb, bank, bb)
                bank2, bb2 = rb.next()
                proj_fm(wkrp, 0, 96, 8, xnT, qb, bank2, bb2)
                t1, b_t1 = rsb.next()
                t2, b_t2 = rsb.next()
                s.op("dve", lambda e: e.tensor_tensor(out=t1[64:96, :], in0=bank[64:96, :], in1=cosB[64:96, cols], op=ALU.mult), reads=[bb], writes=[b_t1])
                s.op("dve", lambda e: e.tensor_tensor(out=t2[64:96, :], in0=bank2[64:96, :], in1=sinB[64:96, cols], op=ALU.mult), reads=[bb2], writes=[b_t2])
                s.op("pool", lambda e: e.tensor_tensor(out=kpe[64:96, cols], in0=t1[64:96, :], in1=t2[64:96, :], op=ALU.add), reads=[b_t1, b_t2])
            s.barrier()
        lst.close()
        with ExitStack() as ps:
            Vb = T(f"Vb{l}", [128, NTB, 6, 65], BF16, ps)
            cosB = T(f"cosb2{l}", [96, S], F32, ps)
            sinB = T(f"sinb2{l}", [96, S], F32, ps)
            wuq = T(f"wuq{l}", [128, 2, 576], BF16, ps)
            wuqp = T(f"wuqp{l}", [128, 2, 576], BF16, ps)
            wuk = T(f"wuk{l}", [128, 384], BF16, ps)
            wuv = T(f"wuv{l}", [128, 384], BF16, ps)
            b_w = Buf()
            load_w_cast(wuq[:], w_uq_d[l].rearrange("(c p) n -> p c n", p=128), b_w)
            load_w_cast(wuqp[:], w_uqp_d[l].rearrange("(c p) n -> p c n", p=128), b_w)
            load_w_cast(wuk[:], w_uk_d[l], b_w)
            load_w_cast(wuv[:], w_uv_d[l], b_w)
            s.dma("sp", lambda e: e.dma_start(out=cosB[64:96, :], in_=cs_b_d), writes=[b_w])
            s.dma("sp", lambda e: e.dma_start(out=sinB[64:96, :], in_=sn_b_d), writes=[b_w])
            s.op("pool", lambda e: e.memset(Vb[:, :, :, 64:65], 1.0), writes=[b_w])
            s.barrier()
            pb2 = Rot([(banks[i], bbuf[i]) for i in range(4)])
            for tb in range(NTB):
                bank, bb = pb2.next()
                s.op("pe", lambda e: e.matmul(bank[:, 0:384], lhsT=ckvn[:, tb * 128:(tb + 1) * 128], rhs=wuv[:, :], start=True, stop=True), writes=[bb])
                evac_copy(Vb[:, tb, :, 0:64], bank[:, 0:384].rearrange("p (h d) -> p h d", h=6), [bb], [])
            s.barrier()
            scale_b = float(96 ** -0.5)
            QTs = [(T(f"QTb{l}_{i}", [96, S], BF16, ps), Buf()) for i in range(2)]
            KTs = [(T(f"KTb{l}_{i}", [96, S], BF16, ps), Buf()) for i in range(2)]
            t1s = Rot([(T(f"t1b{l}_{i}", [96, 512], F32, ps), Buf()) for i in range(2)])
            t2s = Rot([(T(f"t2b{l}_{i}", [96, 512], F32, ps), Buf()) for i in range(2)])
            pbk = Rot([(banks[i], bbuf[i]) for i in range(3)])
            spairs = Rot([(pp[0], [bbuf[0], bbuf[1]]), (pp[1], [bbuf[2], bbuf[3]]), (pp[2], [bbuf[4], bbuf[5]])])
            obk = Rot([(banks[6 + i], bbuf[6 + i]) for i in range(2)])
            pTs = Rot([(T(f"pTb{l}_{i}", [128, 1024], BF16, ps), Buf()) for i in range(4)])
            finB = Rot([(T(f"rdenB{l}_{i}", [65, 1024], F32, ps), Buf(),
                         T(f"bcsB{l}_{i}", [64, 512], F32, ps), Buf(),
                         T(f"obfB{l}_{i}", [64, 512], BF16, ps), Buf()) for i in range(2)])
            pend = []

            def mla_back(ctx):
                hh_, qb_, kp_, O_bank, O_buf, pT, b_pT, fin_t = ctx
                flush_fin()
                for u in range(2):
                    kb = 2 * kp_ + u
                    s.op("pe", lambda e: e.matmul(O_bank[0:65, :], lhsT=Vb[:, kb, hh_, :], rhs=pT[:, u * 512:(u + 1) * 512],
                                                  start=(kb == 0), stop=(kb == NTB - 1)),
                         reads=[b_pT], writes=[O_buf], inc=(u == 1))
                if kp_ == NTB // 2 - 1:
                    f_ = fin_t
                    finalize_heads(O_bank, O_buf, 4 + hh_, qb_ * 512, (f_[0], f_[1], None, None, f_[2], f_[3], f_[4], f_[5]), bank_src=spairs)

            def proj_step(hp, qb):
                QTh, b_Q = QTs[hp % 2]
                KTh, b_K = KTs[hp % 2]
                cols = slice(qb * 512, (qb + 1) * 512)
                Sp1, bS1 = spairs.next()
                bq, bbq = Sp1[:, 0:512], bS1[0]
                bp, bbp = Sp1[:, 512:1024], bS1[1]
                proj_fm(wuq, hp * 96, 96, 2, cqn, qb, bq, bbq)
                proj_fm(wuqp, hp * 96, 96, 2, cqn, qb, bp, bbp)
                s.op("dve", lambda e: e.tensor_copy(out=QTh[0:64, cols], in_=bq[0:64, :]), reads=[bbq], writes=[b_Q])
                t1, b_t1 = t1s.next()
                t2, b_t2 = t2s.next()
                s.op("dve", lambda e: e.tensor_tensor(out=t1[64:96, :], in0=bq[64:96, :], in1=cosB[64:96, cols], op=ALU.mult), reads=[bbq], writes=[b_t1])
                s.op("dve", lambda e: e.tensor_tensor(out=t2[64:96, :], in0=bp[64:96, :], in1=sinB[64:96, cols], op=ALU.mult), reads=[bbp], writes=[b_t2])
                s.op("pool", lambda e: e.tensor_tensor(out=QTh[64:96, cols], in0=t1[64:96, :], in1=t2[64:96, :], op=ALU.add),
                     reads=[b_t1, b_t2], writes=[b_Q])
                Sp2, bS2 = spairs.next()
                bk, bbk = Sp2[:, 0:512], bS2[0]
                s.op("pe", lambda e: e.matmul(bk[0:64, :], lhsT=wuk[:, hp * 64:(hp + 1) * 64], rhs=ckvn[:, cols], start=True, stop=True), writes=[bbk])
                s.op("dve", lambda e: e.tensor_copy(out=KTh[0:64, cols], in_=bk[0:64, :]), reads=[bbk], writes=[b_K])
                s.op("pool", lambda e: e.tensor_copy(out=KTh[64:96, cols], in_=kpe[64:96, cols]), writes=[b_K])

            for qb in range(NQB):
                proj_step(0, qb)
            for hh in range(6):
                QTh, b_Q = QTs[hh % 2]
                KTh, b_K = KTs[hh % 2]
                for qb in range(NQB):
                    if hh + 1 < 6:
                        proj_step(hh + 1, qb)
                    O_bank, O_buf = obk.next()
                    fin_t = finB.next()
                    for kp in range(NTB // 2):
                        Sp, b_Sp = spairs.next()
                        pT, b_pT = pTs.next()
                        for u in range(2):
                            kb = 2 * kp + u
                            s.op("pe", lambda e: e.matmul(Sp[:, u * 512:(u + 1) * 512], lhsT=KTh[0:96, kb * 128:(kb + 1) * 128],
                                                          rhs=QTh[0:96, qb * 512:(qb + 1) * 512], start=True, stop=True),
                                 reads=[b_Q, b_K], writes=b_Sp, inc=(u == 1))
                        s.op("act", lambda e: e.activation(out=pT[:], in_=Sp[:, :], func=AF.Exp, scale=scale_b),
                             reads=b_Sp, writes=[b_pT])
                        pend.append((hh, qb, kp, O_bank, O_buf, pT, b_pT, fin_t))
                        if len(pend) > 2:
                            mla_back(pend.pop(0))
            while pend:
                mla_back(pend.pop(0))
            flush_fin(force=True)
            s.barrier()
        lat.close()
        if stop_after == "mla":
            break

        sel = ExitStack()
        aff_all = T(f"aff{l}", [128, NTB, NE], F32, sel)
        affT = T(f"affT{l}", [128, 512], F32, sel)
        idsT = T(f"idsT{l}", [128, 64], U32, sel)
        gT = T(f"gT{l}", [128, 64], F32, sel)
        with ExitStack() as ps:
            Wo = T(f"Wo{l}", [128, 8, D], BF16, ps)
            gn_t = T(f"gn{l}", [128, 8], F32, ps)
            g2_bc = T(f"g2{l}", [128, D], F32, ps)
            wr_sb = T(f"wr{l}", [128, 8, NE], F32, ps)
            invw = T(f"invw{l}", [128, 3], F32, ps)
            b_w = Buf()
            s.dma("sp", lambda e: e.dma_start(out=gn_t[:], in_=gn_d[l].rearrange("(j p) -> p j", p=128), allow_slow_non_contiguous=True), writes=[b_w])
            s.dma("sp", lambda e: e.dma_start(out=g2_bc[:], in_=ffn_norm_d[l:l + 1, :].partition_broadcast(128)), writes=[b_w])
            s.dma("sp", lambda e: e.dma_start(out=wr_sb[:], in_=w_router_d[l].rearrange("(c p) n -> p c n", p=128)), writes=[b_w])
            s.dma("sp", lambda e: e.dma_start(out=invw[:], in_=invw_d), writes=[b_w])
            stg = Rot([(T(f"wstg{l}_{i}", [128, D], F32, ps), Buf()) for i in range(2)])
            for j in range(8):
                st_, b_st = stg.next()
                s.dma("sp", lambda e: e.dma_start(out=st_[:], in_=w_out_d[l, j * 128:(j + 1) * 128, :]), writes=[b_st])
                s.op("dve", lambda e: e.tensor_scalar(out=Wo[:, j, :], in0=st_[:], scalar1=gn_t[:, j:j + 1], scalar2=None, op0=ALU.mult),
                     reads=[b_st, b_w])
            Zs = [(T(f"Z{l}_{i}", [128, 128], F32, ps), Buf()) for i in range(8)]
            for z, bz in Zs:
                s.op("pool", lambda e: e.memset(z[:], 0.0), writes=[bz])
            s.barrier()
            ocs = Rot([(T(f"oc{l}_{i}", [128, 8, 512], BF16, ps), Buf()) for i in range(2)])
            osq = T(f"osq{l}", [128, 8, 512], BF16, ps)
            b_osq = Buf()
            hts = Rot([(T(f"h3_{l}_{i}", [128, D], F32, ps), Buf()) for i in range(3)])
            h1s = Rot([(T(f"h1_{l}_{i}", [128, D], F32, ps), Buf()) for i in range(4)])
            xfs = Rot([(T(f"xf_{l}_{i}", [128, D], F32, ps), Buf()) for i in range(3)])
            xbs = Rot([(T(f"xb3_{l}_{i}", [128, D], BF16, ps), Buf()) for i in range(2)])
            xTs = Rot([(T(f"xT3_{l}_{i}", [128, 8, 128], F32, ps), Buf()) for i in range(2)])
            junk = T(f"junk3_{l}", [128, D], BF16, ps)
            b_junk = Buf()
            smalls = Rot([(T(f"sm{l}_{i}", [128, 8], F32, ps), Buf()) for i in range(6)])
            lgs = Rot([(T(f"lg{l}_{i}", [128, NE], F32, ps), Buf()) for i in range(2)])
            mix_heads = [[0, 1], [2, 3, 4], [5, 6, 7]]
            obk = Rot([(banks[0], bbuf[0], banks[1], bbuf[1]), (banks[2], bbuf[2], banks[3], bbuf[3])])
            ssb = Rot([(banks[4], bbuf[4])])
            tpk = Rot([(banks[5], bbuf[5], banks[6], bbuf[6])])
            lgb = Rot([(banks[7], bbuf[7])])
            p3_xf = {}

            def p3_stage_b1(tb, h1, b_h1, sm, b_sm):
                s.op("dve", lambda e: e.scalar_tensor_tensor(out=junk[:], in0=h1[:], scalar=1.0, in1=h1[:],
                                                             op0=ALU.mult, op1=ALU.mult, accum_out=sm[:, 3:4]),
                     reads=[b_h1], writes=[b_junk, b_sm])
                s.op("dve", lambda e: e.tensor_scalar(out=sm[:, 3:4], in0=sm[:, 3:4], scalar1=1.0 / D, scalar2=EPS, op0=ALU.mult, op1=ALU.add),
                     reads=[b_sm], writes=[b_sm])
                s.op("act", lambda e: e.activation(out=sm[:, 3:4], in_=sm[:, 3:4], func=AF.Ln), reads=[b_sm], writes=[b_sm])
                s.op("act", lambda e: e.activation(out=sm[:, 3:4], in_=sm[:, 3:4], func=AF.Exp, scale=-0.5), reads=[b_sm], writes=[b_sm])
                xf, b_xf = xfs.next()
                xb, b_xb = xbs.next()
                s.op("dve", lambda e: e.scalar_tensor_tensor(out=xf[:], in0=h1[:], scalar=sm[:, 3:4], in1=g2_bc[:],
                                                             op0=ALU.mult, op1=ALU.mult), reads=[b_h1, b_sm], writes=[b_xf])
                s.op("act", lambda e: e.copy(out=xb[:], in_=xf[:]), reads=[b_xf], writes=[b_xb])
                s.dma("pool", lambda e: e.dma_start(out=xn2_d[tb * 128:(tb + 1) * 128, :], in_=xb[:]), reads=[b_xb])
                p3_xf[tb] = (xf, b_xf)

            def p3_stage_b2(tb, h1, b_h1, sm, b_sm):
                xf, b_xf = p3_xf.pop(tb)
                t0, bt0, t1_, bt1 = tpk.next()
                for c in range(8):
                    bk_, bbk_ = (t0, bt0) if c < 4 else (t1_, bt1)
                    s.op("pe", lambda e: e.transpose(out=bk_[:, (c % 4) * 128:(c % 4 + 1) * 128], in_=xf[:, c * 128:(c + 1) * 128], identity=ident_f[:]),
                         reads=[b_xf], writes=[bbk_], inc=(c % 4 == 3))
                xT, b_xT = xTs.next()
                s.op("act", lambda e: e.copy(out=xT[:, 0:4, :], in_=t0[:, :].rearrange("p (c t) -> p c t", c=4)), reads=[bt0], writes=[b_xT])
                s.op("act", lambda e: e.copy(out=xT[:, 4:8, :], in_=t1_[:, :].rearrange("p (c t) -> p c t", c=4)), reads=[bt1], writes=[b_xT])
                lb, blb = lgb.next()
                for c in range(8):
                    s.op("pe", lambda e: e.matmul(lb[:, 0:NE], lhsT=xT[:, c, :], rhs=wr_sb[:, c, :], start=(c == 0), stop=(c == 7)),
                         reads=[b_xT], writes=[blb], inc=(c == 7))
                lg, b_lg = lgs.next()
                s.op("dve", lambda e: e.reduce_max(out=sm[:, 4:5], in_=lb[:, 0:NE], axis=mybir.AxisListType.X), reads=[blb], writes=[b_sm])
                s.op("dve", lambda e: e.tensor_scalar(out=sm[:, 4:5], in0=sm[:, 4:5], scalar1=-1.0, scalar2=None, op0=ALU.mult), reads=[b_sm], writes=[b_sm])
                s.op("act", lambda e: e.activation(out=lg[:], in_=lb[:, 0:NE], func=AF.Exp, bias=sm[:, 4:5], accum_out=sm[:, 5:6]),
                     reads=[blb, b_sm], writes=[b_lg, b_sm])
                s.op("dve", lambda e: e.reciprocal(out=sm[:, 5:6], in_=sm[:, 5:6]), reads=[b_sm], writes=[b_sm])
                s.op("dve", lambda e: e.tensor_scalar(out=aff_all[:, tb, :], in0=lg[:], scalar1=sm[:, 5:6], scalar2=None, op0=ALU.mult),
                     reads=[b_lg, b_sm])

            p3_pend = []
            for qb in range(NQB):
                oc, b_oc = ocs.next()
                ocv = oc_d.rearrange("(jj two) p t -> two p jj t", two=2)
                for two in range(2):
                    s.dma("sp", lambda e: e.dma_start(out=oc[two * 64:(two + 1) * 64, :, :], in_=ocv[two][:, :, qb * 512:(qb + 1) * 512]), writes=[b_oc])
                s.op("pool", lambda e: e.tensor_tensor(out=osq[:], in0=oc[:], in1=oc[:], op=ALU.mult), reads=[b_oc], writes=[b_osq])
                for sub in range(4):
                    tb = qb * 4 + sub
                    tc_ = slice(sub * 128, (sub + 1) * 128)
                    ht, b_ht = hts.next()
                    s.dma("sp", lambda e: e.dma_start(out=ht[:], in_=src_d[tb * 128:(tb + 1) * 128, :]), writes=[b_ht])
                    ss_bank, ss_buf = ssb.next()
                    for m in range(3):
                        hs = mix_heads[m]
                        for i, j in enumerate(hs):
                            s.op("pe", lambda e: e.matmul(ss_bank[:, m:m + 1], lhsT=osq[:, j, tc_], rhs=ones_bf[:, 0:1],
                                                          start=(i == 0), stop=(i == len(hs) - 1)),
                                 reads=[b_osq], writes=[ss_buf], inc=(i == len(hs) - 1))
                    sm, b_sm = smalls.next()
                    s.op("dve", lambda e: e.tensor_tensor(out=sm[:, 0:3], in0=ss_bank[:, 0:3], in1=invw[:], op=ALU.mult), reads=[ss_buf], writes=[b_sm])
                    s.op("dve", lambda e: e.tensor_scalar(out=sm[:, 0:3], in0=sm[:, 0:3], scalar1=EPS, scalar2=None, op0=ALU.add), reads=[b_sm], writes=[b_sm])
                    s.op("act", lambda e: e.activation(out=sm[:, 0:3], in_=sm[:, 0:3], func=AF.Ln), reads=[b_sm], writes=[b_sm])
                    s.op("act", lambda e: e.activation(out=sm[:, 0:3], in_=sm[:, 0:3], func=AF.Exp, scale=-0.5), reads=[b_sm], writes=[b_sm])
                    if p3_pend:
                        p3_stage_b1(*p3_pend[0])
                    h1, b_h1 = h1s.next()
                    prev, b_prev = ht, b_ht
                    for m in range(3):
                        hs = mix_heads[m]
                        b0, bb0, b1, bb1 = obk.next()
                        for half, (bk_, bbk_) in enumerate(((b0, bb0), (b1, bb1))):
                            for i, j in enumerate(hs):
                                s.op("pe", lambda e: e.matmul(bk_[:, :], lhsT=oc[:, j, tc_], rhs=Wo[:, j, half * 512:(half + 1) * 512],
                                                              start=(i == 0), stop=(i == len(hs) - 1)),
                                     reads=[b_oc], writes=[bbk_], inc=(i == len(hs) - 1))
                            hc = slice(half * 512, (half + 1) * 512)
                            s.op("dve", lambda e: e.scalar_tensor_tensor(out=h1[:, hc], in0=bk_[:, :], scalar=sm[:, m:m + 1], in1=prev[:, hc],
                                                                         op0=ALU.mult, op1=ALU.add),
                                 reads=[bbk_, b_sm, b_prev], writes=[b_h1])
                        prev, b_prev = h1, b_h1
                    s.dma("pool", lambda e: e.dma_start(out=h_d[tb * 128:(tb + 1) * 128, :], in_=h1[:]), reads=[b_h1])
                    if p3_pend:
                        p3_stage_b2(*p3_pend.pop(0))
                    p3_pend.append((tb, h1, b_h1, sm, b_sm))
            while p3_pend:
                p3_stage_b1(*p3_pend[0])
                p3_stage_b2(*p3_pend.pop(0))
            s.barrier()
            for jc in range(4):
                for part in range(8):
                    tb = part * 4 + jc
                    z, bz = Zs[part]
                    zv = z[:].rearrange("p (e q) -> p e q", q=8)[:, :, part:part + 1]
                    s.op("dve", lambda e: e.tensor_copy(out=zv, in_=aff_all[:, tb, :].rearrange("p (e o) -> p e o", o=1)), writes=[bz])
                    s.op("pe", lambda e: e.matmul(banks[0][:, jc * 128:(jc + 1) * 128], lhsT=z[:], rhs=ident_f[:],
                                                  start=(part == 0), stop=(part == 7)), reads=[bz], writes=[bbuf[0]])
            s.op("act", lambda e: e.copy(out=affT[:], in_=banks[0][:, :]), reads=[bbuf[0]])
            s.barrier()
        if stop_after == "p3":
            sel.close()
            break

        wst = ExitStack()
        Ws = [(T(f"Wg{l}_{i}", [128, 8, D], BF16, wst), T(f"Wu{l}_{i}", [128, 8, D], BF16, wst),
               T(f"Wd{l}_{i}", [128, 8, D], BF16, wst), Buf()) for i in range(2)]
        stg = Rot([(T(f"wst{l}_{i}", [128, D], F32, wst), Buf()) for i in range(8)])

        def load_expert_steps(e_):
            Wg, Wu, Wd, b_W = Ws[e_ % 2]
            steps = []
            for dst, srcw in ((Wg, w_gate_d), (Wu, w_up_d), (Wd, w_down_d)):
                for c in range(8):
                    def step(dst=dst, srcw=srcw, c=c):
                        st_, b_st = stg.next()
                        s.dma("sp", lambda e: e.dma_start(out=st_[:], in_=srcw[l, e_, c * 128:(c + 1) * 128, :]), writes=[b_st])
                        s.op("act", lambda e: e.copy(out=dst[:, c, :], in_=st_[:]), reads=[b_st], writes=[b_W])
                    steps.append(step)
            return steps

        def load_expert(e_):
            for st in load_expert_steps(e_):
                st()

        load_expert(0)
        with ExitStack() as ps:
            gmat = T(f"gmat{l}", [128, 128], F32, ps)
            rowoff = T(f"rowoff{l}", [128, 1], F32, ps)
            b_c = Buf()
            s.dma("sp", lambda e: e.dma_start(out=gmat[:], in_=gmat_d), writes=[b_c])
            s.dma("sp", lambda e: e.dma_start(out=rowoff[:], in_=rowoff_d), writes=[b_c])
            mid = T(f"mid{l}", [128, 1], F32, ps)
            lo = T(f"lo{l}", [128, 1], F32, ps)
            cnt = T(f"cnt{l}", [128, 1], F32, ps)
            gef = T(f"gef{l}", [128, 1], F32, ps)
            tt = T(f"tt{l}", [128, 1], F32, ps)
            cmpj = T(f"cmpj{l}", [128, 512], F32, ps)
            am = T(f"am{l}", [128, 512], F32, ps)
            vals = T(f"vals{l}", [128, ROUNDS * 8], F32, ps)
            idxs = T(f"idxs{l}", [128, ROUNDS * 8], mybir.dt.uint16, ps)
            idf = T(f"idf{l}", [128, ROUNDS * 8], F32, ps)
            vmask = T(f"vmask{l}", [128, ROUNDS * 8], F32, ps)
            nrow = T(f"nrow{l}", [128, 1], F32, ps)
            off_sb = T(f"off{l}", [128, 1], F32, ps)
            diag = T(f"diag{l}", [128, 128], F32, ps)
            offB = T(f"offB{l}", [128, 128], F32, ps)
            RT = T(f"RT{l}", [128, ROUNDS * 8 // 128, 128, 4], BF16, ps)
            trimat = T(f"trimat{l}", [128, 128], F32, ps)
            dmat = T(f"dmat{l}", [128, ROUNDS * 8 // 128, 512], mybir.dt.int16, ps)
            s.dma("sp", lambda e: e.dma_start(out=trimat[:], in_=trimat_d), writes=[b_c])
            s.dma("sp", lambda e: e.dma_start(out=dmat[:], in_=dmat_d), writes=[b_c])
            b_s = Buf()
            s.op("dve", lambda e: e.memset(mid[:], 0.5), writes=[b_s])
            s.op("dve", lambda e: e.memset(lo[:], 0.0), writes=[b_s])
            s.barrier()
            step = 0.5
            cb = banks[1]
            b_cb = bbuf[1]
            for it in range(NBIS):
                s.op("dve", lambda e: e.tensor_scalar(out=cmpj[:], in0=affT[:], scalar1=mid[:, 0:1], scalar2=None,
                                                      op0=ALU.is_ge, op1=ALU.add, accum_out=cnt[:]), reads=[b_s], writes=[b_s])
                s.op("pe", lambda e: e.matmul(cb[:, 0:1], lhsT=gmat[:], rhs=cnt[:], start=True, stop=True), reads=[b_s], writes=[b_cb])
                s.op("dve", lambda e: e.tensor_scalar(out=gef[:], in0=cb[:, 0:1], scalar1=511.5, scalar2=None, op0=ALU.is_ge), reads=[b_cb], writes=[b_s])
                s.op("dve", lambda e: e.scalar_tensor_tensor(out=lo[:], in0=gef[:], scalar=mid[:, 0:1], in1=lo[:], op0=ALU.mult, op1=ALU.max),
                     reads=[b_s], writes=[b_s])
                step *= 0.5
                st2 = step
                s.op("dve", lambda e: e.tensor_scalar(out=tt[:], in0=gef[:], scalar1=2.0 * st2, scalar2=-st2, op0=ALU.mult, op1=ALU.add),
                     reads=[b_s], writes=[b_s])
                s.op("dve", lambda e: e.tensor_tensor(out=mid[:], in0=mid[:], in1=tt[:], op=ALU.add), reads=[b_s], writes=[b_s])
            s.op("dve", lambda e: e.scalar_tensor_tensor(out=am[:], in0=affT[:], scalar=lo[:, 0:1], in1=affT[:], op0=ALU.is_ge, op1=ALU.mult),
                 reads=[b_s], writes=[b_s])
            for r in range(ROUNDS):
                sl = slice(r * 8, (r + 1) * 8)
                s.op("dve", lambda e: e.max(out=vals[:, sl], in_=am[:]), reads=[b_s], writes=[b_s])
                s.op("dve", lambda e: e.max_index(out=idxs[:, sl], in_max=vals[:, sl], in_values=am[:]), reads=[b_s], writes=[b_s])
                s.op("dve", lambda e: e.match_replace(out=am[:], in_to_replace=vals[:, sl], in_values=am[:], imm_value=-1.0), reads=[b_s], writes=[b_s])
            NSL = ROUNDS * 8
            NIC = NSL // 128
            U8 = mybir.dt.uint8
            s.op("dve", lambda e: e.tensor_scalar(out=vmask[:], in0=vals[:], scalar1=0.0, scalar2=None, op0=ALU.is_gt, op1=ALU.add, accum_out=nrow[:]),
                 reads=[b_s], writes=[b_s])
            s.op("dve", lambda e: e.tensor_tensor(out=vals[:], in0=vals[:], in1=vmask[:], op=ALU.mult), reads=[b_s], writes=[b_s])
            idx8 = idxs[:].bitcast(U8).rearrange("p (n two) -> p n two", two=2)
            dig = T(f"dig{l}", [128, 4, NSL], BF16, ps)
            tmpf = T(f"tmpf{l}", [128, NSL], F32, ps)
            s.op("dve", lambda e: e.tensor_copy(out=tmpf[:].rearrange("p (n o) -> p n o", o=1), in_=idx8[:, :, 1:2]), reads=[b_s], writes=[b_s])
            s.op("dve", lambda e: e.scalar_tensor_tensor(out=dig[:, 0, :], in0=tmpf[:], scalar=rowoff[:, 0:1], in1=vmask[:], op0=ALU.add, op1=ALU.mult),
                 reads=[b_s, b_c], writes=[b_s])
            s.op("dve", lambda e: e.tensor_copy(out=tmpf[:].rearrange("p (n o) -> p n o", o=1), in_=idx8[:, :, 0:1]), reads=[b_s], writes=[b_s])
            s.op("dve", lambda e: e.tensor_tensor(out=dig[:, 1, :], in0=tmpf[:], in1=vmask[:], op=ALU.mult), reads=[b_s], writes=[b_s])
            s.op("dve", lambda e: e.tensor_copy(out=dig[:, 2, :], in_=vals[:]), reads=[b_s], writes=[b_s])
            s.op("dve", lambda e: e.tensor_tensor(out=dig[:, 3, :], in0=vals[:], in1=dig[:, 2, :], op=ALU.subtract), reads=[b_s], writes=[b_s])
            s.op("pe", lambda e: e.matmul(banks[2][:, 0:1], lhsT=trimat[:], rhs=nrow[:], start=True, stop=True), reads=[b_s, b_c], writes=[bbuf[2]])
            s.op("dve", lambda e: e.tensor_copy(out=off_sb[:], in_=banks[2][:, 0:1]), reads=[bbuf[2]], writes=[b_s])
            s.op("dve", lambda e: e.tensor_scalar(out=diag[:], in0=ident_f[:], scalar1=off_sb[:, 0:1], scalar2=None, op0=ALU.mult), reads=[b_s], writes=[b_s])
            s.op("pe", lambda e: e.matmul(banks[3][:, 0:128], lhsT=ones_f[:], rhs=diag[:], start=True, stop=True), reads=[b_s], writes=[bbuf[3]])
            s.op("act", lambda e: e.copy(out=offB[:], in_=banks[3][:, 0:128]), reads=[bbuf[3]], writes=[b_s])
            tb2 = banks[0][:].bitcast(BF16)
            for ic in range(NIC):
                for k in range(4):
                    col = (ic * 4 + k) * 128
                    s.op("pe", lambda e: e.transpose(out=tb2[:, col:col + 128], in_=dig[:, k, ic * 128:(ic + 1) * 128], identity=ident_bf[:]),
                         reads=[b_s], writes=[bbuf[0]])
                s.op("dve", lambda e: e.tensor_copy(out=RT[:, ic, :, :].rearrange("p r k -> p k r"),
                                                    in_=tb2[:, ic * 512:(ic + 1) * 512].rearrange("p (k r) -> p k r", k=4)), reads=[bbuf[0]], writes=[b_s])
            s.barrier()
            sels = Rot([(T(f"selt{l}_{i}", [128, 512], BF16, ps), Buf()) for i in range(4)])
            for row in range(128):
                e_ = row // 8
                for ic in range(NIC):
                    sel_t, b_sel = sels.next()
                    s.op("dve", lambda e: e.tensor_scalar(out=sel_t[:], in0=dmat[:, ic, :], scalar1=offB[:, row:row + 1], scalar2=None, op0=ALU.is_equal),
                         writes=[b_sel])
                    first = (row % 8 == 0 and ic == 0)
                    last = (row % 8 == 7 and ic == NIC - 1)
                    for jc in range(4):
                        s.op("pe", lambda e: e.matmul(banks[4 + jc][:, e_ * 4:e_ * 4 + 4], lhsT=sel_t[:, jc * 128:(jc + 1) * 128], rhs=RT[:, ic, row, :],
                                                      start=first, stop=last), reads=[b_sel], writes=[bbuf[4 + jc]], inc=(jc == 3))
            rs_sb = T(f"rs_sb{l}", [128, 4, NE, 4], F32, ps)
            b_rs = Buf()
            for jc in range(4):
                s.op("act", lambda e: e.copy(out=rs_sb[:, jc, :, :], in_=banks[4 + jc][:, 0:4 * NE].rearrange("p (e t) -> p e t", t=4)),
                     reads=[bbuf[4 + jc]], writes=[b_rs])
            dg = lambda k: rs_sb[:, :, :, k:k + 1].rearrange("p j e o -> p j (e o)")
            s.op("dve", lambda e: e.scalar_tensor_tensor(out=idsT[:].rearrange("p (e j) -> p j e", j=4), in0=dg(0), scalar=256.0, in1=dg(1),
                                                         op0=ALU.mult, op1=ALU.add), reads=[b_rs])
            s.op("dve", lambda e: e.tensor_tensor(out=gT[:].rearrange("p (e j) -> p j e", j=4), in0=dg(2), in1=dg(3), op=ALU.add), reads=[b_rs])
            if debug:
                s.barrier()
                s.dma("sp", lambda e: e.dma_start(out=aff_dbg, in_=affT[:]))
                s.dma("sp", lambda e: e.dma_start(out=sel_dbg[:, 0:64], in_=idsT[:].bitcast(F32)))
                s.dma("sp", lambda e: e.dma_start(out=sel_dbg[:, 64:128], in_=gT[:]))
            s.barrier()
        if stop_after == "p4":
            s.barrier()
            wst.close()
            sel.close()
            break

        with ExitStack() as ps:
            xss = Rot([(T(f"xs{l}_{i}", [128, D], BF16, ps), Buf()) for i in range(8)])
            xsTs = [(T(f"xsT{l}_{i}", [128, 8, 512], BF16, ps), Buf()) for i in range(2)]
            hidTs = Rot([(T(f"hidT{l}_{i}", [128, 8, 512], BF16, ps), Buf()) for i in range(2)])
            sgs = Rot([(T(f"sg{l}_{i}", [128, 512], F32, ps), Buf()) for i in range(2)])
            ys = Rot([(T(f"y{l}_{i}", [128, D], F32, ps), Buf()) for i in range(2)])
            tpk = Rot([(banks[0], bbuf[0]), (banks[1], bbuf[1])])
            gub = Rot([(banks[2], bbuf[2], banks[3], bbuf[3]), (banks[4], bbuf[4], banks[5], bbuf[5])])
            ybk = Rot([(banks[6], bbuf[6]), (banks[7], bbuf[7])])
            hreg = [[Buf(f"hreg{q}_{p}") for p in range(ROWS_PER_E)] for q in range(2)]
            NCH = NE * SLOT_CHUNKS

            def rows_of(ch):
                e_, half = ch // SLOT_CHUNKS, ch % SLOT_CHUNKS
                return [e_ * ROWS_PER_E + half * 4 + i for i in range(4)]

            gathered = {}

            def issue_gathers(ch):
                lst_ = []
                for r in rows_of(ch):
                    xs, b_xs = xss.next()
                    s.dma("pool", lambda e: e.indirect_dma_start(out=xs[:], out_offset=None, in_=xn2_d,
                                                                 in_offset=bass.IndirectOffsetOnAxis(ap=idsT[:, r:r + 1], axis=0)),
                          writes=[b_xs])
                    lst_.append((xs, b_xs))
                gathered[ch] = lst_

            def do_transposes(ch):
                xsT, b_xsT = xsTs[ch % 2]
                for i, (xs, b_xs) in enumerate(gathered.pop(ch)):
                    pt, b_pt = tpk.next()
                    ptv = pt[:].bitcast(BF16)
                    for c in range(8):
                        s.op("pe", lambda e: e.transpose(out=ptv[:, c * 128:(c + 1) * 128], in_=xs[:, c * 128:(c + 1) * 128], identity=ident_bf[:]),
                             reads=[b_xs], writes=[b_pt], inc=(c == 7))
                    s.op("dve", lambda e: e.tensor_copy(out=xsT[:, :, i * 128:(i + 1) * 128], in_=ptv.rearrange("p (c t) -> p c t", c=8)),
                         reads=[b_pt], writes=[b_xsT])

            issue_gathers(0)
            do_transposes(0)
            for ch in range(NCH):
                e_ = ch // SLOT_CHUNKS
                Wg, Wu, Wd, b_W = Ws[e_ % 2]
                wsteps = load_expert_steps(e_ + 1) if (ch % SLOT_CHUNKS == 0 and e_ + 1 < NE) else []
                if ch + 1 < NCH:
                    issue_gathers(ch + 1)
                xsT, b_xsT = xsTs[ch % 2]
                hidT, b_hid = hidTs.next()
                for f in range(8):
                    for _ in range(3):
                        if wsteps:
                            wsteps.pop(0)()
                    gb, bgb, ub, bub = gub.next()
                    for c in range(8):
                        s.op("pe", lambda e: e.matmul(gb[:, :], lhsT=Wg[:, c, f * 128:(f + 1) * 128], rhs=xsT[:, c, :], start=(c == 0), stop=(c == 7)),
                             reads=[b_W, b_xsT], writes=[bgb], inc=(c == 7))
                    for c in range(8):
                        s.op("pe", lambda e: e.matmul(ub[:, :], lhsT=Wu[:, c, f * 128:(f + 1) * 128], rhs=xsT[:, c, :], start=(c == 0), stop=(c == 7)),
                             reads=[b_W, b_xsT], writes=[bub], inc=(c == 7))
                    sg, b_sg = sgs.next()
                    s.op("act", lambda e: e.activation(out=sg[:], in_=gb[:, :], func=AF.Silu), reads=[bgb], writes=[b_sg])
                    s.op("dve", lambda e: e.tensor_tensor(out=hidT[:, f, :], in0=ub[:, :], in1=sg[:], op=ALU.mult), reads=[bub, b_sg], writes=[b_hid])
                if ch + 1 < NCH:
                    do_transposes(ch + 1)
                for i, r in enumerate(rows_of(ch)):
                    part = r % ROWS_PER_E
                    y, b_y = ys.next()
                    for hc in range(2):
                        yb, byb = ybk.next()
                        for f in range(8):
                            s.op("pe", lambda e: e.matmul(yb[:, :], lhsT=hidT[:, f, i * 128:(i + 1) * 128], rhs=Wd[:, f, hc * 512:(hc + 1) * 512],
                                                          start=(f == 0), stop=(f == 7)),
                                 reads=[b_W, b_hid], writes=[byb], inc=(f == 7))
                        s.op("dve", lambda e: e.tensor_scalar(out=y[:, hc * 512:(hc + 1) * 512], in0=yb[:, :], scalar1=gT[:, r:r + 1], scalar2=None,
                                                              op0=ALU.mult), reads=[byb], writes=[b_y])
                    s.dma("pool", lambda e: e.indirect_dma_start(out=h_d, out_offset=bass.IndirectOffsetOnAxis(ap=idsT[:, r:r + 1], axis=0),
                                                                 in_=y[:], in_offset=None, compute_op=ALU.add),
                          reads=[b_y] + hreg[(e_ + 1) % 2], writes=[hreg[e_ % 2][part]])
            s.barrier()
        wst.close()
        sel.close()

    if stop_after is None:
        with ExitStack() as ps:
            g_bc = T("gfin", [128, D], F32, ps)
            b_g = Buf()
            s.dma("sp", lambda e: e.dma_start(out=g_bc[:], in_=final_norm_d[0:1, :].partition_broadcast(128)), writes=[b_g])
            hts = Rot([(T(f"htf_{i}", [128, D], F32, ps), Buf()) for i in range(3)])
            ots = Rot([(T(f"otf_{i}", [128, D], F32, ps), Buf()) for i in range(3)])
            junk = T("junkf", [128, D], BF16, ps)
            b_junk = Buf()
            sss = Rot([(T(f"ssf_{i}", [128, 1], F32, ps), Buf()) for i in range(2)])
            for tb in range(NTB):
                ht, b_ht = hts.next()
                ot, b_ot = ots.next()
                ss, b_ss = sss.next()
                s.dma("sp", lambda e: e.dma_start(out=ht[:], in_=h_d[tb * 128:(tb + 1) * 128, :]), writes=[b_ht])
                s.op("dve", lambda e: e.scalar_tensor_tensor(out=junk[:], in0=ht[:], scalar=1.0, in1=ht[:],
                                                             op0=ALU.mult, op1=ALU.mult, accum_out=ss[:]),
                     reads=[b_ht], writes=[b_junk, b_ss])
                rstd_from_ss(ss[:], ss[:], 1.0 / D, [b_ss], [b_ss])
                s.op("dve", lambda e: e.scalar_tensor_tensor(out=ot[:], in0=ht[:], scalar=ss[:, 0:1], in1=g_bc[:],
                                                             op0=ALU.mult, op1=ALU.mult), reads=[b_ht, b_ss, b_g], writes=[b_ot])
                s.dma("sp", lambda e: e.dma_start(out=out_d[tb * 128:(tb + 1) * 128, :], in_=ot[:]), reads=[b_ot])
    s.barrier()
    es.close()
    return nc


def _swap_halves(w, hd):
    sh = w.shape
    w4 = w.reshape(sh[:-1] + (sh[-1] // hd, 2, hd // 2))
    return np.ascontiguousarray(w4[..., ::-1, :]).reshape(sh)


def _rope_tables(dim):
    inv = (1.0 / (np.float32(10000.0) ** (np.arange(0, dim, 2, dtype=np.float32) / np.float32(dim)))).astype(np.float32)
    ang = np.arange(S, dtype=np.float32)[:, None] * inv[None, :]
    return np.cos(ang).astype(np.float32), np.sin(ang).astype(np.float32)


def _host_prep(inp):
    f = lambda a: np.ascontiguousarray(a, dtype=np.float32)
    w_in = np.asarray(inp["w_in"])
    shared = {}
    shared["attn_norm"] = f(inp["attn_norm"])
    shared["ffn_norm"] = f(inp["ffn_norm"])
    shared["final_norm"] = f(np.asarray(inp["final_norm"]).reshape(1, D))
    shared["w_na"] = f(w_in[:, :, 0:768])
    shared["w_lat"] = f(w_in[:, :, 768:1152])
    kr96 = w_in[:, :, 1088:1184]
    shared["w_kr"] = f(kr96)
    krp = np.array(kr96, copy=True)
    krp[:, :, 64:96] = _swap_halves(kr96[:, :, 64:96], 32)
    shared["w_krp"] = f(krp)
    cq = w_in[:, :, 1184:1568].reshape(L, D, 6, 64)
    order = [0, 3, 1, 4, 2, 5]
    cq_r = cq[:, :, order, :].reshape(L, D, 384)
    shared["w_cq"] = f(cq_r)
    shared["w_cqp"] = f(_swap_halves(cq_r, 64))
    ck = w_in[:, :, 1568:1696]
    shared["w_ck"] = f(ck)
    shared["w_ckp"] = f(_swap_halves(ck, 64))
    shared["w_cv"] = f(w_in[:, :, 1696:1824])
    rpb = np.asarray(inp["na_rpb"], dtype=np.float32)
    tiles = np.full((L, NV, 4, 128, 320), NEG, np.float32)
    p = np.arange(128)
    kc = p % 64
    krl = p // 64
    c = np.arange(64)
    cs_ = np.clip(c - 8, 0, 48)
    colvalid = (kc[:, None] >= cs_[None, :]) & (kc[:, None] <= cs_[None, :] + 15)
    dc = np.clip(kc[:, None] - c[None, :] + 15, 0, 30)
    for v, (d0, roff, nb) in enumerate(NA_KEYS):
        for b in range(nb):
            kr_rel = 2 * b + krl
            rowvalid = (kr_rel >= roff) & (kr_rel <= roff + 7)
            dr = np.clip(d0 + kr_rel + 7, 0, 14)
            vals = rpb[:, :, dr[:, None], dc]
            ok = (rowvalid[:, None] & colvalid)[None, None]
            tiles[:, v, :, :, b * 64:(b + 1) * 64] = np.where(ok, vals, np.float32(NEG))
    shared["rpb_tiles"] = tiles
    shared["mla_q_norm"] = f(inp["mla_q_norm"])
    shared["mla_kv_norm"] = f(inp["mla_kv_norm"])
    w_uq = np.asarray(inp["mla_w_uq"])
    shared["w_uq"] = f(w_uq)
    uq4 = np.array(w_uq.reshape(L, 256, 6, 96), copy=True)
    uq4[..., 64:96] = _swap_halves(uq4[..., 64:96], 32)
    shared["w_uqp"] = f(uq4.reshape(L, 256, 576))
    ukv = np.asarray(inp["mla_w_ukv"]).reshape(L, 128, 6, 128)
    shared["w_uk"] = f(ukv[..., 0:64].reshape(L, 128, 384))
    shared["w_uv"] = f(ukv[..., 64:128].reshape(L, 128, 384))
    shared["swa_sink"] = f(inp["swa_sink"])
    shared["group_norm"] = f(inp["group_norm"])
    shared["w_out"] = f(inp["w_out"])
    shared["w_router"] = f(inp["w_router"])
    shared["w_gate"] = f(inp["w_gate"])
    shared["w_up"] = f(inp["w_up"])
    shared["w_down"] = f(inp["w_down"])
    cos, sin = _rope_tables(64)
    cT = np.concatenate([cos.T, cos.T], 0)
    sT = np.concatenate([-sin.T, sin.T], 0)
    shared["rope_c_cos"] = f(np.concatenate([cT, cT], 0))
    shared["rope_c_sin"] = f(np.concatenate([sT, sT], 0))
    cos, sin = _rope_tables(32)
    shared["rope_b_cos"] = f(np.concatenate([cos.T, cos.T], 0))
    shared["rope_b_sin"] = f(np.concatenate([-sin.T, sin.T], 0))
    k = np.arange(128)[:, None]
    q = np.arange(512)[None, :]
    m = np.zeros((6, 128, 512), np.float32)
    for j in range(6):
        diff = q - k - (j - 1) * 128
        m[j] = np.where(np.abs(diff) <= 128, 1.0, 0.0)
    shared["swa_mask"] = m.astype(ml_dtypes.bfloat16)
    shared["ident_bf"] = np.eye(128, dtype=np.float32).astype(ml_dtypes.bfloat16)
    shared["ident_f"] = np.eye(128, dtype=np.float32)
    pp = np.arange(128)
    shared["gmat"] = (pp[:, None] // 8 == pp[None, :] // 8).astype(np.float32)
    shared["rowoff"] = ((pp % 8) * 2).astype(np.float32).reshape(128, 1)
    shared["trimat"] = ((pp[:, None] // 8 == pp[None, :] // 8) & (pp[:, None] < pp[None, :])).astype(np.float32)
    nic = ROUNDS * 8 // 128
    ii = np.arange(128)[:, None, None]
    icc = np.arange(nic)[None, :, None]
    jj = np.arange(512)[None, None, :]
    shared["dmat"] = (jj - ii - icc * 128).astype(np.int16)
    shared["invw"] = np.tile(np.array([[1 / 256, 1 / 384, 1 / 384]], np.float32), (128, 1))
    return shared


def kernel(**inputs):
    shared = _host_prep(inputs)
    x = np.asarray(inputs["x"], dtype=np.float32)
    nc = build()
    in_maps = []
    for c in range(NCORES):
        m = dict(shared)
        m["x"] = np.ascontiguousarray(x[c])
        in_maps.append(m)
    res = run_bass_kernel_spmd(nc, in_maps, core_ids=list(range(NCORES)))
    return np.stack([np.asarray(res.results[c]["out"], dtype=np.float32) for c in range(NCORES)], axis=0)
```

```python
import numpy as np
import ml_dtypes
from contextlib import ExitStack
import concourse.bass as bass
import concourse.mybir as mybir
from concourse.bass_utils import run_bass_kernel_spmd

F32 = mybir.dt.float32
BF16 = mybir.dt.bfloat16
U32 = mybir.dt.uint32
AF = mybir.ActivationFunctionType
ALU = mybir.AluOpType

S = 4096
D = 1024
NTB = 32
NQB = 8
L = 2
NE = 16
EPS = 1e-6
NEG = -30000.0
NCORES = 8
ROUNDS = 32
ROWS_PER_E = 4
SLOT_CHUNKS = 1
NBIS = 27


class Buf:
    __slots__ = ("name", "writer", "readers")

    def __init__(self, name=""):
        self.name = name
        self.writer = None
        self.readers = []


class Sched:
    ENG = ("pe", "act", "dve", "pool", "sp")

    def __init__(self, nc, es, same_engine_sync=True):
        self.nc = nc
        self.e = {"pe": nc.tensor, "act": nc.scalar, "dve": nc.vector,
                  "pool": nc.gpsimd, "sp": nc.sync}
        self.sem = {k: es.enter_context(nc.semaphore("s_" + k)) for k in self.ENG}
        self.seq = {k: 0 for k in self.ENG}
        self.waited = {a: {} for a in self.ENG}
        self.same_engine_sync = same_engine_sync
        self.lanes = {}
        self.lane_rr = {}
        for q, n in (("sp", 16), ("pool", 16)):
            self.lanes[q] = [[es.enter_context(nc.semaphore(f"d_{q}{i}")), 0] for i in range(n)]
            self.lane_rr[q] = 0
        self.pending_reads = {k: [] for k in self.ENG}

    def _wait(self, on, dep):
        if dep is None:
            return
        if dep[0] == "eng":
            _, eng, seq = dep
            if eng == on and (not self.same_engine_sync or on == "pe"):
                return
            if self.waited[on].get(eng, 0) >= seq:
                return
            self.e[on].wait_ge(self.sem[eng], seq)
            self.waited[on][eng] = seq
        else:
            _, q, li, cnt = dep
            key = ("dma", q, li)
            if self.waited[on].get(key, 0) >= cnt:
                return
            self.e[on].wait_ge(self.lanes[q][li][0], 16 * cnt)
            self.waited[on][key] = cnt

    def _deps(self, on, reads, writes):
        for r in reads:
            self._wait(on, r.writer)
        for w in writes:
            self._wait(on, w.writer)
            for d in w.readers:
                self._wait(on, d)

    def _commit(self, dep, reads, writes):
        for w in writes:
            w.writer = dep
            w.readers = []
        for r in reads:
            if r not in writes:
                r.readers.append(dep)
                if len(r.readers) > 48:
                    last = {}
                    for d in r.readers:
                        k = d[:2] if d[0] == "eng" else d[:3]
                        if k not in last or d[-1] > last[k][-1]:
                            last[k] = d
                    r.readers = list(last.values())

    def op(self, on, fn, reads=(), writes=(), inc=True):
        reads = list(reads)
        writes = list(writes)
        self._deps(on, reads, writes)
        inst = fn(self.e[on])
        if inc:
            self.seq[on] += 1
            inst.then_inc(self.sem[on], 1)
            dep = ("eng", on, self.seq[on])
            self._commit(dep, reads + self.pending_reads[on], writes)
            self.pending_reads[on] = []
        else:
            self.pending_reads[on].extend(reads)
            for w in writes:
                w.writer = ("eng", on, self.seq[on] + 1)
                w.readers = []
        return inst

    def dma(self, q, fn, reads=(), writes=()):
        reads = list(reads)
        writes = list(writes)
        lanes = self.lanes[q]
        li = self.lane_rr[q]
        self.lane_rr[q] = (li + 1) % len(lanes)
        sem, cnt = lanes[li]
        if cnt > 0:
            self._wait(q, ("dma", q, li, cnt))
        self._deps(q, reads, writes)
        inst = fn(self.e[q])
        inst.then_inc(sem, 16)
        lanes[li][1] = cnt + 1
        dep = ("dma", q, li, cnt + 1)
        self._commit(dep, reads, writes)
        return inst

    def barrier(self):
        for a in self.ENG:
            for b in self.ENG:
                if a != b and self.seq[b] > 0:
                    self._wait(a, ("eng", b, self.seq[b]))
            for q in self.lanes:
                for li, (sem, cnt) in enumerate(self.lanes[q]):
                    if cnt > 0:
                        self._wait(a, ("dma", q, li, cnt))


class Rot:
    def __init__(self, items):
        self.items = items
        self.i = 0

    def next(self):
        it = self.items[self.i]
        self.i = (self.i + 1) % len(self.items)
        return it


def _na_row_info(r):
    rs = min(max(r - 4, 0), 56)
    if rs % 2 == 0:
        start, nb = rs, 4
    else:
        start, nb = rs - 1, 5
    return rs, start, nb


def _na_variants():
    keys = []
    vmap = {}
    for r in range(64):
        rs, start, nb = _na_row_info(r)
        k = (start - r, rs - start, nb)
        if k not in keys:
            keys.append(k)
        vmap[r] = keys.index(k)
    return keys, vmap


NA_KEYS, NA_VMAP = _na_variants()
NV = len(NA_KEYS)


def build(debug=False, nlayers=L, stop_after=None):
    nc = bass.Bass("TRN2", target_bir_lowering=False)
    es = ExitStack()

    def din(name, shape, dt=F32):
        return nc.dram_tensor(name, list(shape), dt, kind="ExternalInput").ap()

    dbg_kind = "ExternalOutput" if debug else "Internal"

    def dscr(name, shape, dt=F32, dbg=True):
        return nc.dram_tensor(name, list(shape), dt, kind=(dbg_kind if dbg else "Internal")).ap()

    x_d = din("x", [S, D])
    attn_norm_d = din("attn_norm", [L, D])
    ffn_norm_d = din("ffn_norm", [L, D])
    final_norm_d = din("final_norm", [1, D])
    w_na_d = din("w_na", [L, D, 768])
    w_lat_d = din("w_lat", [L, D, 384])
    w_kr_d = din("w_kr", [L, D, 96])
    w_krp_d = din("w_krp", [L, D, 96])
    w_cq_d = din("w_cq", [L, D, 384])
    w_cqp_d = din("w_cqp", [L, D, 384])
    w_ck_d = din("w_ck", [L, D, 128])
    w_ckp_d = din("w_ckp", [L, D, 128])
    w_cv_d = din("w_cv", [L, D, 128])
    rpb_d = din("rpb_tiles", [L, NV, 4, 128, 320])
    qn_d = din("mla_q_norm", [L, 256])
    kvn_d = din("mla_kv_norm", [L, 128])
    w_uq_d = din("w_uq", [L, 256, 576])
    w_uqp_d = din("w_uqp", [L, 256, 576])
    w_uk_d = din("w_uk", [L, 128, 384])
    w_uv_d = din("w_uv", [L, 128, 384])
    sink_d = din("swa_sink", [L, 6])
    gn_d = din("group_norm", [L, D])
    w_out_d = din("w_out", [L, D, D])
    w_router_d = din("w_router", [L, D, NE])
    w_gate_d = din("w_gate", [L, NE, D, D])
    w_up_d = din("w_up", [L, NE, D, D])
    w_down_d = din("w_down", [L, NE, D, D])
    cs_c_d = din("rope_c_cos", [128, S])
    sn_c_d = din("rope_c_sin", [128, S])
    cs_b_d = din("rope_b_cos", [32, S])
    sn_b_d = din("rope_b_sin", [32, S])
    swa_mask_d = din("swa_mask", [6, 128, 512], BF16)
    ident_bf_d = din("ident_bf", [128, 128], BF16)
    ident_f_d = din("ident_f", [128, 128])
    gmat_d = din("gmat", [128, 128])
    rowoff_d = din("rowoff", [128, 1])
    invw_d = din("invw", [128, 3])
    trimat_d = din("trimat", [128, 128])
    dmat_d = din("dmat", [128, ROUNDS * 8 // 128, 512], mybir.dt.int16)

    out_d = nc.dram_tensor("out", [S, D], F32, kind="ExternalOutput").ap()
    h_d = dscr("h_scr", [S, D])
    xn2_d = dscr("xn2_scr", [S, D], BF16)
    oc_d = dscr("oc_scr", [16, 64, S], BF16)
    aff_dbg = dscr("aff_dbg", [128, 512]) if debug else None
    sel_dbg = dscr("sel_dbg", [128, 128]) if debug else None

    s = Sched(nc, es)
    T = lambda name, shape, dt, st=es: st.enter_context(nc.sbuf_tensor("t_" + name, list(shape), dt))

    pp = [es.enter_context(nc.psum_tensor(f"pp{i}", [128, 1024], F32)) for i in range(4)]
    banks = [pp[i // 2][:, (i % 2) * 512:(i % 2 + 1) * 512] for i in range(8)]
    bbuf = [Buf(f"bank{i}") for i in range(8)]

    ident_bf = T("ident_bf", [128, 128], BF16)
    ident_f = T("ident_f", [128, 128], F32)
    ones_bf = T("ones_bf", [128, 128], BF16)
    ones_f = T("ones_f", [128, 128], F32)
    eps_t = T("eps_t", [128, 1], F32)
    cbuf = Buf("consts")
    s.dma("sp", lambda e: e.dma_start(out=ident_bf[:], in_=ident_bf_d), writes=[cbuf])
    s.dma("sp", lambda e: e.dma_start(out=ident_f[:], in_=ident_f_d), writes=[cbuf])
    s.op("dve", lambda e: e.memset(ones_bf[:], 1.0), writes=[cbuf])
    s.op("dve", lambda e: e.memset(ones_f[:], 1.0), writes=[cbuf])
    s.op("dve", lambda e: e.memset(eps_t[:], EPS), writes=[cbuf])
    s.barrier()

    def rstd_big(ss_ap, out_ap, scratch_ap, inv_n, rd, wr):
        s.op("dve", lambda e: e.tensor_scalar(out=out_ap, in0=ss_ap, scalar1=inv_n, scalar2=EPS,
                                              op0=ALU.mult, op1=ALU.add), reads=rd, writes=wr)
        s.op("act", lambda e: e.activation(out=out_ap, in_=out_ap, func=AF.Ln), reads=wr, writes=wr)
        s.op("act", lambda e: e.activation(out=out_ap, in_=out_ap, func=AF.Exp, scale=-0.5), reads=wr, writes=wr)

    def rstd_from_ss(ss_ap, out_ap, inv_n, rd, wr):
        s.op("dve", lambda e: e.tensor_scalar(out=out_ap, in0=ss_ap, scalar1=inv_n, scalar2=EPS,
                                              op0=ALU.mult, op1=ALU.add), reads=rd, writes=wr)
        s.op("act", lambda e: e.activation(out=out_ap, in_=out_ap, func=AF.Sqrt), reads=wr, writes=wr)
        s.op("dve", lambda e: e.reciprocal(out=out_ap, in_=out_ap), reads=wr, writes=wr)

    evac_rr = [0]

    def evac_copy(out_ap, in_ap, rd, wr, scale=None):
        evac_rr[0] ^= 1
        if evac_rr[0]:
            if scale is None:
                s.op("act", lambda e: e.copy(out=out_ap, in_=in_ap), reads=rd, writes=wr)
            else:
                s.op("act", lambda e: e.mul(out=out_ap, in_=in_ap, mul=scale), reads=rd, writes=wr)
        else:
            if scale is None:
                s.op("dve", lambda e: e.tensor_copy(out=out_ap, in_=in_ap), reads=rd, writes=wr)
            else:
                s.op("dve", lambda e: e.tensor_scalar(out=out_ap, in0=in_ap, scalar1=scale, scalar2=None,
                                                      op0=ALU.mult), reads=rd, writes=wr)

    fin_queue = []
    fin_age = [3]

    def flush_fin(force=False):
        for ent in fin_queue:
            ent[0] += 1
        while fin_queue and (force or fin_queue[0][0] > fin_age[0]):
            fin_queue.pop(0)[1]()

    def finalize_heads(O_bank, O_buf, j_oc, col0, st_tiles, extra_den=None, bank_src=None, use_act=False):
        rden, b_rden, bc_bank, b_bc, bc_sb, b_bcsb, o_bf, b_obf = st_tiles
        if use_act:
            if extra_den is not None:
                s.op("act", lambda e: e.activation(out=rden[64:65, 0:512], in_=O_bank[64:65, :], func=AF.Ln, bias=extra_den),
                     reads=[O_buf], writes=[b_rden])
            else:
                s.op("act", lambda e: e.activation(out=rden[64:65, 0:512], in_=O_bank[64:65, :], func=AF.Ln),
                     reads=[O_buf], writes=[b_rden])
            s.op("act", lambda e: e.activation(out=rden[64:65, 0:512], in_=rden[64:65, 0:512], func=AF.Exp, scale=-1.0),
                 reads=[b_rden], writes=[b_rden])
        elif extra_den is not None:
            s.op("dve", lambda e: e.tensor_scalar(out=rden[64:65, 0:512], in0=O_bank[64:65, :], scalar1=extra_den,
                                                  scalar2=None, op0=ALU.add), reads=[O_buf], writes=[b_rden])
            s.op("dve", lambda e: e.reciprocal(out=rden[64:65, 0:512], in_=rden[64:65, 0:512]), reads=[b_rden], writes=[b_rden])
        else:
            s.op("dve", lambda e: e.reciprocal(out=rden[64:65, 0:512], in_=O_bank[64:65, :]), reads=[O_buf], writes=[b_rden])

        def fin_b(bc_bank=bc_bank, b_bc=b_bc):
            if bank_src is not None:
                Spb, b_Spb = bank_src.next()
                bc_bank, b_bc = Spb[:, 0:512], b_Spb[0]
            s.op("pe", lambda e: e.matmul(bc_bank[0:64, :], lhsT=ones_f[64:65, 0:64], rhs=rden[64:65, 0:512],
                                          start=True, stop=True), reads=[b_rden], writes=[b_bc])
            s.op("act" if use_act else "dve", (lambda e: e.copy(out=bc_sb[0:64, :], in_=bc_bank[0:64, :])) if use_act else
                 (lambda e: e.tensor_copy(out=bc_sb[0:64, :], in_=bc_bank[0:64, :])), reads=[b_bc], writes=[b_bcsb])
            s.op("dve", lambda e: e.tensor_tensor(out=o_bf[0:64, :], in0=O_bank[0:64, :], in1=bc_sb[0:64, :], op=ALU.mult),
                 reads=[O_buf, b_bcsb], writes=[b_obf])
            s.dma("sp", lambda e: e.dma_start(out=oc_d[j_oc, :, col0:col0 + 512], in_=o_bf[0:64, :]), reads=[b_obf])
        fin_queue.append([0, fin_b])

    for l in range(nlayers):
        src_d = x_d if l == 0 else h_d
        lat = ExitStack()
        cqn = T(f"cqn{l}", [128, 2, S], BF16, lat)
        ckvn = T(f"ckvn{l}", [128, S], BF16, lat)
        kpe = T(f"kpe{l}", [96, S], BF16, lat)
        lst = ExitStack()
        xnT = T(f"xnT{l}", [128, 8, S], BF16, lst)
        nst = ExitStack()
        rpb_sb = T(f"rpb{l}", [128, NV * 4, 320], BF16, nst)
        b_rpb = Buf()
        for v in range(NV):
            s.dma("pool", lambda e: e.dma_start(out=rpb_sb[:, v * 4:(v + 1) * 4, :], in_=rpb_d[l, v].rearrange("h p c -> p h c")), writes=[b_rpb])
        with ExitStack() as ps:
            g_bc = T(f"g_bc{l}", [128, D], F32, ps)
            b_g = Buf()
            s.dma("sp", lambda e: e.dma_start(out=g_bc[:], in_=attn_norm_d[l:l + 1, :].partition_broadcast(128)), writes=[b_g])
            hts = Rot([(T(f"ht{l}_{i}", [128, D], F32, ps), Buf()) for i in range(4)])
            xbs = Rot([(T(f"xb{l}_{i}", [128, D], BF16, ps), Buf()) for i in range(2)])
            junk = T(f"junk{l}", [128, D], BF16, ps)
            b_junk = Buf()
            sss = Rot([(T(f"ss{l}_{i}", [128, 1], F32, ps), Buf()) for i in range(4)])
            pts = Rot([(banks[i], bbuf[i]) for i in range(2)])
            def p1_a(tb):
                ht, b_ht = hts.next()
                ss, b_ss = sss.next()
                s.dma("sp", lambda e: e.dma_start(out=ht[:], in_=src_d[tb * 128:(tb + 1) * 128, :]), writes=[b_ht])
                s.op("dve", lambda e: e.scalar_tensor_tensor(out=junk[:], in0=ht[:], scalar=1.0, in1=ht[:],
                                                             op0=ALU.mult, op1=ALU.mult, accum_out=ss[:]),
                     reads=[b_ht], writes=[b_junk, b_ss])
                s.op("dve", lambda e: e.tensor_scalar(out=ss[:], in0=ss[:], scalar1=1.0 / D, scalar2=EPS, op0=ALU.mult, op1=ALU.add),
                     reads=[b_ss], writes=[b_ss])
                s.op("act", lambda e: e.activation(out=ss[:], in_=ss[:], func=AF.Sqrt), reads=[b_ss], writes=[b_ss])
                return (tb, ht, b_ht, ss, b_ss)

            def p1_b(ctx):
                tb, ht, b_ht, ss, b_ss = ctx
                xb, b_xb = xbs.next()
                pt, b_pt = pts.next()
                s.op("dve", lambda e: e.reciprocal(out=ss[:], in_=ss[:]), reads=[b_ss], writes=[b_ss])
                s.op("dve", lambda e: e.scalar_tensor_tensor(out=xb[:], in0=ht[:], scalar=ss[:, 0:1], in1=g_bc[:],
                                                             op0=ALU.mult, op1=ALU.mult),
                     reads=[b_ht, b_ss, b_g], writes=[b_xb])
                ptv = pt[:].bitcast(BF16)
                for c in range(8):
                    s.op("pe", lambda e: e.transpose(out=ptv[:, c * 128:(c + 1) * 128], in_=xb[:, c * 128:(c + 1) * 128],
                                                     identity=ident_bf[:]),
                         reads=[b_xb], writes=[b_pt], inc=(c == 7))
                xv = xnT[:, :, tb * 128:(tb + 1) * 128]
                pv = ptv.rearrange("p (c t) -> p c t", c=8)
                s.op("act", lambda e: e.copy(out=xv, in_=pv), reads=[b_pt])

            p1_pend = []
            for tb in range(NTB):
                p1_pend.append(p1_a(tb))
                if len(p1_pend) > 1:
                    p1_b(p1_pend.pop(0))
            while p1_pend:
                p1_b(p1_pend.pop(0))
            s.barrier()

        def proj_fm(w_sb, col0, ncol, nchunk, src, qb, bank, bbank, src_cols=None):
            for c in range(nchunk):
                lw = w_sb[:, c, col0:col0 + ncol]
                rr = src[:, c, qb * 512:(qb + 1) * 512]
                s.op("pe", lambda e: e.matmul(bank[0:ncol, :], lhsT=lw, rhs=rr, start=(c == 0), stop=(c == nchunk - 1)),
                     writes=[bbank], inc=(c == nchunk - 1))

        def load_w_cast(dst, src_ap, wbuf):
            s.dma("pool", lambda e: e.dma_start(out=dst, in_=src_ap), writes=[wbuf])

        with ExitStack() as ps:
            b_w = Buf()
            QT = T(f"QTa{l}", [128, 2, S], BF16, ps)
            KT = T(f"KTa{l}", [128, 4, S], BF16, ps)
            Va = T(f"Va{l}", [128, NTB, 4, 65], BF16, ps)
            s.op("pool", lambda e: e.memset(Va[:, :, :, 64:65], 1.0), writes=[b_w])
            s.op("pool", lambda e: e.memset(KT[:], 0.0), writes=[b_w])
            pw = ExitStack()
            w_sb = T(f"w_na{l}", [128, 8, 768], BF16, pw)
            for c in range(8):
                load_w_cast(w_sb[:, c, :], w_na_d[l, c * 128:(c + 1) * 128, :], b_w)
            s.barrier()
            pbk = Rot([(banks[i], bbuf[i]) for i in range(4)])
            for g in range(4):
                for qb in range(NQB):
                    bank, bb = pbk.next()
                    proj_fm(w_sb, g * 128, 128, 8, xnT, qb, bank, bb)
                    if g < 2:
                        evac_copy(QT[:, g, qb * 512:(qb + 1) * 512], bank[:, :], [bb], [], scale=0.125)
                    else:
                        for hf in range(2):
                            rr = slice(hf * 64, (hf + 1) * 64)
                            evac_copy(KT[rr, (g - 2) * 2 + hf, qb * 512:(qb + 1) * 512], bank[rr, :], [bb], [])
            for tb in range(NTB):
                bank, bb = pbk.next()
                for c in range(8):
                    s.op("pe", lambda e: e.matmul(bank[:, 0:256], lhsT=xnT[:, c, tb * 128:(tb + 1) * 128],
                                                  rhs=w_sb[:, c, 512:768], start=(c == 0), stop=(c == 7)),
                         writes=[bb], inc=(c == 7))
                evac_copy(Va[:, tb, :, 0:64], bank[:, 0:256].rearrange("p (h d) -> p h d", h=4), [bb], [])
            s.barrier()
            pw.close()
            sbk = Rot([(banks[i], bbuf[i]) for i in range(4)])
            obk = Rot([(banks[4 + i], bbuf[4 + i]) for i in range(2)])
            pTs = Rot([(T(f"pTa{l}_{i}", [128, 320], BF16, ps), Buf()) for i in range(4)])
            fin = Rot([(T(f"rdenA{l}_{i}", [65, 512], F32, ps), Buf(), banks[6 + i], bbuf[6 + i],
                        T(f"bcsA{l}_{i}", [64, 512], F32, ps), Buf(),
                        T(f"obfA{l}_{i}", [64, 512], BF16, ps), Buf()) for i in range(2)])
            pend = []

            def na_back(ctx):
                hh_, ri_, r8_, nb_, start_, O_bank, O_buf, pT, b_pT, fin_t = ctx
                flush_fin()
                for b in range(nb_):
                    kb = (start_ * 64 + b * 128) // 128
                    s.op("pe", lambda e: e.matmul(O_bank[0:65, ri_ * 64:(ri_ + 1) * 64],
                                                  lhsT=Va[:, kb, hh_, :], rhs=pT[:, b * 64:(b + 1) * 64],
                                                  start=(b == 0), stop=(b == nb_ - 1)),
                         reads=[b_pT], writes=[O_buf], inc=(b == nb_ - 1))
                if ri_ == 7:
                    finalize_heads(O_bank, O_buf, hh_, r8_ * 512, fin_t, use_act=True)

            for hh in range(4):
                t, pb = hh // 2, (hh % 2) * 64
                for r8 in range(8):
                    O_bank, O_buf = obk.next()
                    fin_t = fin.next()
                    for ri in range(8):
                        r = r8 * 8 + ri
                        rs, start, nb = _na_row_info(r)
                        v = NA_VMAP[r]
                        w = nb * 64
                        S_bank, S_buf = sbk.next()
                        pT, b_pT = pTs.next()
                        s.op("pe", lambda e: e.matmul(S_bank[:, 0:w], lhsT=ident_bf[:], rhs=rpb_sb[:, v * 4 + hh, 0:w],
                                                      start=True, stop=False), writes=[S_buf], inc=False)
                        for b in range(nb):
                            k0 = start * 64 + b * 128
                            s.op("pe", lambda e: e.matmul(S_bank[:, b * 64:(b + 1) * 64],
                                                          lhsT=KT[:, hh, k0:k0 + 128],
                                                          rhs=QT[:, t, r * 64:(r + 1) * 64],
                                                          start=False, stop=(b == nb - 1)),
                                 writes=[S_buf], inc=(b == nb - 1))
                        s.op("act", lambda e: e.activation(out=pT[:, 0:w], in_=S_bank[:, 0:w], func=AF.Exp),
                             reads=[S_buf], writes=[b_pT])
                        pend.append((hh, ri, r8, nb, start, O_bank, O_buf, pT, b_pT, fin_t))
                        if len(pend) > 3:
                            na_back(pend.pop(0))
            while pend:
                na_back(pend.pop(0))
            flush_fin(force=True)
            s.barrier()
        nst.close()
        if stop_after == "na":
            lst.close(); lat.close()
            break

        lwst = ExitStack()
        wl = T(f"wl{l}", [128, 8, 384], BF16, lwst)
        wkr = T(f"wkr{l}", [128, 8, 96], BF16, lwst)
        wkrp = T(f"wkrp{l}", [128, 8, 96], BF16, lwst)
        b_lw = Buf()
        load_w_cast(wl[:], w_lat_d[l].rearrange("(c p) n -> p c n", p=128), b_lw)
        load_w_cast(wkr[:], w_kr_d[l].rearrange("(c p) n -> p c n", p=128), b_lw)
        load_w_cast(wkrp[:], w_krp_d[l].rearrange("(c p) n -> p c n", p=128), b_lw)
        with ExitStack() as ps:
            wq = T(f"wq{l}", [128, 8, 384], BF16, ps)
            wqp = T(f"wqp{l}", [128, 8, 384], BF16, ps)
            wk = T(f"wk{l}", [128, 8, 128], BF16, ps)
            wkp = T(f"wkp{l}", [128, 8, 128], BF16, ps)
            wv = T(f"wv{l}", [128, 8, 128], BF16, ps)
            b_w = Buf()
            for dst, srcw in ((wq, w_cq_d), (wqp, w_cqp_d), (wk, w_ck_d), (wkp, w_ckp_d), (wv, w_cv_d)):
                load_w_cast(dst[:], srcw[l].rearrange("(c p) n -> p c n", p=128), b_w)
            tabs = Rot([(T(f"cosc{l}_{i}", [128, 512], F32, ps), T(f"sinc{l}_{i}", [128, 512], F32, ps), Buf()) for i in range(2)])
            msk = T(f"msk{l}", [128, 6, 512], BF16, ps)
            s.dma("sp", lambda e: e.dma_start(out=msk[:], in_=swa_mask_d.rearrange("j p c -> p j c")), writes=[b_w])
            esink = T(f"esink{l}", [65, 6], F32, ps)
            s.dma("sp", lambda e: e.dma_start(out=esink[64:65, :], in_=sink_d[l:l + 1, :]), writes=[b_w])
            QT = T(f"QTc{l}", [128, 3, S], BF16, ps)
            KT = T(f"KTc{l}", [128, 2, S], BF16, ps)
            s.op("pool", lambda e: e.memset(KT[:], 0.0), writes=[b_w])
            Vc = T(f"Vc{l}", [128, NTB, 2, 65], BF16, ps)
            s.op("pool", lambda e: e.memset(Vc[:, :, :, 64:65], 1.0), writes=[b_w])
            s.barrier()
            s.op("act", lambda e: e.activation(out=esink[64:65, :], in_=esink[64:65, :], func=AF.Exp))
            pbk = Rot([(banks[2 * i], bbuf[2 * i], banks[2 * i + 1], bbuf[2 * i + 1]) for i in range(4)])
            pj = ExitStack()
            t1s = Rot([(T(f"t1c{l}_{i}", [128, 512], F32, pj), Buf()) for i in range(2)])
            t2s = Rot([(T(f"t2c{l}_{i}", [128, 512], F32, pj), Buf()) for i in range(2)])
            for g in range(4):
                for qb in range(NQB):
                    bq, bbq, bp, bbp = pbk.next()
                    if g < 3:
                        proj_fm(wq, g * 128, 128, 8, xnT, qb, bq, bbq)
                        proj_fm(wqp, g * 128, 128, 8, xnT, qb, bp, bbp)
                        dst = QT[:, g, qb * 512:(qb + 1) * 512]
                    else:
                        proj_fm(wk, 0, 128, 8, xnT, qb, bq, bbq)
                        proj_fm(wkp, 0, 128, 8, xnT, qb, bp, bbp)
                        dst = None
                    t1, b_t1 = t1s.next()
                    t2, b_t2 = t2s.next()
                    ctab, stab, b_tab = tabs.next()
                    s.dma("sp", lambda e: e.dma_start(out=ctab[:], in_=cs_c_d[:, qb * 512:(qb + 1) * 512]), writes=[b_tab])
                    s.dma("sp", lambda e: e.dma_start(out=stab[:], in_=sn_c_d[:, qb * 512:(qb + 1) * 512]), writes=[b_tab])
                    s.op("dve", lambda e: e.tensor_tensor(out=t1[:], in0=bq[:, :], in1=ctab[:], op=ALU.mult), reads=[bbq, b_tab], writes=[b_t1])
                    s.op("dve", lambda e: e.tensor_tensor(out=t2[:], in0=bp[:, :], in1=stab[:], op=ALU.mult), reads=[bbp, b_tab], writes=[b_t2])
                    if g < 3:
                        s.op("pool", lambda e: e.tensor_tensor(out=t1[:], in0=t1[:], in1=t2[:], op=ALU.add), reads=[b_t1, b_t2], writes=[b_t1])
                        s.op("act", lambda e: e.mul(out=dst, in_=t1[:], mul=0.125), reads=[b_t1])
                    else:
                        for kv_ in range(2):
                            rr = slice(kv_ * 64, (kv_ + 1) * 64)
                            s.op("pool", lambda e: e.tensor_tensor(out=KT[rr, kv_, qb * 512:(qb + 1) * 512], in0=t1[rr, :], in1=t2[rr, :], op=ALU.add),
                                 reads=[b_t1, b_t2])
            pb2 = Rot([(banks[i], bbuf[i]) for i in range(4)])
            for tb in range(NTB):
                bank, bb = pb2.next()
                for c in range(8):
                    s.op("pe", lambda e: e.matmul(bank[:, 0:128], lhsT=xnT[:, c, tb * 128:(tb + 1) * 128],
                                                  rhs=wv[:, c, :], start=(c == 0), stop=(c == 7)),
                         writes=[bb], inc=(c == 7))
                evac_copy(Vc[:, tb, :, 0:64], bank[:, 0:128].rearrange("p (h d) -> p h d", h=2), [bb], [])
            s.barrier()
            pj.close()
            spairs = Rot([(pp[0], [bbuf[0], bbuf[1]]), (pp[1], [bbuf[2], bbuf[3]]), (pp[2], [bbuf[4], bbuf[5]])])
            obk = Rot([(banks[6 + i], bbuf[6 + i]) for i in range(2)])
            pTs = Rot([(T(f"pTc{l}_{i}", [128, 1024], BF16, ps), Buf()) for i in range(3)])
            fin = Rot([(T(f"rdenC{l}_{i}", [65, 512], F32, ps), Buf(),
                        T(f"bcsC{l}_{i}", [64, 512], F32, ps), Buf(),
                        T(f"obfC{l}_{i}", [64, 512], BF16, ps), Buf()) for i in range(2)])
            pend = []
            fin_age[0] = 1

            def swa_back(ctx):
                hh_, qb_, grp, first, last, O_bank, O_buf, pT, b_pT, fin_t = ctx
                flush_fin()
                kvh_ = hh_ // 3
                for u, kb in enumerate(grp):
                    s.op("pe", lambda e: e.matmul(O_bank[0:65, :], lhsT=Vc[:, kb, kvh_, :], rhs=pT[:, u * 512:(u + 1) * 512],
                                                  start=(first and u == 0), stop=(last and u == len(grp) - 1)),
                         reads=[b_pT], writes=[O_buf], inc=(u == len(grp) - 1))
                if last:
                    f_ = fin_t
                    finalize_heads(O_bank, O_buf, 10 + hh_, qb_ * 512, (f_[0], f_[1], None, None, f_[2], f_[3], f_[4], f_[5]),
                                   extra_den=esink[64:65, hh_:hh_ + 1], bank_src=spairs, use_act=True)

            for hh in range(6):
                kvh = hh // 3
                t = hh % 3
                pb = kvh * 64
                for qb in range(NQB):
                    O_bank, O_buf = obk.next()
                    fin_t = fin.next()
                    kbs = [kb for kb in range(4 * qb - 1, 4 * qb + 5) if 0 <= kb < NTB]
                    grps = [kbs[i:i + 2] for i in range(0, len(kbs), 2)]
                    for gi, grp in enumerate(grps):
                        Sp, b_Sp = spairs.next()
                        pT, b_pT = pTs.next()
                        for u, kb in enumerate(grp):
                            j = kb - (4 * qb - 1)
                            s.op("pe", lambda e: e.matmul(Sp[:, u * 512:(u + 1) * 512], lhsT=KT[:, kvh, kb * 128:(kb + 1) * 128],
                                                          rhs=QT[:, t, qb * 512:(qb + 1) * 512],
                                                          start=True, stop=True), writes=b_Sp, inc=(u == len(grp) - 1))
                        wdt = len(grp) * 512
                        j0 = grp[0] - (4 * qb - 1)
                        s.op("act", lambda e: e.activation(out=pT[:, 0:wdt], in_=Sp[:, 0:wdt], func=AF.Exp),
                             reads=b_Sp, writes=[b_pT])
                        s.op("dve", lambda e: e.tensor_tensor(out=pT[:, 0:wdt], in0=pT[:, 0:wdt],
                                                              in1=msk[:, j0:j0 + len(grp), :].rearrange("p j c -> p (j c)"), op=ALU.mult),
                             reads=[b_pT], writes=[b_pT])
                        pend.append((hh, qb, grp, gi == 0, gi == len(grps) - 1, O_bank, O_buf, pT, b_pT, fin_t))
                        if len(pend) > 2:
                            swa_back(pend.pop(0))
            while pend:
                swa_back(pend.pop(0))
            flush_fin(force=True)
            fin_age[0] = 3
            s.barrier()
        if stop_after == "swa":
            lwst.close(); lst.close(); lat.close()
            break

        with ExitStack() as ps:
            cosB = T(f"cosb{l}", [96, S], F32, ps)
            sinB = T(f"sinb{l}", [96, S], F32, ps)
            qn_t = T(f"qn{l}", [128, 2], F32, ps)
            kvn_t = T(f"kvn{l}", [128, 1], F32, ps)
            b_w = Buf()
            s.dma("sp", lambda e: e.dma_start(out=qn_t[:], in_=qn_d[l].rearrange("(c p) -> p c", p=128), allow_slow_non_contiguous=True), writes=[b_w])
            s.dma("sp", lambda e: e.dma_start(out=kvn_t[:], in_=kvn_d[l].rearrange("(c p) -> p c", p=128), allow_slow_non_contiguous=True), writes=[b_w])
            s.dma("sp", lambda e: e.dma_start(out=cosB[64:96, :], in_=cs_b_d), writes=[b_w])
            s.dma("sp", lambda e: e.dma_start(out=sinB[64:96, :], in_=sn_b_d), writes=[b_w])
            s.barrier()
            rb = Rot([(banks[i], bbuf[i]) for i in range(3)])
            sqs = Rot([(T(f"sq{l}_{i}", [128, 512], BF16, ps), Buf()) for i in range(3)])
            rsb = Rot([(T(f"rsb{l}_{i}", [128, 512], F32, ps), Buf()) for i in range(2)])
            rscr = T(f"rscr{l}", [128, 512], F32, ps)
            ssb = Rot([(banks[3 + i], bbuf[3 + i]) for i in range(2)])
            for qb in range(NQB):
                cols = slice(qb * 512, (qb + 1) * 512)
                raws = []
                for c2 in range(2):
                    bank, bb = rb.next()
                    proj_fm(wl, c2 * 128, 128, 8, xnT, qb, bank, bb)
                    sq, b_sq = sqs.next()
                    s.op("act", lambda e: e.activation(out=sq[:], in_=bank[:, :], func=AF.Square), reads=[bb], writes=[b_sq])
                    raws.append((bank, bb, sq, b_sq))
                ss_bank, ss_buf = ssb.next()
                for c2 in range(2):
                    s.op("pe", lambda e: e.matmul(ss_bank[:, :], lhsT=ones_bf[:], rhs=raws[c2][2][:], start=(c2 == 0), stop=(c2 == 1)),
                         reads=[raws[c2][3]], writes=[ss_buf], inc=(c2 == 1))
                rs_t, b_rs = rsb.next()
                rstd_big(ss_bank[:, :], rs_t[:], rscr[:], 1.0 / 256, [ss_buf], [b_rs])
                for c2 in range(2):
                    bank, bb = raws[c2][0], raws[c2][1]
                    s.op("dve", lambda e: e.scalar_tensor_tensor(out=cqn[:, c2, cols], in0=bank[:, :], scalar=qn_t[:, c2:c2 + 1],
                                                                 in1=rs_t[:], op0=ALU.mult, op1=ALU.mult), reads=[bb, b_rs])
                bank, bb = rb.next()
                proj_fm(wl, 256, 128, 8, xnT, qb, bank, bb)
                sq, b_sq = sqs.next()
                s.op("act", lambda e: e.activation(out=sq[:], in_=bank[:, :], func=AF.Square), reads=[bb], writes=[b_sq])
                ss_bank, ss_buf = ssb.next()
                s.op("pe", lambda e: e.matmul(ss_bank[:, :], lhsT=ones_bf[:], rhs=sq[:], start=True, stop=True), reads=[b_sq], writes=[ss_buf])
                rs_t, b_rs = rsb.next()
                rstd_big(ss_bank[:, :], rs_t[:], rscr[:], 1.0 / 128, [ss_buf], [b_rs])
                s.op("dve", lambda e: e.scalar_tensor_tensor(out=ckvn[:, cols], in0=bank[:, :], scalar=kvn_t[:, 0:1],
                                                             in1=rs_t[:], op0=ALU.mult, op1=ALU.mult), reads=[bb, b_rs])
                bank, bb = rb.next()
                proj_fm(wkr, 0, 96, 8, xnT, qb, bank, bb)
                bank2, bb2 = rb.next()
                proj_fm(wkrp, 0, 96, 8, xnT, qb, bank2, bb2)
                t1, b_t1 = rsb.next()
                t2, b_t2 = rsb.next()
                s.op("dve", lambda e: e.tensor_tensor(out=t1[64:96, :], in0=bank[64:96, :], in1=cosB[64:96, cols], op=ALU.mult), reads=[bb], writes=[b_t1])
                s.op("dve", lambda e: e.tensor_tensor(out=t2[64:96, :], in0=bank2[64:96, :], in1=sinB[64:96, cols], op=ALU.mult), reads=[bb2], writes=[b_t2])
                s.op("pool", lambda e: e.tensor_tensor(out=kpe[64:96, cols], in0=t1[64:96, :], in1=t2[64:96, :], op=ALU.add), reads=[b_t1, b_t2])
            s.barrier()
        lwst.close()
        lst.close()
        with ExitStack() as ps:
            Vb = T(f"Vb{l}", [128, NTB, 6, 65], BF16, ps)
            cosB = T(f"cosb2{l}", [96, S], F32, ps)
            sinB = T(f"sinb2{l}", [96, S], F32, ps)
            wuq = T(f"wuq{l}", [128, 2, 576], BF16, ps)
            wuqp = T(f"wuqp{l}", [128, 2, 576], BF16, ps)
            wuk = T(f"wuk{l}", [128, 384], BF16, ps)
            wuv = T(f"wuv{l}", [128, 384], BF16, ps)
            b_w = Buf()
            load_w_cast(wuq[:], w_uq_d[l].rearrange("(c p) n -> p c n", p=128), b_w)
            load_w_cast(wuqp[:], w_uqp_d[l].rearrange("(c p) n -> p c n", p=128), b_w)
            load_w_cast(wuk[:], w_uk_d[l], b_w)
            load_w_cast(wuv[:], w_uv_d[l], b_w)
            s.dma("sp", lambda e: e.dma_start(out=cosB[64:96, :], in_=cs_b_d), writes=[b_w])
            s.dma("sp", lambda e: e.dma_start(out=sinB[64:96, :], in_=sn_b_d), writes=[b_w])
            s.op("pool", lambda e: e.memset(Vb[:, :, :, 64:65], 1.0), writes=[b_w])
            s.barrier()
            pb2 = Rot([(banks[i], bbuf[i]) for i in range(4)])
            for tb in range(NTB):
                bank, bb = pb2.next()
                s.op("pe", lambda e: e.matmul(bank[:, 0:384], lhsT=ckvn[:, tb * 128:(tb + 1) * 128], rhs=wuv[:, :], start=True, stop=True), writes=[bb])
                evac_copy(Vb[:, tb, :, 0:64], bank[:, 0:384].rearrange("p (h d) -> p h d", h=6), [bb], [])
            s.barrier()
            scale_b = float(96 ** -0.5)
            QTs = [(T(f"QTb{l}_{i}", [96, S], BF16, ps), Buf()) for i in range(2)]
            KTs = [(T(f"KTb{l}_{i}", [96, S], BF16, ps), Buf()) for i in range(2)]
            t1s = Rot([(T(f"t1b{l}_{i}", [96, 512], F32, ps), Buf()) for i in range(2)])
            t2s = Rot([(T(f"t2b{l}_{i}", [96, 512], F32, ps), Buf()) for i in range(2)])
            pbk = Rot([(banks[i], bbuf[i]) for i in range(3)])
            spairs = Rot([(pp[0], [bbuf[0], bbuf[1]]), (pp[1], [bbuf[2], bbuf[3]]), (pp[2], [bbuf[4], bbuf[5]])])
            obk = Rot([(banks[6 + i], bbuf[6 + i]) for i in range(2)])
            pTs = Rot([(T(f"pTb{l}_{i}", [128, 1024], BF16, ps), Buf()) for i in range(4)])
            finB = Rot([(T(f"rdenB{l}_{i}", [65, 512], F32, ps), Buf(),
                         T(f"bcsB{l}_{i}", [64, 512], F32, ps), Buf(),
                         T(f"obfB{l}_{i}", [64, 512], BF16, ps), Buf()) for i in range(2)])
            pend = []

            def mla_back(ctx):
                hh_, qb_, kp_, O_bank, O_buf, pT, b_pT, fin_t = ctx
                flush_fin()
                for u in range(2):
                    kb = 2 * kp_ + u
                    s.op("pe", lambda e: e.matmul(O_bank[0:65, :], lhsT=Vb[:, kb, hh_, :], rhs=pT[:, u * 512:(u + 1) * 512],
                                                  start=(kb == 0), stop=(kb == NTB - 1)),
                         reads=[b_pT], writes=[O_buf], inc=(u == 1))
                if kp_ == NTB // 2 - 1:
                    f_ = fin_t
                    finalize_heads(O_bank, O_buf, 4 + hh_, qb_ * 512, (f_[0], f_[1], None, None, f_[2], f_[3], f_[4], f_[5]), bank_src=spairs)

            def proj_step(hp, qb):
                QTh, b_Q = QTs[hp % 2]
                KTh, b_K = KTs[hp % 2]
                cols = slice(qb * 512, (qb + 1) * 512)
                Sp1, bS1 = spairs.next()
                bq, bbq = Sp1[:, 0:512], bS1[0]
                bp, bbp = Sp1[:, 512:1024], bS1[1]
                proj_fm(wuq, hp * 96, 96, 2, cqn, qb, bq, bbq)
                proj_fm(wuqp, hp * 96, 96, 2, cqn, qb, bp, bbp)
                s.op("dve", lambda e: e.tensor_copy(out=QTh[0:64, cols], in_=bq[0:64, :]), reads=[bbq], writes=[b_Q])
                t1, b_t1 = t1s.next()
                t2, b_t2 = t2s.next()
                s.op("dve", lambda e: e.tensor_tensor(out=t1[64:96, :], in0=bq[64:96, :], in1=cosB[64:96, cols], op=ALU.mult), reads=[bbq], writes=[b_t1])
                s.op("dve", lambda e: e.tensor_tensor(out=t2[64:96, :], in0=bp[64:96, :], in1=sinB[64:96, cols], op=ALU.mult), reads=[bbp], writes=[b_t2])
                s.op("pool", lambda e: e.tensor_tensor(out=QTh[64:96, cols], in0=t1[64:96, :], in1=t2[64:96, :], op=ALU.add),
                     reads=[b_t1, b_t2], writes=[b_Q])
                Sp2, bS2 = spairs.next()
                bk, bbk = Sp2[:, 0:512], bS2[0]
                s.op("pe", lambda e: e.matmul(bk[0:64, :], lhsT=wuk[:, hp * 64:(hp + 1) * 64], rhs=ckvn[:, cols], start=True, stop=True), writes=[bbk])
                s.op("dve", lambda e: e.tensor_copy(out=KTh[0:64, cols], in_=bk[0:64, :]), reads=[bbk], writes=[b_K])
                s.op("pool", lambda e: e.tensor_copy(out=KTh[64:96, cols], in_=kpe[64:96, cols]), writes=[b_K])

            for qb in range(NQB):
                proj_step(0, qb)
            for hh in range(6):
                QTh, b_Q = QTs[hh % 2]
                KTh, b_K = KTs[hh % 2]
                for qb in range(NQB):
                    if hh + 1 < 6:
                        proj_step(hh + 1, qb)
                    O_bank, O_buf = obk.next()
                    fin_t = finB.next()
                    for kp in range(NTB // 2):
                        Sp, b_Sp = spairs.next()
                        pT, b_pT = pTs.next()
                        for u in range(2):
                            kb = 2 * kp + u
                            s.op("pe", lambda e: e.matmul(Sp[:, u * 512:(u + 1) * 512], lhsT=KTh[0:96, kb * 128:(kb + 1) * 128],
                                                          rhs=QTh[0:96, qb * 512:(qb + 1) * 512], start=True, stop=True),
                                 reads=[b_Q, b_K], writes=b_Sp, inc=(u == 1))
                        s.op("act", lambda e: e.activation(out=pT[:], in_=Sp[:, :], func=AF.Exp, scale=scale_b),
                             reads=b_Sp, writes=[b_pT])
                        pend.append((hh, qb, kp, O_bank, O_buf, pT, b_pT, fin_t))
                        if len(pend) > 2:
                            mla_back(pend.pop(0))
            while pend:
                mla_back(pend.pop(0))
            flush_fin(force=True)
            s.barrier()
        lat.close()
        if stop_after == "mla":
            break

        sel = ExitStack()
        aff_all = T(f"aff{l}", [128, NTB, NE], F32, sel)
        affT = T(f"affT{l}", [128, 512], F32, sel)
        idsT = T(f"idsT{l}", [128, 64], U32, sel)
        gT = T(f"gT{l}", [128, 64], F32, sel)
        with ExitStack() as ps:
            Wo = T(f"Wo{l}", [128, 8, D], BF16, ps)
            gn_t = T(f"gn{l}", [128, 8], F32, ps)
            g2_bc = T(f"g2{l}", [128, D], F32, ps)
            wr_sb = T(f"wr{l}", [128, 8, NE], F32, ps)
            invw = T(f"invw{l}", [128, 3], F32, ps)
            b_w = Buf()
            s.dma("sp", lambda e: e.dma_start(out=gn_t[:], in_=gn_d[l].rearrange("(j p) -> p j", p=128), allow_slow_non_contiguous=True), writes=[b_w])
            s.dma("sp", lambda e: e.dma_start(out=g2_bc[:], in_=ffn_norm_d[l:l + 1, :].partition_broadcast(128)), writes=[b_w])
            s.dma("sp", lambda e: e.dma_start(out=wr_sb[:], in_=w_router_d[l].rearrange("(c p) n -> p c n", p=128)), writes=[b_w])
            s.dma("sp", lambda e: e.dma_start(out=invw[:], in_=invw_d), writes=[b_w])
            stg = Rot([(T(f"wstg{l}_{i}", [128, D], F32, ps), Buf()) for i in range(4)])
            for j in range(8):
                st_, b_st = stg.next()
                s.dma("sp", lambda e: e.dma_start(out=st_[:], in_=w_out_d[l, j * 128:(j + 1) * 128, :]), writes=[b_st])
                s.op("dve", lambda e: e.tensor_scalar(out=Wo[:, j, :], in0=st_[:], scalar1=gn_t[:, j:j + 1], scalar2=None, op0=ALU.mult),
                     reads=[b_st, b_w])
            Zs = [(T(f"Z{l}_{i}", [128, 128], F32, ps), Buf()) for i in range(8)]
            for z, bz in Zs:
                s.op("pool", lambda e: e.memset(z[:], 0.0), writes=[bz])
            s.barrier()
            ocs = Rot([(T(f"oc{l}_{i}", [128, 8, 512], BF16, ps), Buf()) for i in range(2)])
            osq = T(f"osq{l}", [128, 8, 512], BF16, ps)
            b_osq = Buf()
            hts = Rot([(T(f"h3_{l}_{i}", [128, D], F32, ps), Buf()) for i in range(3)])
            h1s = Rot([(T(f"h1_{l}_{i}", [128, D], F32, ps), Buf()) for i in range(4)])
            xfs = Rot([(T(f"xf_{l}_{i}", [128, D], F32, ps), Buf()) for i in range(3)])
            xbs = Rot([(T(f"xb3_{l}_{i}", [128, D], BF16, ps), Buf()) for i in range(2)])
            xTs = Rot([(T(f"xT3_{l}_{i}", [128, 8, 128], F32, ps), Buf()) for i in range(2)])
            junk = T(f"junk3_{l}", [128, D], BF16, ps)
            b_junk = Buf()
            smalls = Rot([(T(f"sm{l}_{i}", [128, 8], F32, ps), Buf()) for i in range(6)])
            lgs = Rot([(T(f"lg{l}_{i}", [128, NE], F32, ps), Buf()) for i in range(2)])
            mix_heads = [[0, 1], [2, 3, 4], [5, 6, 7]]
            obk = Rot([(banks[0], bbuf[0], banks[1], bbuf[1]), (banks[2], bbuf[2], banks[3], bbuf[3])])
            ssb = Rot([(banks[4], bbuf[4])])
            tpk = Rot([(banks[5], bbuf[5], banks[6], bbuf[6])])
            lgb = Rot([(banks[7], bbuf[7])])
            p3_xf = {}

            def p3_stage_b1(tb, h1, b_h1, sm, b_sm):
                s.op("dve", lambda e: e.scalar_tensor_tensor(out=junk[:], in0=h1[:], scalar=1.0, in1=h1[:],
                                                             op0=ALU.mult, op1=ALU.mult, accum_out=sm[:, 3:4]),
                     reads=[b_h1], writes=[b_junk, b_sm])
                s.op("dve", lambda e: e.tensor_scalar(out=sm[:, 3:4], in0=sm[:, 3:4], scalar1=1.0 / D, scalar2=EPS, op0=ALU.mult, op1=ALU.add),
                     reads=[b_sm], writes=[b_sm])
                s.op("act", lambda e: e.activation(out=sm[:, 3:4], in_=sm[:, 3:4], func=AF.Ln), reads=[b_sm], writes=[b_sm])
                s.op("act", lambda e: e.activation(out=sm[:, 3:4], in_=sm[:, 3:4], func=AF.Exp, scale=-0.5), reads=[b_sm], writes=[b_sm])
                xf, b_xf = xfs.next()
                xb, b_xb = xbs.next()
                s.op("dve", lambda e: e.scalar_tensor_tensor(out=xf[:], in0=h1[:], scalar=sm[:, 3:4], in1=g2_bc[:],
                                                             op0=ALU.mult, op1=ALU.mult), reads=[b_h1, b_sm], writes=[b_xf])
                s.op("act", lambda e: e.copy(out=xb[:], in_=xf[:]), reads=[b_xf], writes=[b_xb])
                s.dma("pool", lambda e: e.dma_start(out=xn2_d[tb * 128:(tb + 1) * 128, :], in_=xb[:]), reads=[b_xb])
                p3_xf[tb] = (xf, b_xf)

            def p3_stage_b2(tb, h1, b_h1, sm, b_sm):
                xf, b_xf = p3_xf.pop(tb)
                t0, bt0, t1_, bt1 = tpk.next()
                for c in range(8):
                    bk_, bbk_ = (t0, bt0) if c < 4 else (t1_, bt1)
                    s.op("pe", lambda e: e.transpose(out=bk_[:, (c % 4) * 128:(c % 4 + 1) * 128], in_=xf[:, c * 128:(c + 1) * 128], identity=ident_f[:]),
                         reads=[b_xf], writes=[bbk_], inc=(c % 4 == 3))
                xT, b_xT = xTs.next()
                s.op("act", lambda e: e.copy(out=xT[:, 0:4, :], in_=t0[:, :].rearrange("p (c t) -> p c t", c=4)), reads=[bt0], writes=[b_xT])
                s.op("act", lambda e: e.copy(out=xT[:, 4:8, :], in_=t1_[:, :].rearrange("p (c t) -> p c t", c=4)), reads=[bt1], writes=[b_xT])
                lb, blb = lgb.next()
                for c in range(8):
                    s.op("pe", lambda e: e.matmul(lb[:, 0:NE], lhsT=xT[:, c, :], rhs=wr_sb[:, c, :], start=(c == 0), stop=(c == 7)),
                         reads=[b_xT], writes=[blb], inc=(c == 7))
                lg, b_lg = lgs.next()
                s.op("dve", lambda e: e.reduce_max(out=sm[:, 4:5], in_=lb[:, 0:NE], axis=mybir.AxisListType.X), reads=[blb], writes=[b_sm])
                s.op("dve", lambda e: e.tensor_scalar(out=sm[:, 4:5], in0=sm[:, 4:5], scalar1=-1.0, scalar2=None, op0=ALU.mult), reads=[b_sm], writes=[b_sm])
                s.op("act", lambda e: e.activation(out=lg[:], in_=lb[:, 0:NE], func=AF.Exp, bias=sm[:, 4:5], accum_out=sm[:, 5:6]),
                     reads=[blb, b_sm], writes=[b_lg, b_sm])
                s.op("dve", lambda e: e.reciprocal(out=sm[:, 5:6], in_=sm[:, 5:6]), reads=[b_sm], writes=[b_sm])
                s.op("dve", lambda e: e.tensor_scalar(out=aff_all[:, tb, :], in0=lg[:], scalar1=sm[:, 5:6], scalar2=None, op0=ALU.mult),
                     reads=[b_lg, b_sm])

            p3_pend = []
            for qb in range(NQB):
                oc, b_oc = ocs.next()
                ocv = oc_d.rearrange("(jj two) p t -> two p jj t", two=2)
                for two in range(2):
                    s.dma("sp", lambda e: e.dma_start(out=oc[two * 64:(two + 1) * 64, :, :], in_=ocv[two][:, :, qb * 512:(qb + 1) * 512]), writes=[b_oc])
                s.op("pool", lambda e: e.tensor_tensor(out=osq[:], in0=oc[:], in1=oc[:], op=ALU.mult), reads=[b_oc], writes=[b_osq])
                for sub in range(4):
                    tb = qb * 4 + sub
                    tc_ = slice(sub * 128, (sub + 1) * 128)
                    ht, b_ht = hts.next()
                    s.dma("sp", lambda e: e.dma_start(out=ht[:], in_=src_d[tb * 128:(tb + 1) * 128, :]), writes=[b_ht])
                    ss_bank, ss_buf = ssb.next()
                    for m in range(3):
                        hs = mix_heads[m]
                        for i, j in enumerate(hs):
                            s.op("pe", lambda e: e.matmul(ss_bank[:, m:m + 1], lhsT=osq[:, j, tc_], rhs=ones_bf[:, 0:1],
                                                          start=(i == 0), stop=(i == len(hs) - 1)),
                                 reads=[b_osq], writes=[ss_buf], inc=(i == len(hs) - 1))
                    sm, b_sm = smalls.next()
                    s.op("dve", lambda e: e.tensor_tensor(out=sm[:, 0:3], in0=ss_bank[:, 0:3], in1=invw[:], op=ALU.mult), reads=[ss_buf], writes=[b_sm])
                    s.op("dve", lambda e: e.tensor_scalar(out=sm[:, 0:3], in0=sm[:, 0:3], scalar1=EPS, scalar2=None, op0=ALU.add), reads=[b_sm], writes=[b_sm])
                    s.op("act", lambda e: e.activation(out=sm[:, 0:3], in_=sm[:, 0:3], func=AF.Ln), reads=[b_sm], writes=[b_sm])
                    s.op("act", lambda e: e.activation(out=sm[:, 0:3], in_=sm[:, 0:3], func=AF.Exp, scale=-0.5), reads=[b_sm], writes=[b_sm])
                    if p3_pend:
                        p3_stage_b1(*p3_pend[0])
                    h1, b_h1 = h1s.next()
                    prev, b_prev = ht, b_ht
                    for m in range(3):
                        hs = mix_heads[m]
                        b0, bb0, b1, bb1 = obk.next()
                        for half, (bk_, bbk_) in enumerate(((b0, bb0), (b1, bb1))):
                            for i, j in enumerate(hs):
                                s.op("pe", lambda e: e.matmul(bk_[:, :], lhsT=oc[:, j, tc_], rhs=Wo[:, j, half * 512:(half + 1) * 512],
                                                              start=(i == 0), stop=(i == len(hs) - 1)),
                                     reads=[b_oc], writes=[bbk_], inc=(i == len(hs) - 1))
                            hc = slice(half * 512, (half + 1) * 512)
                            s.op("dve", lambda e: e.scalar_tensor_tensor(out=h1[:, hc], in0=bk_[:, :], scalar=sm[:, m:m + 1], in1=prev[:, hc],
                                                                         op0=ALU.mult, op1=ALU.add),
                                 reads=[bbk_, b_sm, b_prev], writes=[b_h1])
                        prev, b_prev = h1, b_h1
                    s.dma("pool", lambda e: e.dma_start(out=h_d[tb * 128:(tb + 1) * 128, :], in_=h1[:]), reads=[b_h1])
                    if p3_pend:
                        p3_stage_b2(*p3_pend.pop(0))
                    p3_pend.append((tb, h1, b_h1, sm, b_sm))
            while p3_pend:
                p3_stage_b1(*p3_pend[0])
                p3_stage_b2(*p3_pend.pop(0))
            s.barrier()
            for jc in range(4):
                for part in range(8):
                    tb = part * 4 + jc
                    z, bz = Zs[part]
                    zv = z[:].rearrange("p (e q) -> p e q", q=8)[:, :, part:part + 1]
                    s.op("dve", lambda e: e.tensor_copy(out=zv, in_=aff_all[:, tb, :].rearrange("p (e o) -> p e o", o=1)), writes=[bz])
                    s.op("pe", lambda e: e.matmul(banks[0][:, jc * 128:(jc + 1) * 128], lhsT=z[:], rhs=ident_f[:],
                                                  start=(part == 0), stop=(part == 7)), reads=[bz], writes=[bbuf[0]])
            s.op("act", lambda e: e.copy(out=affT[:], in_=banks[0][:, :]), reads=[bbuf[0]])
            s.barrier()
        if stop_after == "p3":
            sel.close()
            break

        wst = ExitStack()
        Ws = [(T(f"Wg{l}_{i}", [128, 8, D], BF16, wst), T(f"Wu{l}_{i}", [128, 8, D], BF16, wst),
               T(f"Wd{l}_{i}", [128, 8, D], BF16, wst), Buf()) for i in range(2)]
        stg = Rot([(T(f"wst{l}_{i}", [128, D], F32, wst), Buf()) for i in range(8)])

        def load_expert_steps(e_):
            Wg, Wu, Wd, b_W = Ws[e_ % 2]
            steps = []
            for dst, srcw in ((Wg, w_gate_d), (Wu, w_up_d), (Wd, w_down_d)):
                for c in range(8):
                    def step(dst=dst, srcw=srcw, c=c):
                        st_, b_st = stg.next()
                        s.dma("sp", lambda e: e.dma_start(out=st_[:], in_=srcw[l, e_, c * 128:(c + 1) * 128, :]), writes=[b_st])
                        s.op("act", lambda e: e.copy(out=dst[:, c, :], in_=st_[:]), reads=[b_st], writes=[b_W])
                    steps.append(step)
            return steps

        def load_expert(e_):
            for st in load_expert_steps(e_):
                st()

        load_expert(0)
        with ExitStack() as ps:
            gmat = T(f"gmat{l}", [128, 128], F32, ps)
            rowoff = T(f"rowoff{l}", [128, 1], F32, ps)
            b_c = Buf()
            s.dma("sp", lambda e: e.dma_start(out=gmat[:], in_=gmat_d), writes=[b_c])
            s.dma("sp", lambda e: e.dma_start(out=rowoff[:], in_=rowoff_d), writes=[b_c])
            mid = T(f"mid{l}", [128, 1], F32, ps)
            lo = T(f"lo{l}", [128, 1], F32, ps)
            cnt = T(f"cnt{l}", [128, 1], F32, ps)
            gef = T(f"gef{l}", [128, 1], F32, ps)
            tt = T(f"tt{l}", [128, 1], F32, ps)
            cmpj = T(f"cmpj{l}", [128, 512], F32, ps)
            am = T(f"am{l}", [128, 512], F32, ps)
            vals = T(f"vals{l}", [128, ROUNDS * 8], F32, ps)
            idxs = T(f"idxs{l}", [128, ROUNDS * 8], mybir.dt.uint16, ps)
            idf = T(f"idf{l}", [128, ROUNDS * 8], F32, ps)
            vmask = T(f"vmask{l}", [128, ROUNDS * 8], F32, ps)
            nrow = T(f"nrow{l}", [128, 1], F32, ps)
            off_sb = T(f"off{l}", [128, 1], F32, ps)
            diag = T(f"diag{l}", [128, 128], F32, ps)
            offB = T(f"offB{l}", [128, 128], F32, ps)
            RT = T(f"RT{l}", [128, ROUNDS * 8 // 128, 128, 4], BF16, ps)
            trimat = T(f"trimat{l}", [128, 128], F32, ps)
            dmat = T(f"dmat{l}", [128, ROUNDS * 8 // 128, 512], mybir.dt.int16, ps)
            s.dma("sp", lambda e: e.dma_start(out=trimat[:], in_=trimat_d), writes=[b_c])
            s.dma("sp", lambda e: e.dma_start(out=dmat[:], in_=dmat_d), writes=[b_c])
            b_s = Buf()
            s.op("dve", lambda e: e.memset(mid[:], 0.5), writes=[b_s])
            s.op("dve", lambda e: e.memset(lo[:], 0.0), writes=[b_s])
            s.barrier()
            step = 0.5
            cb = banks[1]
            b_cb = bbuf[1]
            for it in range(NBIS):
                s.op("dve", lambda e: e.tensor_scalar(out=cmpj[:], in0=affT[:], scalar1=mid[:, 0:1], scalar2=None,
                                                      op0=ALU.is_ge, op1=ALU.add, accum_out=cnt[:]), reads=[b_s], writes=[b_s])
                s.op("pe", lambda e: e.matmul(cb[:, 0:1], lhsT=gmat[:], rhs=cnt[:], start=True, stop=True), reads=[b_s], writes=[b_cb])
                s.op("dve", lambda e: e.tensor_scalar(out=gef[:], in0=cb[:, 0:1], scalar1=511.5, scalar2=None, op0=ALU.is_ge), reads=[b_cb], writes=[b_s])
                s.op("dve", lambda e: e.scalar_tensor_tensor(out=lo[:], in0=gef[:], scalar=mid[:, 0:1], in1=lo[:], op0=ALU.mult, op1=ALU.max),
                     reads=[b_s], writes=[b_s])
                step *= 0.5
                st2 = step
                s.op("dve", lambda e: e.tensor_scalar(out=tt[:], in0=gef[:], scalar1=2.0 * st2, scalar2=-st2, op0=ALU.mult, op1=ALU.add),
                     reads=[b_s], writes=[b_s])
                s.op("dve", lambda e: e.tensor_tensor(out=mid[:], in0=mid[:], in1=tt[:], op=ALU.add), reads=[b_s], writes=[b_s])
            s.op("dve", lambda e: e.scalar_tensor_tensor(out=am[:], in0=affT[:], scalar=lo[:, 0:1], in1=affT[:], op0=ALU.is_ge, op1=ALU.mult),
                 reads=[b_s], writes=[b_s])
            for r in range(ROUNDS):
                sl = slice(r * 8, (r + 1) * 8)
                s.op("dve", lambda e: e.max(out=vals[:, sl], in_=am[:]), reads=[b_s], writes=[b_s])
                s.op("dve", lambda e: e.max_index(out=idxs[:, sl], in_max=vals[:, sl], in_values=am[:]), reads=[b_s], writes=[b_s])
                s.op("dve", lambda e: e.match_replace(out=am[:], in_to_replace=vals[:, sl], in_values=am[:], imm_value=-1.0), reads=[b_s], writes=[b_s])
            NSL = ROUNDS * 8
            NIC = NSL // 128
            U8 = mybir.dt.uint8
            s.op("dve", lambda e: e.tensor_scalar(out=vmask[:], in0=vals[:], scalar1=0.0, scalar2=None, op0=ALU.is_gt, op1=ALU.add, accum_out=nrow[:]),
                 reads=[b_s], writes=[b_s])
            s.op("dve", lambda e: e.tensor_tensor(out=vals[:], in0=vals[:], in1=vmask[:], op=ALU.mult), reads=[b_s], writes=[b_s])
            idx8 = idxs[:].bitcast(U8).rearrange("p (n two) -> p n two", two=2)
            dig = T(f"dig{l}", [128, 4, NSL], BF16, ps)
            tmpf = T(f"tmpf{l}", [128, NSL], F32, ps)
            s.op("dve", lambda e: e.tensor_copy(out=tmpf[:].rearrange("p (n o) -> p n o", o=1), in_=idx8[:, :, 1:2]), reads=[b_s], writes=[b_s])
            s.op("dve", lambda e: e.scalar_tensor_tensor(out=dig[:, 0, :], in0=tmpf[:], scalar=rowoff[:, 0:1], in1=vmask[:], op0=ALU.add, op1=ALU.mult),
                 reads=[b_s, b_c], writes=[b_s])
            s.op("dve", lambda e: e.tensor_copy(out=tmpf[:].rearrange("p (n o) -> p n o", o=1), in_=idx8[:, :, 0:1]), reads=[b_s], writes=[b_s])
            s.op("dve", lambda e: e.tensor_tensor(out=dig[:, 1, :], in0=tmpf[:], in1=vmask[:], op=ALU.mult), reads=[b_s], writes=[b_s])
            s.op("dve", lambda e: e.tensor_copy(out=dig[:, 2, :], in_=vals[:]), reads=[b_s], writes=[b_s])
            s.op("dve", lambda e: e.tensor_tensor(out=dig[:, 3, :], in0=vals[:], in1=dig[:, 2, :], op=ALU.subtract), reads=[b_s], writes=[b_s])
            s.op("pe", lambda e: e.matmul(banks[2][:, 0:1], lhsT=trimat[:], rhs=nrow[:], start=True, stop=True), reads=[b_s, b_c], writes=[bbuf[2]])
            s.op("dve", lambda e: e.tensor_copy(out=off_sb[:], in_=banks[2][:, 0:1]), reads=[bbuf[2]], writes=[b_s])
            s.op("dve", lambda e: e.tensor_scalar(out=diag[:], in0=ident_f[:], scalar1=off_sb[:, 0:1], scalar2=None, op0=ALU.mult), reads=[b_s], writes=[b_s])
            s.op("pe", lambda e: e.matmul(banks[3][:, 0:128], lhsT=ones_f[:], rhs=diag[:], start=True, stop=True), reads=[b_s], writes=[bbuf[3]])
            s.op("act", lambda e: e.copy(out=offB[:], in_=banks[3][:, 0:128]), reads=[bbuf[3]], writes=[b_s])
            tb2 = banks[0][:].bitcast(BF16)
            for ic in range(NIC):
                for k in range(4):
                    col = (ic * 4 + k) * 128
                    s.op("pe", lambda e: e.transpose(out=tb2[:, col:col + 128], in_=dig[:, k, ic * 128:(ic + 1) * 128], identity=ident_bf[:]),
                         reads=[b_s], writes=[bbuf[0]])
                s.op("dve", lambda e: e.tensor_copy(out=RT[:, ic, :, :].rearrange("p r k -> p k r"),
                                                    in_=tb2[:, ic * 512:(ic + 1) * 512].rearrange("p (k r) -> p k r", k=4)), reads=[bbuf[0]], writes=[b_s])
            s.barrier()
            sels = Rot([(T(f"selt{l}_{i}", [128, 512], BF16, ps), Buf()) for i in range(4)])
            for row in range(128):
                e_ = row // 8
                for ic in range(NIC):
                    sel_t, b_sel = sels.next()
                    s.op("dve", lambda e: e.tensor_scalar(out=sel_t[:], in0=dmat[:, ic, :], scalar1=offB[:, row:row + 1], scalar2=None, op0=ALU.is_equal),
                         writes=[b_sel])
                    first = (row % 8 == 0 and ic == 0)
                    last = (row % 8 == 7 and ic == NIC - 1)
                    for jc in range(4):
                        s.op("pe", lambda e: e.matmul(banks[4 + jc][:, e_ * 4:e_ * 4 + 4], lhsT=sel_t[:, jc * 128:(jc + 1) * 128], rhs=RT[:, ic, row, :],
                                                      start=first, stop=last), reads=[b_sel], writes=[bbuf[4 + jc]], inc=(jc == 3))
            rs_sb = T(f"rs_sb{l}", [128, 4, NE, 4], F32, ps)
            b_rs = Buf()
            for jc in range(4):
                s.op("act", lambda e: e.copy(out=rs_sb[:, jc, :, :], in_=banks[4 + jc][:, 0:4 * NE].rearrange("p (e t) -> p e t", t=4)),
                     reads=[bbuf[4 + jc]], writes=[b_rs])
            dg = lambda k: rs_sb[:, :, :, k:k + 1].rearrange("p j e o -> p j (e o)")
            s.op("dve", lambda e: e.scalar_tensor_tensor(out=idsT[:].rearrange("p (e j) -> p j e", j=4), in0=dg(0), scalar=256.0, in1=dg(1),
                                                         op0=ALU.mult, op1=ALU.add), reads=[b_rs])
            s.op("dve", lambda e: e.tensor_tensor(out=gT[:].rearrange("p (e j) -> p j e", j=4), in0=dg(2), in1=dg(3), op=ALU.add), reads=[b_rs])
            if debug:
                s.barrier()
                s.dma("sp", lambda e: e.dma_start(out=aff_dbg, in_=affT[:]))
                s.dma("sp", lambda e: e.dma_start(out=sel_dbg[:, 0:64], in_=idsT[:].bitcast(F32)))
                s.dma("sp", lambda e: e.dma_start(out=sel_dbg[:, 64:128], in_=gT[:]))
            s.barrier()
        if stop_after == "p4":
            s.barrier()
            wst.close()
            sel.close()
            break

        with ExitStack() as ps:
            xss = Rot([(T(f"xs{l}_{i}", [128, D], BF16, ps), Buf()) for i in range(8)])
            xsTs = [(T(f"xsT{l}_{i}", [128, 8, 512], BF16, ps), Buf()) for i in range(2)]
            hidTs = Rot([(T(f"hidT{l}_{i}", [128, 8, 512], BF16, ps), Buf()) for i in range(2)])
            sgs = Rot([(T(f"sg{l}_{i}", [128, 512], F32, ps), Buf()) for i in range(2)])
            ys = Rot([(T(f"y{l}_{i}", [128, D], F32, ps), Buf()) for i in range(2)])
            tpk = Rot([(banks[0], bbuf[0]), (banks[1], bbuf[1])])
            gub = Rot([(banks[2], bbuf[2], banks[3], bbuf[3]), (banks[4], bbuf[4], banks[5], bbuf[5])])
            ybk = Rot([(banks[6], bbuf[6]), (banks[7], bbuf[7])])
            hreg = [[Buf(f"hreg{q}_{p}") for p in range(ROWS_PER_E)] for q in range(2)]
            NCH = NE * SLOT_CHUNKS

            def rows_of(ch):
                e_, half = ch // SLOT_CHUNKS, ch % SLOT_CHUNKS
                return [e_ * ROWS_PER_E + half * 4 + i for i in range(4)]

            gathered = {}

            def issue_gathers(ch):
                lst_ = []
                for r in rows_of(ch):
                    xs, b_xs = xss.next()
                    s.dma("pool", lambda e: e.indirect_dma_start(out=xs[:], out_offset=None, in_=xn2_d,
                                                                 in_offset=bass.IndirectOffsetOnAxis(ap=idsT[:, r:r + 1], axis=0)),
                          writes=[b_xs])
                    lst_.append((xs, b_xs))
                gathered[ch] = lst_

            def do_transposes(ch):
                xsT, b_xsT = xsTs[ch % 2]
                for i, (xs, b_xs) in enumerate(gathered.pop(ch)):
                    pt, b_pt = tpk.next()
                    ptv = pt[:].bitcast(BF16)
                    for c in range(8):
                        s.op("pe", lambda e: e.transpose(out=ptv[:, c * 128:(c + 1) * 128], in_=xs[:, c * 128:(c + 1) * 128], identity=ident_bf[:]),
                             reads=[b_xs], writes=[b_pt], inc=(c == 7))
                    s.op("dve", lambda e: e.tensor_copy(out=xsT[:, :, i * 128:(i + 1) * 128], in_=ptv.rearrange("p (c t) -> p c t", c=8)),
                         reads=[b_pt], writes=[b_xsT])

            issue_gathers(0)
            do_transposes(0)
            for ch in range(NCH):
                e_ = ch // SLOT_CHUNKS
                Wg, Wu, Wd, b_W = Ws[e_ % 2]
                wsteps = load_expert_steps(e_ + 1) if (ch % SLOT_CHUNKS == 0 and e_ + 1 < NE) else []
                if ch + 1 < NCH:
                    issue_gathers(ch + 1)
                xsT, b_xsT = xsTs[ch % 2]
                hidT, b_hid = hidTs.next()
                for f in range(8):
                    for _ in range(3):
                        if wsteps:
                            wsteps.pop(0)()
                    gb, bgb, ub, bub = gub.next()
                    for c in range(8):
                        s.op("pe", lambda e: e.matmul(gb[:, :], lhsT=Wg[:, c, f * 128:(f + 1) * 128], rhs=xsT[:, c, :], start=(c == 0), stop=(c == 7)),
                             reads=[b_W, b_xsT], writes=[bgb], inc=(c == 7))
                    for c in range(8):
                        s.op("pe", lambda e: e.matmul(ub[:, :], lhsT=Wu[:, c, f * 128:(f + 1) * 128], rhs=xsT[:, c, :], start=(c == 0), stop=(c == 7)),
                             reads=[b_W, b_xsT], writes=[bub], inc=(c == 7))
                    sg, b_sg = sgs.next()
                    s.op("act", lambda e: e.activation(out=sg[:], in_=gb[:, :], func=AF.Silu), reads=[bgb], writes=[b_sg])
                    s.op("dve", lambda e: e.tensor_tensor(out=hidT[:, f, :], in0=ub[:, :], in1=sg[:], op=ALU.mult), reads=[bub, b_sg], writes=[b_hid])
                if ch + 1 < NCH:
                    do_transposes(ch + 1)
                for i, r in enumerate(rows_of(ch)):
                    part = r % ROWS_PER_E
                    y, b_y = ys.next()
                    for hc in range(2):
                        yb, byb = ybk.next()
                        for f in range(8):
                            s.op("pe", lambda e: e.matmul(yb[:, :], lhsT=hidT[:, f, i * 128:(i + 1) * 128], rhs=Wd[:, f, hc * 512:(hc + 1) * 512],
                                                          start=(f == 0), stop=(f == 7)),
                                 reads=[b_W, b_hid], writes=[byb], inc=(f == 7))
                        s.op("dve", lambda e: e.tensor_scalar(out=y[:, hc * 512:(hc + 1) * 512], in0=yb[:, :], scalar1=gT[:, r:r + 1], scalar2=None,
                                                              op0=ALU.mult), reads=[byb], writes=[b_y])
                    s.dma("pool", lambda e: e.indirect_dma_start(out=h_d, out_offset=bass.IndirectOffsetOnAxis(ap=idsT[:, r:r + 1], axis=0),
                                                                 in_=y[:], in_offset=None, compute_op=ALU.add),
                          reads=[b_y] + hreg[(e_ + 1) % 2], writes=[hreg[e_ % 2][part]])
            s.barrier()
        wst.close()
        sel.close()

    if stop_after is None:
        with ExitStack() as ps:
            g_bc = T("gfin", [128, D], F32, ps)
            b_g = Buf()
            s.dma("sp", lambda e: e.dma_start(out=g_bc[:], in_=final_norm_d[0:1, :].partition_broadcast(128)), writes=[b_g])
            hts = Rot([(T(f"htf_{i}", [128, D], F32, ps), Buf()) for i in range(3)])
            ots = Rot([(T(f"otf_{i}", [128, D], F32, ps), Buf()) for i in range(3)])
            junk = T("junkf", [128, D], BF16, ps)
            b_junk = Buf()
            sss = Rot([(T(f"ssf_{i}", [128, 1], F32, ps), Buf()) for i in range(2)])
            for tb in range(NTB):
                ht, b_ht = hts.next()
                ot, b_ot = ots.next()
                ss, b_ss = sss.next()
                s.dma("sp", lambda e: e.dma_start(out=ht[:], in_=h_d[tb * 128:(tb + 1) * 128, :]), writes=[b_ht])
                s.op("dve", lambda e: e.scalar_tensor_tensor(out=junk[:], in0=ht[:], scalar=1.0, in1=ht[:],
                                                             op0=ALU.mult, op1=ALU.mult, accum_out=ss[:]),
                     reads=[b_ht], writes=[b_junk, b_ss])
                rstd_from_ss(ss[:], ss[:], 1.0 / D, [b_ss], [b_ss])
                s.op("dve", lambda e: e.scalar_tensor_tensor(out=ot[:], in0=ht[:], scalar=ss[:, 0:1], in1=g_bc[:],
                                                             op0=ALU.mult, op1=ALU.mult), reads=[b_ht, b_ss, b_g], writes=[b_ot])
                s.dma("sp", lambda e: e.dma_start(out=out_d[tb * 128:(tb + 1) * 128, :], in_=ot[:]), reads=[b_ot])
    s.barrier()
    es.close()
    return nc


def _swap_halves(w, hd):
    sh = w.shape
    w4 = w.reshape(sh[:-1] + (sh[-1] // hd, 2, hd // 2))
    return np.ascontiguousarray(w4[..., ::-1, :]).reshape(sh)


def _rope_tables(dim):
    inv = (1.0 / (np.float32(10000.0) ** (np.arange(0, dim, 2, dtype=np.float32) / np.float32(dim)))).astype(np.float32)
    ang = np.arange(S, dtype=np.float32)[:, None] * inv[None, :]
    return np.cos(ang).astype(np.float32), np.sin(ang).astype(np.float32)


def _host_prep(inp):
    f = lambda a: np.ascontiguousarray(a, dtype=np.float32)
    w_in = np.asarray(inp["w_in"])
    shared = {}
    shared["attn_norm"] = f(inp["attn_norm"])
    shared["ffn_norm"] = f(inp["ffn_norm"])
    shared["final_norm"] = f(np.asarray(inp["final_norm"]).reshape(1, D))
    shared["w_na"] = f(w_in[:, :, 0:768])
    shared["w_lat"] = f(w_in[:, :, 768:1152])
    kr96 = w_in[:, :, 1088:1184]
    shared["w_kr"] = f(kr96)
    krp = np.array(kr96, copy=True)
    krp[:, :, 64:96] = _swap_halves(kr96[:, :, 64:96], 32)
    shared["w_krp"] = f(krp)
    cq = w_in[:, :, 1184:1568].reshape(L, D, 6, 64)
    order = [0, 3, 1, 4, 2, 5]
    cq_r = cq[:, :, order, :].reshape(L, D, 384)
    shared["w_cq"] = f(cq_r)
    shared["w_cqp"] = f(_swap_halves(cq_r, 64))
    ck = w_in[:, :, 1568:1696]
    shared["w_ck"] = f(ck)
    shared["w_ckp"] = f(_swap_halves(ck, 64))
    shared["w_cv"] = f(w_in[:, :, 1696:1824])
    rpb = np.asarray(inp["na_rpb"], dtype=np.float32)
    tiles = np.full((L, NV, 4, 128, 320), NEG, np.float32)
    p = np.arange(128)
    kc = p % 64
    krl = p // 64
    c = np.arange(64)
    cs_ = np.clip(c - 8, 0, 48)
    colvalid = (kc[:, None] >= cs_[None, :]) & (kc[:, None] <= cs_[None, :] + 15)
    dc = np.clip(kc[:, None] - c[None, :] + 15, 0, 30)
    for v, (d0, roff, nb) in enumerate(NA_KEYS):
        for b in range(nb):
            kr_rel = 2 * b + krl
            rowvalid = (kr_rel >= roff) & (kr_rel <= roff + 7)
            dr = np.clip(d0 + kr_rel + 7, 0, 14)
            vals = rpb[:, :, dr[:, None], dc]
            ok = (rowvalid[:, None] & colvalid)[None, None]
            tiles[:, v, :, :, b * 64:(b + 1) * 64] = np.where(ok, vals, np.float32(NEG))
    shared["rpb_tiles"] = tiles
    shared["mla_q_norm"] = f(inp["mla_q_norm"])
    shared["mla_kv_norm"] = f(inp["mla_kv_norm"])
    w_uq = np.asarray(inp["mla_w_uq"])
    shared["w_uq"] = f(w_uq)
    uq4 = np.array(w_uq.reshape(L, 256, 6, 96), copy=True)
    uq4[..., 64:96] = _swap_halves(uq4[..., 64:96], 32)
    shared["w_uqp"] = f(uq4.reshape(L, 256, 576))
    ukv = np.asarray(inp["mla_w_ukv"]).reshape(L, 128, 6, 128)
    shared["w_uk"] = f(ukv[..., 0:64].reshape(L, 128, 384))
    shared["w_uv"] = f(ukv[..., 64:128].reshape(L, 128, 384))
    shared["swa_sink"] = f(inp["swa_sink"])
    shared["group_norm"] = f(inp["group_norm"])
    shared["w_out"] = f(inp["w_out"])
    shared["w_router"] = f(inp["w_router"])
    shared["w_gate"] = f(inp["w_gate"])
    shared["w_up"] = f(inp["w_up"])
    shared["w_down"] = f(inp["w_down"])
    cos, sin = _rope_tables(64)
    cT = np.concatenate([cos.T, cos.T], 0)
    sT = np.concatenate([-sin.T, sin.T], 0)
    shared["rope_c_cos"] = f(np.concatenate([cT, cT], 0))
    shared["rope_c_sin"] = f(np.concatenate([sT, sT], 0))
    cos, sin = _rope_tables(32)
    shared["rope_b_cos"] = f(np.concatenate([cos.T, cos.T], 0))
    shared["rope_b_sin"] = f(np.concatenate([-sin.T, sin.T], 0))
    k = np.arange(128)[:, None]
    q = np.arange(512)[None, :]
    m = np.zeros((6, 128, 512), np.float32)
    for j in range(6):
        diff = q - k - (j - 1) * 128
        m[j] = np.where(np.abs(diff) <= 128, 1.0, 0.0)
    shared["swa_mask"] = m.astype(ml_dtypes.bfloat16)
    shared["ident_bf"] = np.eye(128, dtype=np.float32).astype(ml_dtypes.bfloat16)
    shared["ident_f"] = np.eye(128, dtype=np.float32)
    pp = np.arange(128)
    shared["gmat"] = (pp[:, None] // 8 == pp[None, :] // 8).astype(np.float32)
    shared["rowoff"] = ((pp % 8) * 2).astype(np.float32).reshape(128, 1)
    shared["trimat"] = ((pp[:, None] // 8 == pp[None, :] // 8) & (pp[:, None] < pp[None, :])).astype(np.float32)
    nic = ROUNDS * 8 // 128
    ii = np.arange(128)[:, None, None]
    icc = np.arange(nic)[None, :, None]
    jj = np.arange(512)[None, None, :]
    shared["dmat"] = (jj - ii - icc * 128).astype(np.int16)
    shared["invw"] = np.tile(np.array([[1 / 256, 1 / 384, 1 / 384]], np.float32), (128, 1))
    return shared


def kernel(**inputs):
    shared = _host_prep(inputs)
    x = np.asarray(inputs["x"], dtype=np.float32)
    nc = build()
    in_maps = []
    for c in range(NCORES):
        m = dict(shared)
        m["x"] = np.ascontiguousarray(x[c])
        in_maps.append(m)
    res = run_bass_kernel_spmd(nc, in_maps, core_ids=list(range(NCORES)))
    return np.stack([np.asarray(res.results[c]["out"], dtype=np.float32) for c in range(NCORES)], axis=0)
```

```python
import numpy as np
import ml_dtypes
from contextlib import ExitStack
import concourse.bass as bass
import concourse.mybir as mybir
from concourse.bass_utils import run_bass_kernel_spmd

F32 = mybir.dt.float32
BF16 = mybir.dt.bfloat16
U32 = mybir.dt.uint32
AF = mybir.ActivationFunctionType
ALU = mybir.AluOpType

S = 4096
D = 1024
NTB = 32
NQB = 8
L = 2
NE = 16
EPS = 1e-6
NEG = -30000.0
NCORES = 8
ROUNDS = 32
ROWS_PER_E = 4
SLOT_CHUNKS = 1
NBIS = 27


class Buf:
    __slots__ = ("name", "writer", "readers")

    def __init__(self, name=""):
        self.name = name
        self.writer = None
        self.readers = []


class Sched:
    ENG = ("pe", "act", "dve", "pool", "sp")

    def __init__(self, nc, es, same_engine_sync=True):
        self.nc = nc
        self.e = {"pe": nc.tensor, "act": nc.scalar, "dve": nc.vector,
                  "pool": nc.gpsimd, "sp": nc.sync}
        self.sem = {k: es.enter_context(nc.semaphore("s_" + k)) for k in self.ENG}
        self.seq = {k: 0 for k in self.ENG}
        self.waited = {a: {} for a in self.ENG}
        self.same_engine_sync = same_engine_sync
        self.lanes = {}
        self.lane_rr = {}
        for q, n in (("sp", 16), ("pool", 16)):
            self.lanes[q] = [[es.enter_context(nc.semaphore(f"d_{q}{i}")), 0] for i in range(n)]
            self.lane_rr[q] = 0
        self.pending_reads = {k: [] for k in self.ENG}

    def _wait(self, on, dep):
        if dep is None:
            return
        if dep[0] == "eng":
            _, eng, seq = dep
            if eng == on and (not self.same_engine_sync or on == "pe"):
                return
            if self.waited[on].get(eng, 0) >= seq:
                return
            self.e[on].wait_ge(self.sem[eng], seq)
            self.waited[on][eng] = seq
        else:
            _, q, li, cnt = dep
            key = ("dma", q, li)
            if self.waited[on].get(key, 0) >= cnt:
                return
            self.e[on].wait_ge(self.lanes[q][li][0], 16 * cnt)
            self.waited[on][key] = cnt

    def _deps(self, on, reads, writes):
        for r in reads:
            self._wait(on, r.writer)
        for w in writes:
            self._wait(on, w.writer)
            for d in w.readers:
                self._wait(on, d)

    def _commit(self, dep, reads, writes):
        for w in writes:
            w.writer = dep
            w.readers = []
        for r in reads:
            if r not in writes:
                r.readers.append(dep)
                if len(r.readers) > 48:
                    last = {}
                    for d in r.readers:
                        k = d[:2] if d[0] == "eng" else d[:3]
                        if k not in last or d[-1] > last[k][-1]:
                            last[k] = d
                    r.readers = list(last.values())

    def op(self, on, fn, reads=(), writes=(), inc=True):
        reads = list(reads)
        writes = list(writes)
        self._deps(on, reads, writes)
        inst = fn(self.e[on])
        if inc:
            self.seq[on] += 1
            inst.then_inc(self.sem[on], 1)
            dep = ("eng", on, self.seq[on])
            self._commit(dep, reads + self.pending_reads[on], writes)
            self.pending_reads[on] = []
        else:
            self.pending_reads[on].extend(reads)
            for w in writes:
                w.writer = ("eng", on, self.seq[on] + 1)
                w.readers = []
        return inst

    def dma(self, q, fn, reads=(), writes=()):
        reads = list(reads)
        writes = list(writes)
        lanes = self.lanes[q]
        li = self.lane_rr[q]
        self.lane_rr[q] = (li + 1) % len(lanes)
        sem, cnt = lanes[li]
        if cnt > 0:
            self._wait(q, ("dma", q, li, cnt))
        self._deps(q, reads, writes)
        inst = fn(self.e[q])
        inst.then_inc(sem, 16)
        lanes[li][1] = cnt + 1
        dep = ("dma", q, li, cnt + 1)
        self._commit(dep, reads, writes)
        return inst

    def barrier(self):
        for a in self.ENG:
            for b in self.ENG:
                if a != b and self.seq[b] > 0:
                    self._wait(a, ("eng", b, self.seq[b]))
            for q in self.lanes:
                for li, (sem, cnt) in enumerate(self.lanes[q]):
                    if cnt > 0:
                        self._wait(a, ("dma", q, li, cnt))


class Rot:
    def __init__(self, items):
        self.items = items
        self.i = 0

    def next(self):
        it = self.items[self.i]
        self.i = (self.i + 1) % len(self.items)
        return it


def _na_row_info(r):
    rs = min(max(r - 4, 0), 56)
    if rs % 2 == 0:
        start, nb = rs, 4
    else:
        start, nb = rs - 1, 5
    return rs, start, nb


def _na_variants():
    keys = []
    vmap = {}
    for r in range(64):
        rs, start, nb = _na_row_info(r)
        k = (start - r, rs - start, nb)
        if k not in keys:
            keys.append(k)
        vmap[r] = keys.index(k)
    return keys, vmap


NA_KEYS, NA_VMAP = _na_variants()
NV = len(NA_KEYS)


def build(debug=False, nlayers=L, stop_after=None):
    nc = bass.Bass("TRN2", target_bir_lowering=False)
    es = ExitStack()

    def din(name, shape, dt=F32):
        return nc.dram_tensor(name, list(shape), dt, kind="ExternalInput").ap()

    dbg_kind = "ExternalOutput" if debug else "Internal"

    def dscr(name, shape, dt=F32, dbg=True):
        return nc.dram_tensor(name, list(shape), dt, kind=(dbg_kind if dbg else "Internal")).ap()

    x_d = din("x", [S, D])
    attn_norm_d = din("attn_norm", [L, D])
    ffn_norm_d = din("ffn_norm", [L, D])
    final_norm_d = din("final_norm", [1, D])
    w_na_d = din("w_na", [L, D, 768])
    w_lat_d = din("w_lat", [L, D, 384])
    w_kr_d = din("w_kr", [L, D, 96])
    w_krp_d = din("w_krp", [L, D, 96])
    w_cq_d = din("w_cq", [L, D, 384])
    w_cqp_d = din("w_cqp", [L, D, 384])
    w_ck_d = din("w_ck", [L, D, 128])
    w_ckp_d = din("w_ckp", [L, D, 128])
    w_cv_d = din("w_cv", [L, D, 128])
    rpb_d = din("rpb_tiles", [L, NV, 4, 128, 320])
    qn_d = din("mla_q_norm", [L, 256])
    kvn_d = din("mla_kv_norm", [L, 128])
    w_uq_d = din("w_uq", [L, 256, 576])
    w_uqp_d = din("w_uqp", [L, 256, 576])
    w_uk_d = din("w_uk", [L, 128, 384])
    w_uv_d = din("w_uv", [L, 128, 384])
    sink_d = din("swa_sink", [L, 6])
    gn_d = din("group_norm", [L, D])
    w_out_d = din("w_out", [L, D, D])
    w_router_d = din("w_router", [L, D, NE])
    w_gate_d = din("w_gate", [L, NE, D, D])
    w_up_d = din("w_up", [L, NE, D, D])
    w_down_d = din("w_down", [L, NE, D, D])
    cs_c_d = din("rope_c_cos", [128, S])
    sn_c_d = din("rope_c_sin", [128, S])
    cs_b_d = din("rope_b_cos", [32, S])
    sn_b_d = din("rope_b_sin", [32, S])
    swa_mask_d = din("swa_mask", [6, 128, 512], BF16)
    ident_bf_d = din("ident_bf", [128, 128], BF16)
    ident_f_d = din("ident_f", [128, 128])
    gmat_d = din("gmat", [128, 128])
    rowoff_d = din("rowoff", [128, 1])
    invw_d = din("invw", [128, 3])
    trimat_d = din("trimat", [128, 128])
    dmat_d = din("dmat", [128, ROUNDS * 8 // 128, 512], mybir.dt.int16)

    out_d = nc.dram_tensor("out", [S, D], F32, kind="ExternalOutput").ap()
    h_d = dscr("h_scr", [S, D])
    xn2_d = dscr("xn2_scr", [S, D], BF16)
    oc_d = dscr("oc_scr", [16, 64, S], BF16)
    aff_dbg = dscr("aff_dbg", [128, 512]) if debug else None
    sel_dbg = dscr("sel_dbg", [128, 128]) if debug else None

    s = Sched(nc, es)
    T = lambda name, shape, dt, st=es: st.enter_context(nc.sbuf_tensor("t_" + name, list(shape), dt))

    pp = [es.enter_context(nc.psum_tensor(f"pp{i}", [128, 1024], F32)) for i in range(4)]
    banks = [pp[i // 2][:, (i % 2) * 512:(i % 2 + 1) * 512] for i in range(8)]
    bbuf = [Buf(f"bank{i}") for i in range(8)]

    ident_bf = T("ident_bf", [128, 128], BF16)
    ident_f = T("ident_f", [128, 128], F32)
    ones_bf = T("ones_bf", [128, 128], BF16)
    ones_f = T("ones_f", [128, 128], F32)
    eps_t = T("eps_t", [128, 1], F32)
    cbuf = Buf("consts")
    s.dma("sp", lambda e: e.dma_start(out=ident_bf[:], in_=ident_bf_d), writes=[cbuf])
    s.dma("sp", lambda e: e.dma_start(out=ident_f[:], in_=ident_f_d), writes=[cbuf])
    s.op("dve", lambda e: e.memset(ones_bf[:], 1.0), writes=[cbuf])
    s.op("dve", lambda e: e.memset(ones_f[:], 1.0), writes=[cbuf])
    s.op("dve", lambda e: e.memset(eps_t[:], EPS), writes=[cbuf])
    s.barrier()

    def rstd_big(ss_ap, out_ap, scratch_ap, inv_n, rd, wr):
        s.op("dve", lambda e: e.tensor_scalar(out=out_ap, in0=ss_ap, scalar1=inv_n, scalar2=EPS,
                                              op0=ALU.mult, op1=ALU.add), reads=rd, writes=wr)
        s.op("act", lambda e: e.activation(out=out_ap, in_=out_ap, func=AF.Ln), reads=wr, writes=wr)
        s.op("act", lambda e: e.activation(out=out_ap, in_=out_ap, func=AF.Exp, scale=-0.5), reads=wr, writes=wr)

    def rstd_from_ss(ss_ap, out_ap, inv_n, rd, wr):
        s.op("dve", lambda e: e.tensor_scalar(out=out_ap, in0=ss_ap, scalar1=inv_n, scalar2=EPS,
                                              op0=ALU.mult, op1=ALU.add), reads=rd, writes=wr)
        s.op("act", lambda e: e.activation(out=out_ap, in_=out_ap, func=AF.Sqrt), reads=wr, writes=wr)
        s.op("dve", lambda e: e.reciprocal(out=out_ap, in_=out_ap), reads=wr, writes=wr)

    evac_rr = [0]

    def evac_copy(out_ap, in_ap, rd, wr, scale=None):
        evac_rr[0] ^= 1
        if evac_rr[0]:
            if scale is None:
                s.op("act", lambda e: e.copy(out=out_ap, in_=in_ap), reads=rd, writes=wr)
            else:
                s.op("act", lambda e: e.mul(out=out_ap, in_=in_ap, mul=scale), reads=rd, writes=wr)
        else:
            if scale is None:
                s.op("dve", lambda e: e.tensor_copy(out=out_ap, in_=in_ap), reads=rd, writes=wr)
            else:
                s.op("dve", lambda e: e.tensor_scalar(out=out_ap, in0=in_ap, scalar1=scale, scalar2=None,
                                                      op0=ALU.mult), reads=rd, writes=wr)

    fin_queue = []
    fin_age = [3]

    def flush_fin(force=False):
        for ent in fin_queue:
            ent[0] += 1
        while fin_queue and (force or fin_queue[0][0] > fin_age[0]):
            fin_queue.pop(0)[1]()

    def finalize_heads(O_bank, O_buf, j_oc, col0, st_tiles, extra_den=None, bank_src=None, use_act=False):
        rden, b_rden, bc_bank, b_bc, bc_sb, b_bcsb, o_bf, b_obf = st_tiles
        if use_act:
            if extra_den is not None:
                s.op("act", lambda e: e.activation(out=rden[64:65, 0:512], in_=O_bank[64:65, :], func=AF.Ln, bias=extra_den),
                     reads=[O_buf], writes=[b_rden])
            else:
                s.op("act", lambda e: e.activation(out=rden[64:65, 0:512], in_=O_bank[64:65, :], func=AF.Ln),
                     reads=[O_buf], writes=[b_rden])
            s.op("act", lambda e: e.activation(out=rden[64:65, 0:512], in_=rden[64:65, 0:512], func=AF.Exp, scale=-1.0),
                 reads=[b_rden], writes=[b_rden])
        elif extra_den is not None:
            s.op("dve", lambda e: e.tensor_scalar(out=rden[64:65, 0:512], in0=O_bank[64:65, :], scalar1=extra_den,
                                                  scalar2=None, op0=ALU.add), reads=[O_buf], writes=[b_rden])
            s.op("dve", lambda e: e.reciprocal(out=rden[64:65, 0:512], in_=rden[64:65, 0:512]), reads=[b_rden], writes=[b_rden])
        else:
            s.op("dve", lambda e: e.reciprocal(out=rden[64:65, 0:512], in_=O_bank[64:65, :]), reads=[O_buf], writes=[b_rden])

        def fin_b(bc_bank=bc_bank, b_bc=b_bc):
            if bank_src is not None:
                Spb, b_Spb = bank_src.next()
                bc_bank, b_bc = Spb[:, 0:512], b_Spb[0]
            s.op("pe", lambda e: e.matmul(bc_bank[0:64, :], lhsT=ones_f[64:65, 0:64], rhs=rden[64:65, 0:512],
                                          start=True, stop=True), reads=[b_rden], writes=[b_bc])
            s.op("act" if use_act else "dve", (lambda e: e.copy(out=bc_sb[0:64, :], in_=bc_bank[0:64, :])) if use_act else
                 (lambda e: e.tensor_copy(out=bc_sb[0:64, :], in_=bc_bank[0:64, :])), reads=[b_bc], writes=[b_bcsb])
            s.op("dve", lambda e: e.tensor_tensor(out=o_bf[0:64, :], in0=O_bank[0:64, :], in1=bc_sb[0:64, :], op=ALU.mult),
                 reads=[O_buf, b_bcsb], writes=[b_obf])
            s.dma("sp", lambda e: e.dma_start(out=oc_d[j_oc, :, col0:col0 + 512], in_=o_bf[0:64, :]), reads=[b_obf])
        fin_queue.append([0, fin_b])

    for l in range(nlayers):
        src_d = x_d if l == 0 else h_d
        lat = ExitStack()
        cqn = T(f"cqn{l}", [128, 2, S], BF16, lat)
        ckvn = T(f"ckvn{l}", [128, S], BF16, lat)
        kpe = T(f"kpe{l}", [96, S], BF16, lat)
        lst = ExitStack()
        xnT = T(f"xnT{l}", [128, 8, S], BF16, lst)
        nst = ExitStack()
        rpb_sb = T(f"rpb{l}", [128, NV * 4, 320], BF16, nst)
        b_rpb = Buf()
        for v in range(NV):
            s.dma("pool", lambda e: e.dma_start(out=rpb_sb[:, v * 4:(v + 1) * 4, :], in_=rpb_d[l, v].rearrange("h p c -> p h c")), writes=[b_rpb])
        with ExitStack() as ps:
            g_bc = T(f"g_bc{l}", [128, D], F32, ps)
            b_g = Buf()
            s.dma("sp", lambda e: e.dma_start(out=g_bc[:], in_=attn_norm_d[l:l + 1, :].partition_broadcast(128)), writes=[b_g])
            hts = Rot([(T(f"ht{l}_{i}", [128, D], F32, ps), Buf()) for i in range(4)])
            xbs = Rot([(T(f"xb{l}_{i}", [128, D], BF16, ps), Buf()) for i in range(2)])
            junk = T(f"junk{l}", [128, D], BF16, ps)
            b_junk = Buf()
            sss = Rot([(T(f"ss{l}_{i}", [128, 1], F32, ps), Buf()) for i in range(4)])
            pts = Rot([(banks[i], bbuf[i]) for i in range(2)])
            def p1_a(tb):
                ht, b_ht = hts.next()
                ss, b_ss = sss.next()
                s.dma("sp", lambda e: e.dma_start(out=ht[:], in_=src_d[tb * 128:(tb + 1) * 128, :]), writes=[b_ht])
                s.op("dve", lambda e: e.scalar_tensor_tensor(out=junk[:], in0=ht[:], scalar=1.0, in1=ht[:],
                                                             op0=ALU.mult, op1=ALU.mult, accum_out=ss[:]),
                     reads=[b_ht], writes=[b_junk, b_ss])
                s.op("dve", lambda e: e.tensor_scalar(out=ss[:], in0=ss[:], scalar1=1.0 / D, scalar2=EPS, op0=ALU.mult, op1=ALU.add),
                     reads=[b_ss], writes=[b_ss])
                s.op("act", lambda e: e.activation(out=ss[:], in_=ss[:], func=AF.Sqrt), reads=[b_ss], writes=[b_ss])
                return (tb, ht, b_ht, ss, b_ss)

            def p1_b(ctx):
                tb, ht, b_ht, ss, b_ss = ctx
                xb, b_xb = xbs.next()
                pt, b_pt = pts.next()
                s.op("dve", lambda e: e.reciprocal(out=ss[:], in_=ss[:]), reads=[b_ss], writes=[b_ss])
                s.op("dve", lambda e: e.scalar_tensor_tensor(out=xb[:], in0=ht[:], scalar=ss[:, 0:1], in1=g_bc[:],
                                                             op0=ALU.mult, op1=ALU.mult),
                     reads=[b_ht, b_ss, b_g], writes=[b_xb])
                ptv = pt[:].bitcast(BF16)
                for c in range(8):
                    s.op("pe", lambda e: e.transpose(out=ptv[:, c * 128:(c + 1) * 128], in_=xb[:, c * 128:(c + 1) * 128],
                                                     identity=ident_bf[:]),
                         reads=[b_xb], writes=[b_pt], inc=(c == 7))
                xv = xnT[:, :, tb * 128:(tb + 1) * 128]
                pv = ptv.rearrange("p (c t) -> p c t", c=8)
                s.op("act", lambda e: e.copy(out=xv, in_=pv), reads=[b_pt])

            p1_pend = []
            for tb in range(NTB):
                p1_pend.append(p1_a(tb))
                if len(p1_pend) > 1:
                    p1_b(p1_pend.pop(0))
            while p1_pend:
                p1_b(p1_pend.pop(0))
            s.barrier()

        def proj_fm(w_sb, col0, ncol, nchunk, src, qb, bank, bbank, src_cols=None):
            for c in range(nchunk):
                lw = w_sb[:, c, col0:col0 + ncol]
                rr = src[:, c, qb * 512:(qb + 1) * 512]
                s.op("pe", lambda e: e.matmul(bank[0:ncol, :], lhsT=lw, rhs=rr, start=(c == 0), stop=(c == nchunk - 1)),
                     writes=[bbank], inc=(c == nchunk - 1))

        def load_w_cast(dst, src_ap, wbuf):
            s.dma("pool", lambda e: e.dma_start(out=dst, in_=src_ap), writes=[wbuf])

        hw_rr = [0]

        def load_w_hw(dst, src_ap, wbuf, stg_rot, ncol):
            st_, b_st = stg_rot.next()
            s.dma("sp", lambda e: e.dma_start(out=st_[:, 0:ncol], in_=src_ap), writes=[b_st])
            hw_rr[0] ^= 1
            if hw_rr[0]:
                s.op("act", lambda e: e.copy(out=dst, in_=st_[:, 0:ncol]), reads=[b_st], writes=[wbuf])
            else:
                s.op("dve", lambda e: e.tensor_copy(out=dst, in_=st_[:, 0:ncol]), reads=[b_st], writes=[wbuf])

        with ExitStack() as ps:
            b_w = Buf()
            QT = T(f"QTa{l}", [128, 2, S], BF16, ps)
            KT = T(f"KTa{l}", [128, 4, S], BF16, ps)
            Va = T(f"Va{l}", [128, NTB, 4, 65], BF16, ps)
            s.op("pool", lambda e: e.memset(Va[:, :, :, 64:65], 1.0), writes=[b_w])
            s.op("pool", lambda e: e.memset(KT[:], 0.0), writes=[b_w])
            pw = ExitStack()
            w_sb = T(f"w_na{l}", [128, 8, 768], BF16, pw)
            wstg = Rot([(T(f"wsna{l}_{i}", [128, 768], F32, pw), Buf()) for i in range(2)])
            for c in range(8):
                load_w_hw(w_sb[:, c, :], w_na_d[l, c * 128:(c + 1) * 128, :], b_w, wstg, 768)
            s.barrier()
            pbk = Rot([(banks[i], bbuf[i]) for i in range(4)])
            for g in range(4):
                for qb in range(NQB):
                    bank, bb = pbk.next()
                    proj_fm(w_sb, g * 128, 128, 8, xnT, qb, bank, bb)
                    if g < 2:
                        evac_copy(QT[:, g, qb * 512:(qb + 1) * 512], bank[:, :], [bb], [], scale=0.125)
                    else:
                        for hf in range(2):
                            rr = slice(hf * 64, (hf + 1) * 64)
                            evac_copy(KT[rr, (g - 2) * 2 + hf, qb * 512:(qb + 1) * 512], bank[rr, :], [bb], [])
            for tb in range(NTB):
                bank, bb = pbk.next()
                for c in range(8):
                    s.op("pe", lambda e: e.matmul(bank[:, 0:256], lhsT=xnT[:, c, tb * 128:(tb + 1) * 128],
                                                  rhs=w_sb[:, c, 512:768], start=(c == 0), stop=(c == 7)),
                         writes=[bb], inc=(c == 7))
                evac_copy(Va[:, tb, :, 0:64], bank[:, 0:256].rearrange("p (h d) -> p h d", h=4), [bb], [])
            s.barrier()
            pw.close()
            sbk = Rot([(banks[i], bbuf[i]) for i in range(4)])
            obk = Rot([(banks[4 + i], bbuf[4 + i]) for i in range(2)])
            pTs = Rot([(T(f"pTa{l}_{i}", [128, 320], BF16, ps), Buf()) for i in range(4)])
            fin = Rot([(T(f"rdenA{l}_{i}", [65, 1024], F32, ps), Buf(), banks[6 + i], bbuf[6 + i],
                        T(f"bcsA{l}_{i}", [64, 512], F32, ps), Buf(),
                        T(f"obfA{l}_{i}", [64, 512], BF16, ps), Buf()) for i in range(2)])
            pend = []

            def na_back(ctx):
                hh_, ri_, r8_, nb_, start_, O_bank, O_buf, pT, b_pT, fin_t = ctx
                flush_fin()
                for b in range(nb_):
                    kb = (start_ * 64 + b * 128) // 128
                    s.op("pe", lambda e: e.matmul(O_bank[0:65, ri_ * 64:(ri_ + 1) * 64],
                                                  lhsT=Va[:, kb, hh_, :], rhs=pT[:, b * 64:(b + 1) * 64],
                                                  start=(b == 0), stop=(b == nb_ - 1)),
                         reads=[b_pT], writes=[O_buf], inc=(b == nb_ - 1))
                if ri_ == 7:
                    finalize_heads(O_bank, O_buf, hh_, r8_ * 512, fin_t, use_act=True)

            for hh in range(4):
                t, pb = hh // 2, (hh % 2) * 64
                for r8 in range(8):
                    O_bank, O_buf = obk.next()
                    fin_t = fin.next()
                    for ri in range(8):
                        r = r8 * 8 + ri
                        rs, start, nb = _na_row_info(r)
                        v = NA_VMAP[r]
                        w = nb * 64
                        S_bank, S_buf = sbk.next()
                        pT, b_pT = pTs.next()
                        s.op("pe", lambda e: e.matmul(S_bank[:, 0:w], lhsT=ident_bf[:], rhs=rpb_sb[:, v * 4 + hh, 0:w],
                                                      start=True, stop=False), writes=[S_buf], inc=False)
                        for b in range(nb):
                            k0 = start * 64 + b * 128
                            s.op("pe", lambda e: e.matmul(S_bank[:, b * 64:(b + 1) * 64],
                                                          lhsT=KT[:, hh, k0:k0 + 128],
                                                          rhs=QT[:, t, r * 64:(r + 1) * 64],
                                                          start=False, stop=(b == nb - 1)),
                                 writes=[S_buf], inc=(b == nb - 1))
                        s.op("act", lambda e: e.activation(out=pT[:, 0:w], in_=S_bank[:, 0:w], func=AF.Exp),
                             reads=[S_buf], writes=[b_pT])
                        pend.append((hh, ri, r8, nb, start, O_bank, O_buf, pT, b_pT, fin_t))
                        if len(pend) > 3:
                            na_back(pend.pop(0))
            while pend:
                na_back(pend.pop(0))
            flush_fin(force=True)
            s.barrier()
        nst.close()
        if stop_after == "na":
            lst.close(); lat.close()
            break

        with ExitStack() as ps:
            wq = T(f"wq{l}", [128, 8, 384], BF16, ps)
            wqp = T(f"wqp{l}", [128, 8, 384], BF16, ps)
            wk = T(f"wk{l}", [128, 8, 128], BF16, ps)
            wkp = T(f"wkp{l}", [128, 8, 128], BF16, ps)
            wv = T(f"wv{l}", [128, 8, 128], BF16, ps)
            b_w = Buf()
            tabs = Rot([(T(f"cosc{l}_{i}", [128, 512], F32, ps), T(f"sinc{l}_{i}", [128, 512], F32, ps), Buf()) for i in range(2)])
            msk = T(f"msk{l}", [128, 6, 512], BF16, ps)
            s.dma("sp", lambda e: e.dma_start(out=msk[:], in_=swa_mask_d.rearrange("j p c -> p j c")), writes=[b_w])
            esink = T(f"esink{l}", [65, 6], F32, ps)
            s.dma("sp", lambda e: e.dma_start(out=esink[64:65, :], in_=sink_d[l:l + 1, :]), writes=[b_w])
            QT = T(f"QTc{l}", [128, 3, S], BF16, ps)
            KT = T(f"KTc{l}", [128, 2, S], BF16, ps)
            s.op("pool", lambda e: e.memset(KT[:], 0.0), writes=[b_w])
            Vc = T(f"Vc{l}", [128, NTB, 2, 65], BF16, ps)
            s.op("pool", lambda e: e.memset(Vc[:, :, :, 64:65], 1.0), writes=[b_w])
            pj = ExitStack()
            wstg = Rot([(T(f"wssw{l}_{i}", [128, 384], F32, pj), Buf()) for i in range(2)])
            for dst, srcw, ncol in ((wq, w_cq_d, 384), (wqp, w_cqp_d, 384), (wk, w_ck_d, 128), (wkp, w_ckp_d, 128), (wv, w_cv_d, 128)):
                for c in range(8):
                    load_w_hw(dst[:, c, :], srcw[l, c * 128:(c + 1) * 128, :], b_w, wstg, ncol)
            s.barrier()
            s.op("act", lambda e: e.activation(out=esink[64:65, :], in_=esink[64:65, :], func=AF.Exp))
            pbk = Rot([(banks[2 * i], bbuf[2 * i], banks[2 * i + 1], bbuf[2 * i + 1]) for i in range(4)])
            t1s = Rot([(T(f"t1c{l}_{i}", [128, 512], F32, pj), Buf()) for i in range(2)])
            t2s = Rot([(T(f"t2c{l}_{i}", [128, 512], F32, pj), Buf()) for i in range(2)])
            for g in range(4):
                for qb in range(NQB):
                    bq, bbq, bp, bbp = pbk.next()
                    if g < 3:
                        proj_fm(wq, g * 128, 128, 8, xnT, qb, bq, bbq)
                        proj_fm(wqp, g * 128, 128, 8, xnT, qb, bp, bbp)
                        dst = QT[:, g, qb * 512:(qb + 1) * 512]
                    else:
                        proj_fm(wk, 0, 128, 8, xnT, qb, bq, bbq)
                        proj_fm(wkp, 0, 128, 8, xnT, qb, bp, bbp)
                        dst = None
                    t1, b_t1 = t1s.next()
                    t2, b_t2 = t2s.next()
                    ctab, stab, b_tab = tabs.next()
                    s.dma("sp", lambda e: e.dma_start(out=ctab[:], in_=cs_c_d[:, qb * 512:(qb + 1) * 512]), writes=[b_tab])
                    s.dma("sp", lambda e: e.dma_start(out=stab[:], in_=sn_c_d[:, qb * 512:(qb + 1) * 512]), writes=[b_tab])
                    s.op("dve", lambda e: e.tensor_tensor(out=t1[:], in0=bq[:, :], in1=ctab[:], op=ALU.mult), reads=[bbq, b_tab], writes=[b_t1])
                    s.op("dve", lambda e: e.tensor_tensor(out=t2[:], in0=bp[:, :], in1=stab[:], op=ALU.mult), reads=[bbp, b_tab], writes=[b_t2])
                    if g < 3:
                        s.op("pool", lambda e: e.tensor_tensor(out=t1[:], in0=t1[:], in1=t2[:], op=ALU.add), reads=[b_t1, b_t2], writes=[b_t1])
                        s.op("act", lambda e: e.mul(out=dst, in_=t1[:], mul=0.125), reads=[b_t1])
                    else:
                        for kv_ in range(2):
                            rr = slice(kv_ * 64, (kv_ + 1) * 64)
                            s.op("pool", lambda e: e.tensor_tensor(out=KT[rr, kv_, qb * 512:(qb + 1) * 512], in0=t1[rr, :], in1=t2[rr, :], op=ALU.add),
                                 reads=[b_t1, b_t2])
            pb2 = Rot([(banks[i], bbuf[i]) for i in range(4)])
            for tb in range(NTB):
                bank, bb = pb2.next()
                for c in range(8):
                    s.op("pe", lambda e: e.matmul(bank[:, 0:128], lhsT=xnT[:, c, tb * 128:(tb + 1) * 128],
                                                  rhs=wv[:, c, :], start=(c == 0), stop=(c == 7)),
                         writes=[bb], inc=(c == 7))
                evac_copy(Vc[:, tb, :, 0:64], bank[:, 0:128].rearrange("p (h d) -> p h d", h=2), [bb], [])
            s.barrier()
            pj.close()
            spairs = Rot([(pp[0], [bbuf[0], bbuf[1]]), (pp[1], [bbuf[2], bbuf[3]]), (pp[2], [bbuf[4], bbuf[5]])])
            obk = Rot([(banks[6 + i], bbuf[6 + i]) for i in range(2)])
            pTs = Rot([(T(f"pTc{l}_{i}", [128, 1024], BF16, ps), Buf()) for i in range(4)])
            fin = Rot([(T(f"rdenC{l}_{i}", [65, 1024], F32, ps), Buf(),
                        T(f"bcsC{l}_{i}", [64, 512], F32, ps), Buf(),
                        T(f"obfC{l}_{i}", [64, 512], BF16, ps), Buf()) for i in range(2)])
            pend = []
            fin_age[0] = 1

            def swa_back(ctx):
                hh_, qb_, grp, first, last, O_bank, O_buf, pT, b_pT, fin_t = ctx
                flush_fin()
                kvh_ = hh_ // 3
                for u, kb in enumerate(grp):
                    s.op("pe", lambda e: e.matmul(O_bank[0:65, :], lhsT=Vc[:, kb, kvh_, :], rhs=pT[:, u * 512:(u + 1) * 512],
                                                  start=(first and u == 0), stop=(last and u == len(grp) - 1)),
                         reads=[b_pT], writes=[O_buf], inc=(u == len(grp) - 1))
                if last:
                    f_ = fin_t
                    finalize_heads(O_bank, O_buf, 10 + hh_, qb_ * 512, (f_[0], f_[1], None, None, f_[2], f_[3], f_[4], f_[5]),
                                   extra_den=esink[64:65, hh_:hh_ + 1], bank_src=spairs, use_act=True)

            for hh in range(6):
                kvh = hh // 3
                t = hh % 3
                pb = kvh * 64
                for qb in range(NQB):
                    O_bank, O_buf = obk.next()
                    fin_t = fin.next()
                    kbs = [kb for kb in range(4 * qb - 1, 4 * qb + 5) if 0 <= kb < NTB]
                    grps = [kbs[i:i + 2] for i in range(0, len(kbs), 2)]
                    for gi, grp in enumerate(grps):
                        Sp, b_Sp = spairs.next()
                        pT, b_pT = pTs.next()
                        for u, kb in enumerate(grp):
                            j = kb - (4 * qb - 1)
                            s.op("pe", lambda e: e.matmul(Sp[:, u * 512:(u + 1) * 512], lhsT=KT[:, kvh, kb * 128:(kb + 1) * 128],
                                                          rhs=QT[:, t, qb * 512:(qb + 1) * 512],
                                                          start=True, stop=True), writes=b_Sp, inc=(u == len(grp) - 1))
                        wdt = len(grp) * 512
                        j0 = grp[0] - (4 * qb - 1)
                        s.op("act", lambda e: e.activation(out=pT[:, 0:wdt], in_=Sp[:, 0:wdt], func=AF.Exp),
                             reads=b_Sp, writes=[b_pT])
                        s.op("dve", lambda e: e.tensor_tensor(out=pT[:, 0:wdt], in0=pT[:, 0:wdt],
                                                              in1=msk[:, j0:j0 + len(grp), :].rearrange("p j c -> p (j c)"), op=ALU.mult),
                             reads=[b_pT], writes=[b_pT])
                        pend.append((hh, qb, grp, gi == 0, gi == len(grps) - 1, O_bank, O_buf, pT, b_pT, fin_t))
                        if len(pend) > 2:
                            swa_back(pend.pop(0))
            while pend:
                swa_back(pend.pop(0))
            flush_fin(force=True)
            fin_age[0] = 3
            s.barrier()
        if stop_after == "swa":
            lst.close(); lat.close()
            break

        with ExitStack() as ps:
            cosB = T(f"cosb{l}", [96, S], F32, ps)
            sinB = T(f"sinb{l}", [96, S], F32, ps)
            wl = T(f"wl{l}", [128, 8, 384], BF16, ps)
            wkr = T(f"wkr{l}", [128, 8, 96], BF16, ps)
            wkrp = T(f"wkrp{l}", [128, 8, 96], BF16, ps)
            qn_t = T(f"qn{l}", [128, 2], F32, ps)
            kvn_t = T(f"kvn{l}", [128, 1], F32, ps)
            b_w = Buf()
            load_w_cast(wl[:], w_lat_d[l].rearrange("(c p) n -> p c n", p=128), b_w)
            load_w_cast(wkr[:], w_kr_d[l].rearrange("(c p) n -> p c n", p=128), b_w)
            load_w_cast(wkrp[:], w_krp_d[l].rearrange("(c p) n -> p c n", p=128), b_w)
            s.dma("sp", lambda e: e.dma_start(out=qn_t[:], in_=qn_d[l].rearrange("(c p) -> p c", p=128), allow_slow_non_contiguous=True), writes=[b_w])
            s.dma("sp", lambda e: e.dma_start(out=kvn_t[:], in_=kvn_d[l].rearrange("(c p) -> p c", p=128), allow_slow_non_contiguous=True), writes=[b_w])
            s.dma("sp", lambda e: e.dma_start(out=cosB[64:96, :], in_=cs_b_d), writes=[b_w])
            s.dma("sp", lambda e: e.dma_start(out=sinB[64:96, :], in_=sn_b_d), writes=[b_w])
            s.barrier()
            rb = Rot([(banks[i], bbuf[i]) for i in range(3)])
            sqs = Rot([(T(f"sq{l}_{i}", [128, 512], BF16, ps), Buf()) for i in range(3)])
            rsb = Rot([(T(f"rsb{l}_{i}", [128, 512], F32, ps), Buf()) for i in range(2)])
            rscr = T(f"rscr{l}", [128, 512], F32, ps)
            ssb = Rot([(banks[3 + i], bbuf[3 + i]) for i in range(2)])
            for qb in range(NQB):
                cols = slice(qb * 512, (qb + 1) * 512)
                raws = []
                for c2 in range(2):
                    bank, bb = rb.next()
                    proj_fm(wl, c2 * 128, 128, 8, xnT, qb, bank, bb)
                    sq, b_sq = sqs.next()
                    s.op("act", lambda e: e.activation(out=sq[:], in_=bank[:, :], func=AF.Square), reads=[bb], writes=[b_sq])
                    raws.append((bank, bb, sq, b_sq))
                ss_bank, ss_buf = ssb.next()
                for c2 in range(2):
                    s.op("pe", lambda e: e.matmul(ss_bank[:, :], lhsT=ones_bf[:], rhs=raws[c2][2][:], start=(c2 == 0), stop=(c2 == 1)),
                         reads=[raws[c2][3]], writes=[ss_buf], inc=(c2 == 1))
                rs_t, b_rs = rsb.next()
                rstd_big(ss_bank[:, :], rs_t[:], rscr[:], 1.0 / 256, [ss_buf], [b_rs])
                for c2 in range(2):
                    bank, bb = raws[c2][0], raws[c2][1]
                    s.op("dve", lambda e: e.scalar_tensor_tensor(out=cqn[:, c2, cols], in0=bank[:, :], scalar=qn_t[:, c2:c2 + 1],
                                                                 in1=rs_t[:], op0=ALU.mult, op1=ALU.mult), reads=[bb, b_rs])
                bank, bb = rb.next()
                proj_fm(wl, 256, 128, 8, xnT, qb, bank, bb)
                sq, b_sq = sqs.next()
                s.op("act", lambda e: e.activation(out=sq[:], in_=bank[:, :], func=AF.Square), reads=[bb], writes=[b_sq])
                ss_bank, ss_buf = ssb.next()
                s.op("pe", lambda e: e.matmul(ss_bank[:, :], lhsT=ones_bf[:], rhs=sq[:], start=True, stop=True), reads=[b_sq], writes=[ss_buf])
                rs_t, b_rs = rsb.next()
                rstd_big(ss_bank[:, :], rs_t[:], rscr[:], 1.0 / 128, [ss_buf], [b_rs])
                s.op("dve", lambda e: e.scalar_tensor_tensor(out=ckvn[:, cols], in0=bank[:, :], scalar=kvn_t[:, 0:1],
                                                             in1=rs_t[:], op0=ALU.mult, op1=ALU.mult), reads=[bb, b_rs])
                bank, bb = rb.next()
                proj_fm(wkr, 0, 96, 8, xnT, qb, bank, bb)
                bank2, bb2 = rb.next()
                proj_fm(wkrp, 0, 96, 8, xnT, qb, bank2, bb2)
                t1, b_t1 = rsb.next()
                t2, b_t2 = rsb.next()
                s.op("dve", lambda e: e.tensor_tensor(out=t1[64:96, :], in0=bank[64:96, :], in1=cosB[64:96, cols], op=ALU.mult), reads=[bb], writes=[b_t1])
                s.op("dve", lambda e: e.tensor_tensor(out=t2[64:96, :], in0=bank2[64:96, :], in1=sinB[64:96, cols], op=ALU.mult), reads=[bb2], writes=[b_t2])
                s.op("pool", lambda e: e.tensor_tensor(out=kpe[64:96, cols], in0=t1[64:96, :], in1=t2[64:96, :], op=ALU.add), reads=[b_t1, b_t2])
            s.barrier()
        lst.close()
        with ExitStack() as ps:
            Vb = T(f"Vb{l}", [128, NTB, 6, 65], BF16, ps)
            cosB = T(f"cosb2{l}", [96, S], F32, ps)
            sinB = T(f"sinb2{l}", [96, S], F32, ps)
            wuq = T(f"wuq{l}", [128, 2, 576], BF16, ps)
            wuqp = T(f"wuqp{l}", [128, 2, 576], BF16, ps)
            wuk = T(f"wuk{l}", [128, 384], BF16, ps)
            wuv = T(f"wuv{l}", [128, 384], BF16, ps)
            b_w = Buf()
            load_w_cast(wuq[:], w_uq_d[l].rearrange("(c p) n -> p c n", p=128), b_w)
            load_w_cast(wuqp[:], w_uqp_d[l].rearrange("(c p) n -> p c n", p=128), b_w)
            load_w_cast(wuk[:], w_uk_d[l], b_w)
            load_w_cast(wuv[:], w_uv_d[l], b_w)
            s.dma("sp", lambda e: e.dma_start(out=cosB[64:96, :], in_=cs_b_d), writes=[b_w])
            s.dma("sp", lambda e: e.dma_start(out=sinB[64:96, :], in_=sn_b_d), writes=[b_w])
            s.op("pool", lambda e: e.memset(Vb[:, :, :, 64:65], 1.0), writes=[b_w])
            s.barrier()
            pb2 = Rot([(banks[i], bbuf[i]) for i in range(4)])
            for tb in range(NTB):
                bank, bb = pb2.next()
                s.op("pe", lambda e: e.matmul(bank[:, 0:384], lhsT=ckvn[:, tb * 128:(tb + 1) * 128], rhs=wuv[:, :], start=True, stop=True), writes=[bb])
                evac_copy(Vb[:, tb, :, 0:64], bank[:, 0:384].rearrange("p (h d) -> p h d", h=6), [bb], [])
            s.barrier()
            scale_b = float(96 ** -0.5)
            QTs = [(T(f"QTb{l}_{i}", [96, S], BF16, ps), Buf()) for i in range(2)]
            KTs = [(T(f"KTb{l}_{i}", [96, S], BF16, ps), Buf()) for i in range(2)]
            t1s = Rot([(T(f"t1b{l}_{i}", [96, 512], F32, ps), Buf()) for i in range(2)])
            t2s = Rot([(T(f"t2b{l}_{i}", [96, 512], F32, ps), Buf()) for i in range(2)])
            pbk = Rot([(banks[i], bbuf[i]) for i in range(3)])
            spairs = Rot([(pp[0], [bbuf[0], bbuf[1]]), (pp[1], [bbuf[2], bbuf[3]]), (pp[2], [bbuf[4], bbuf[5]])])
            obk = Rot([(banks[6 + i], bbuf[6 + i]) for i in range(2)])
            pTs = Rot([(T(f"pTb{l}_{i}", [128, 1024], BF16, ps), Buf()) for i in range(4)])
            finB = Rot([(T(f"rdenB{l}_{i}", [65, 1024], F32, ps), Buf(),
                         T(f"bcsB{l}_{i}", [64, 512], F32, ps), Buf(),
                         T(f"obfB{l}_{i}", [64, 512], BF16, ps), Buf()) for i in range(2)])
            pend = []

            def mla_back(ctx):
                hh_, qb_, kp_, O_bank, O_buf, pT, b_pT, fin_t = ctx
                flush_fin()
                for u in range(2):
                    kb = 2 * kp_ + u
                    s.op("pe", lambda e: e.matmul(O_bank[0:65, :], lhsT=Vb[:, kb, hh_, :], rhs=pT[:, u * 512:(u + 1) * 512],
                                                  start=(kb == 0), stop=(kb == NTB - 1)),
                         reads=[b_pT], writes=[O_buf], inc=(u == 1))
                if kp_ == NTB // 2 - 1:
                    f_ = fin_t
                    finalize_heads(O_bank, O_buf, 4 + hh_, qb_ * 512, (f_[0], f_[1], None, None, f_[2], f_[3], f_[4], f_[5]), bank_src=spairs)

            def proj_step(hp, qb):
                QTh, b_Q = QTs[hp % 2]
                KTh, b_K = KTs[hp % 2]
                cols = slice(qb * 512, (qb + 1) * 512)
                Sp1, bS1 = spairs.next()
                bq, bbq = Sp1[:, 0:512], bS1[0]
                bp, bbp = Sp1[:, 512:1024], bS1[1]
                proj_fm(wuq, hp * 96, 96, 2, cqn, qb, bq, bbq)
                proj_fm(wuqp, hp * 96, 96, 2, cqn, qb, bp, bbp)
                s.op("dve", lambda e: e.tensor_copy(out=QTh[0:64, cols], in_=bq[0:64, :]), reads=[bbq], writes=[b_Q])
                t1, b_t1 = t1s.next()
                t2, b_t2 = t2s.next()
                s.op("dve", lambda e: e.tensor_tensor(out=t1[64:96, :], in0=bq[64:96, :], in1=cosB[64:96, cols], op=ALU.mult), reads=[bbq], writes=[b_t1])
                s.op("dve", lambda e: e.tensor_tensor(out=t2[64:96, :], in0=bp[64:96, :], in1=sinB[64:96, cols], op=ALU.mult), reads=[bbp], writes=[b_t2])
                s.op("pool", lambda e: e.tensor_tensor(out=QTh[64:96, cols], in0=t1[64:96, :], in1=t2[64:96, :], op=ALU.add),
                     reads=[b_t1, b_t2], writes=[b_Q])
                Sp2, bS2 = spairs.next()
                bk, bbk = Sp2[:, 0:512], bS2[0]
                s.op("pe", lambda e: e.matmul(bk[0:64, :], lhsT=wuk[:, hp * 64:(hp + 1) * 64], rhs=ckvn[:, cols], start=True, stop=True), writes=[bbk])
                s.op("dve", lambda e: e.tensor_copy(out=KTh[0:64, cols], in_=bk[0:64, :]), reads=[bbk], writes=[b_K])
                s.op("pool", lambda e: e.tensor_copy(out=KTh[64:96, cols], in_=kpe[64:96, cols]), writes=[b_K])

            for qb in range(NQB):
                proj_step(0, qb)
            for hh in range(6):
                QTh, b_Q = QTs[hh % 2]
                KTh, b_K = KTs[hh % 2]
                for qb in range(NQB):
                    if hh + 1 < 6:
                        proj_step(hh + 1, qb)
                    O_bank, O_buf = obk.next()
                    fin_t = finB.next()
                    for kp in range(NTB // 2):
                        Sp, b_Sp = spairs.next()
                        pT, b_pT = pTs.next()
                        for u in range(2):
                            kb = 2 * kp + u
                            s.op("pe", lambda e: e.matmul(Sp[:, u * 512:(u + 1) * 512], lhsT=KTh[0:96, kb * 128:(kb + 1) * 128],
                                                          rhs=QTh[0:96, qb * 512:(qb + 1) * 512], start=True, stop=True),
                                 reads=[b_Q, b_K], writes=b_Sp, inc=(u == 1))
                        s.op("act", lambda e: e.activation(out=pT[:], in_=Sp[:, :], func=AF.Exp, scale=scale_b),
                             reads=b_Sp, writes=[b_pT])
                        pend.append((hh, qb, kp, O_bank, O_buf, pT, b_pT, fin_t))
                        if len(pend) > 2:
                            mla_back(pend.pop(0))
            while pend:
                mla_back(pend.pop(0))
            flush_fin(force=True)
            s.barrier()
        lat.close()
        if stop_after == "mla":
            break

        sel = ExitStack()
        aff_all = T(f"aff{l}", [128, NTB, NE], F32, sel)
        affT = T(f"affT{l}", [128, 512], F32, sel)
        idsT = T(f"idsT{l}", [128, 64], U32, sel)
        gT = T(f"gT{l}", [128, 64], F32, sel)
        with ExitStack() as ps:
            Wo = T(f"Wo{l}", [128, 8, D], BF16, ps)
            gn_t = T(f"gn{l}", [128, 8], F32, ps)
            g2_bc = T(f"g2{l}", [128, D], F32, ps)
            wr_sb = T(f"wr{l}", [128, 8, NE], F32, ps)
            invw = T(f"invw{l}", [128, 3], F32, ps)
            b_w = Buf()
            s.dma("sp", lambda e: e.dma_start(out=gn_t[:], in_=gn_d[l].rearrange("(j p) -> p j", p=128), allow_slow_non_contiguous=True), writes=[b_w])
            s.dma("sp", lambda e: e.dma_start(out=g2_bc[:], in_=ffn_norm_d[l:l + 1, :].partition_broadcast(128)), writes=[b_w])
            s.dma("sp", lambda e: e.dma_start(out=wr_sb[:], in_=w_router_d[l].rearrange("(c p) n -> p c n", p=128)), writes=[b_w])
            s.dma("sp", lambda e: e.dma_start(out=invw[:], in_=invw_d), writes=[b_w])
            stg = Rot([(T(f"wstg{l}_{i}", [128, D], F32, ps), Buf()) for i in range(2)])
            for j in range(8):
                st_, b_st = stg.next()
                s.dma("sp", lambda e: e.dma_start(out=st_[:], in_=w_out_d[l, j * 128:(j + 1) * 128, :]), writes=[b_st])
                s.op("dve", lambda e: e.tensor_scalar(out=Wo[:, j, :], in0=st_[:], scalar1=gn_t[:, j:j + 1], scalar2=None, op0=ALU.mult),
                     reads=[b_st, b_w])
            Zs = [(T(f"Z{l}_{i}", [128, 128], F32, ps), Buf()) for i in range(8)]
            for z, bz in Zs:
                s.op("pool", lambda e: e.memset(z[:], 0.0), writes=[bz])
            s.barrier()
            ocs = Rot([(T(f"oc{l}_{i}", [128, 8, 512], BF16, ps), Buf()) for i in range(2)])
            osq = T(f"osq{l}", [128, 8, 512], BF16, ps)
            b_osq = Buf()
            hts = Rot([(T(f"h3_{l}_{i}", [128, D], F32, ps), Buf()) for i in range(3)])
            h1s = Rot([(T(f"h1_{l}_{i}", [128, D], F32, ps), Buf()) for i in range(4)])
            xfs = Rot([(T(f"xf_{l}_{i}", [128, D], F32, ps), Buf()) for i in range(3)])
            xbs = Rot([(T(f"xb3_{l}_{i}", [128, D], BF16, ps), Buf()) for i in range(2)])
            xTs = Rot([(T(f"xT3_{l}_{i}", [128, 8, 128], F32, ps), Buf()) for i in range(2)])
            junk = T(f"junk3_{l}", [128, D], BF16, ps)
            b_junk = Buf()
            smalls = Rot([(T(f"sm{l}_{i}", [128, 8], F32, ps), Buf()) for i in range(6)])
            lgs = Rot([(T(f"lg{l}_{i}", [128, NE], F32, ps), Buf()) for i in range(2)])
            mix_heads = [[0, 1], [2, 3, 4], [5, 6, 7]]
            obk = Rot([(banks[0], bbuf[0], banks[1], bbuf[1]), (banks[2], bbuf[2], banks[3], bbuf[3])])
            ssb = Rot([(banks[4], bbuf[4])])
            tpk = Rot([(banks[5], bbuf[5], banks[6], bbuf[6])])
            lgb = Rot([(banks[7], bbuf[7])])
            p3_xf = {}

            def p3_stage_b1(tb, h1, b_h1, sm, b_sm):
                s.op("dve", lambda e: e.scalar_tensor_tensor(out=junk[:], in0=h1[:], scalar=1.0, in1=h1[:],
                                                             op0=ALU.mult, op1=ALU.mult, accum_out=sm[:, 3:4]),
                     reads=[b_h1], writes=[b_junk, b_sm])
                s.op("dve", lambda e: e.tensor_scalar(out=sm[:, 3:4], in0=sm[:, 3:4], scalar1=1.0 / D, scalar2=EPS, op0=ALU.mult, op1=ALU.add),
                     reads=[b_sm], writes=[b_sm])
                s.op("act", lambda e: e.activation(out=sm[:, 3:4], in_=sm[:, 3:4], func=AF.Ln), reads=[b_sm], writes=[b_sm])
                s.op("act", lambda e: e.activation(out=sm[:, 3:4], in_=sm[:, 3:4], func=AF.Exp, scale=-0.5), reads=[b_sm], writes=[b_sm])
                xf, b_xf = xfs.next()
                xb, b_xb = xbs.next()
                s.op("dve", lambda e: e.scalar_tensor_tensor(out=xf[:], in0=h1[:], scalar=sm[:, 3:4], in1=g2_bc[:],
                                                             op0=ALU.mult, op1=ALU.mult), reads=[b_h1, b_sm], writes=[b_xf])
                s.op("act", lambda e: e.copy(out=xb[:], in_=xf[:]), reads=[b_xf], writes=[b_xb])
                s.dma("pool", lambda e: e.dma_start(out=xn2_d[tb * 128:(tb + 1) * 128, :], in_=xb[:]), reads=[b_xb])
                p3_xf[tb] = (xf, b_xf)

            def p3_stage_b2(tb, h1, b_h1, sm, b_sm):
                xf, b_xf = p3_xf.pop(tb)
                t0, bt0, t1_, bt1 = tpk.next()
                for c in range(8):
                    bk_, bbk_ = (t0, bt0) if c < 4 else (t1_, bt1)
                    s.op("pe", lambda e: e.transpose(out=bk_[:, (c % 4) * 128:(c % 4 + 1) * 128], in_=xf[:, c * 128:(c + 1) * 128], identity=ident_f[:]),
                         reads=[b_xf], writes=[bbk_], inc=(c % 4 == 3))
                xT, b_xT = xTs.next()
                s.op("act", lambda e: e.copy(out=xT[:, 0:4, :], in_=t0[:, :].rearrange("p (c t) -> p c t", c=4)), reads=[bt0], writes=[b_xT])
                s.op("act", lambda e: e.copy(out=xT[:, 4:8, :], in_=t1_[:, :].rearrange("p (c t) -> p c t", c=4)), reads=[bt1], writes=[b_xT])
                lb, blb = lgb.next()
                for c in range(8):
                    s.op("pe", lambda e: e.matmul(lb[:, 0:NE], lhsT=xT[:, c, :], rhs=wr_sb[:, c, :], start=(c == 0), stop=(c == 7)),
                         reads=[b_xT], writes=[blb], inc=(c == 7))
                lg, b_lg = lgs.next()
                s.op("dve", lambda e: e.reduce_max(out=sm[:, 4:5], in_=lb[:, 0:NE], axis=mybir.AxisListType.X), reads=[blb], writes=[b_sm])
                s.op("dve", lambda e: e.tensor_scalar(out=sm[:, 4:5], in0=sm[:, 4:5], scalar1=-1.0, scalar2=None, op0=ALU.mult), reads=[b_sm], writes=[b_sm])
                s.op("act", lambda e: e.activation(out=lg[:], in_=lb[:, 0:NE], func=AF.Exp, bias=sm[:, 4:5], accum_out=sm[:, 5:6]),
                     reads=[blb, b_sm], writes=[b_lg, b_sm])
                s.op("dve", lambda e: e.reciprocal(out=sm[:, 5:6], in_=sm[:, 5:6]), reads=[b_sm], writes=[b_sm])
                s.op("dve", lambda e: e.tensor_scalar(out=aff_all[:, tb, :], in0=lg[:], scalar1=sm[:, 5:6], scalar2=None, op0=ALU.mult),
                     reads=[b_lg, b_sm])

            p3_pend = []
            for qb in range(NQB):
                oc, b_oc = ocs.next()
                ocv = oc_d.rearrange("(jj two) p t -> two p jj t", two=2)
                for two in range(2):
                    s.dma("sp", lambda e: e.dma_start(out=oc[two * 64:(two + 1) * 64, :, :], in_=ocv[two][:, :, qb * 512:(qb + 1) * 512]), writes=[b_oc])
                s.op("pool", lambda e: e.tensor_tensor(out=osq[:], in0=oc[:], in1=oc[:], op=ALU.mult), reads=[b_oc], writes=[b_osq])
                for sub in range(4):
                    tb = qb * 4 + sub
                    tc_ = slice(sub * 128, (sub + 1) * 128)
                    ht, b_ht = hts.next()
                    s.dma("sp", lambda e: e.dma_start(out=ht[:], in_=src_d[tb * 128:(tb + 1) * 128, :]), writes=[b_ht])
                    ss_bank, ss_buf = ssb.next()
                    for m in range(3):
                        hs = mix_heads[m]
                        for i, j in enumerate(hs):
                            s.op("pe", lambda e: e.matmul(ss_bank[:, m:m + 1], lhsT=osq[:, j, tc_], rhs=ones_bf[:, 0:1],
                                                          start=(i == 0), stop=(i == len(hs) - 1)),
                                 reads=[b_osq], writes=[ss_buf], inc=(i == len(hs) - 1))
                    sm, b_sm = smalls.next()
                    s.op("dve", lambda e: e.tensor_tensor(out=sm[:, 0:3], in0=ss_bank[:, 0:3], in1=invw[:], op=ALU.mult), reads=[ss_buf], writes=[b_sm])
                    s.op("dve", lambda e: e.tensor_scalar(out=sm[:, 0:3], in0=sm[:, 0:3], scalar1=EPS, scalar2=None, op0=ALU.add), reads=[b_sm], writes=[b_sm])
                    s.op("act", lambda e: e.activation(out=sm[:, 0:3], in_=sm[:, 0:3], func=AF.Ln), reads=[b_sm], writes=[b_sm])
                    s.op("act", lambda e: e.activation(out=sm[:, 0:3], in_=sm[:, 0:3], func=AF.Exp, scale=-0.5), reads=[b_sm], writes=[b_sm])
                    if p3_pend:
                        p3_stage_b1(*p3_pend[0])
                    h1, b_h1 = h1s.next()
                    prev, b_prev = ht, b_ht
                    for m in range(3):
                        hs = mix_heads[m]
                        b0, bb0, b1, bb1 = obk.next()
                        for half, (bk_, bbk_) in enumerate(((b0, bb0), (b1, bb1))):
                            for i, j in enumerate(hs):
                                s.op("pe", lambda e: e.matmul(bk_[:, :], lhsT=oc[:, j, tc_], rhs=Wo[:, j, half * 512:(half + 1) * 512],
                                                              start=(i == 0), stop=(i == len(hs) - 1)),
                                     reads=[b_oc], writes=[bbk_], inc=(i == len(hs) - 1))
                            hc = slice(half * 512, (half + 1) * 512)
                            s.op("dve", lambda e: e.scalar_tensor_tensor(out=h1[:, hc], in0=bk_[:, :], scalar=sm[:, m:m + 1], in1=prev[:, hc],
                                                                         op0=ALU.mult, op1=ALU.add),
                                 reads=[bbk_, b_sm, b_prev], writes=[b_h1])
                        prev, b_prev = h1, b_h1
                    s.dma("pool", lambda e: e.dma_start(out=h_d[tb * 128:(tb + 1) * 128, :], in_=h1[:]), reads=[b_h1])
                    if p3_pend:
                        p3_stage_b2(*p3_pend.pop(0))
                    p3_pend.append((tb, h1, b_h1, sm, b_sm))
            while p3_pend:
                p3_stage_b1(*p3_pend[0])
                p3_stage_b2(*p3_pend.pop(0))
            s.barrier()
            for jc in range(4):
                for part in range(8):
                    tb = part * 4 + jc
                    z, bz = Zs[part]
                    zv = z[:].rearrange("p (e q) -> p e q", q=8)[:, :, part:part + 1]
                    s.op("dve", lambda e: e.tensor_copy(out=zv, in_=aff_all[:, tb, :].rearrange("p (e o) -> p e o", o=1)), writes=[bz])
                    s.op("pe", lambda e: e.matmul(banks[0][:, jc * 128:(jc + 1) * 128], lhsT=z[:], rhs=ident_f[:],
                                                  start=(part == 0), stop=(part == 7)), reads=[bz], writes=[bbuf[0]])
            s.op("act", lambda e: e.copy(out=affT[:], in_=banks[0][:, :]), reads=[bbuf[0]])
            s.barrier()
        if stop_after == "p3":
            sel.close()
            break

        wst = ExitStack()
        Ws = [(T(f"Wg{l}_{i}", [128, 8, D], BF16, wst), T(f"Wu{l}_{i}", [128, 8, D], BF16, wst),
               T(f"Wd{l}_{i}", [128, 8, D], BF16, wst), Buf()) for i in range(2)]
        stg = Rot([(T(f"wst{l}_{i}", [128, D], F32, wst), Buf()) for i in range(8)])

        def load_expert_steps(e_):
            Wg, Wu, Wd, b_W = Ws[e_ % 2]
            steps = []
            for dst, srcw in ((Wg, w_gate_d), (Wu, w_up_d), (Wd, w_down_d)):
                for c in range(8):
                    def step(dst=dst, srcw=srcw, c=c):
                        st_, b_st = stg.next()
                        s.dma("sp", lambda e: e.dma_start(out=st_[:], in_=srcw[l, e_, c * 128:(c + 1) * 128, :]), writes=[b_st])
                        s.op("act", lambda e: e.copy(out=dst[:, c, :], in_=st_[:]), reads=[b_st], writes=[b_W])
                    steps.append(step)
            return steps

        def load_expert(e_):
            for st in load_expert_steps(e_):
                st()

        load_expert(0)
        with ExitStack() as ps:
            gmat = T(f"gmat{l}", [128, 128], F32, ps)
            rowoff = T(f"rowoff{l}", [128, 1], F32, ps)
            b_c = Buf()
            s.dma("sp", lambda e: e.dma_start(out=gmat[:], in_=gmat_d), writes=[b_c])
            s.dma("sp", lambda e: e.dma_start(out=rowoff[:], in_=rowoff_d), writes=[b_c])
            mid = T(f"mid{l}", [128, 1], F32, ps)
            lo = T(f"lo{l}", [128, 1], F32, ps)
            cnt = T(f"cnt{l}", [128, 1], F32, ps)
            gef = T(f"gef{l}", [128, 1], F32, ps)
            tt = T(f"tt{l}", [128, 1], F32, ps)
            cmpj = T(f"cmpj{l}", [128, 512], F32, ps)
            am = T(f"am{l}", [128, 512], F32, ps)
            vals = T(f"vals{l}", [128, ROUNDS * 8], F32, ps)
            idxs = T(f"idxs{l}", [128, ROUNDS * 8], mybir.dt.uint16, ps)
            idf = T(f"idf{l}", [128, ROUNDS * 8], F32, ps)
            vmask = T(f"vmask{l}", [128, ROUNDS * 8], F32, ps)
            nrow = T(f"nrow{l}", [128, 1], F32, ps)
            off_sb = T(f"off{l}", [128, 1], F32, ps)
            diag = T(f"diag{l}", [128, 128], F32, ps)
            offB = T(f"offB{l}", [128, 128], F32, ps)
            RT = T(f"RT{l}", [128, ROUNDS * 8 // 128, 128, 4], BF16, ps)
            trimat = T(f"trimat{l}", [128, 128], F32, ps)
            dmat = T(f"dmat{l}", [128, ROUNDS * 8 // 128, 512], mybir.dt.int16, ps)
            s.dma("sp", lambda e: e.dma_start(out=trimat[:], in_=trimat_d), writes=[b_c])
            s.dma("sp", lambda e: e.dma_start(out=dmat[:], in_=dmat_d), writes=[b_c])
            b_s = Buf()
            s.op("dve", lambda e: e.memset(mid[:], 0.5), writes=[b_s])
            s.op("dve", lambda e: e.memset(lo[:], 0.0), writes=[b_s])
            s.barrier()
            step = 0.5
            cb = banks[1]
            b_cb = bbuf[1]
            for it in range(NBIS):
                s.op("dve", lambda e: e.tensor_scalar(out=cmpj[:], in0=affT[:], scalar1=mid[:, 0:1], scalar2=None,
                                                      op0=ALU.is_ge, op1=ALU.add, accum_out=cnt[:]), reads=[b_s], writes=[b_s])
                s.op("pe", lambda e: e.matmul(cb[:, 0:1], lhsT=gmat[:], rhs=cnt[:], start=True, stop=True), reads=[b_s], writes=[b_cb])
                s.op("dve", lambda e: e.tensor_scalar(out=gef[:], in0=cb[:, 0:1], scalar1=511.5, scalar2=None, op0=ALU.is_ge), reads=[b_cb], writes=[b_s])
                s.op("dve", lambda e: e.scalar_tensor_tensor(out=lo[:], in0=gef[:], scalar=mid[:, 0:1], in1=lo[:], op0=ALU.mult, op1=ALU.max),
                     reads=[b_s], writes=[b_s])
                step *= 0.5
                st2 = step
                s.op("dve", lambda e: e.tensor_scalar(out=tt[:], in0=gef[:], scalar1=2.0 * st2, scalar2=-st2, op0=ALU.mult, op1=ALU.add),
                     reads=[b_s], writes=[b_s])
                s.op("dve", lambda e: e.tensor_tensor(out=mid[:], in0=mid[:], in1=tt[:], op=ALU.add), reads=[b_s], writes=[b_s])
            s.op("dve", lambda e: e.scalar_tensor_tensor(out=am[:], in0=affT[:], scalar=lo[:, 0:1], in1=affT[:], op0=ALU.is_ge, op1=ALU.mult),
                 reads=[b_s], writes=[b_s])
            for r in range(ROUNDS):
                sl = slice(r * 8, (r + 1) * 8)
                s.op("dve", lambda e: e.max(out=vals[:, sl], in_=am[:]), reads=[b_s], writes=[b_s])
                s.op("dve", lambda e: e.max_index(out=idxs[:, sl], in_max=vals[:, sl], in_values=am[:]), reads=[b_s], writes=[b_s])
                s.op("dve", lambda e: e.match_replace(out=am[:], in_to_replace=vals[:, sl], in_values=am[:], imm_value=-1.0), reads=[b_s], writes=[b_s])
            NSL = ROUNDS * 8
            NIC = NSL // 128
            U8 = mybir.dt.uint8
            s.op("dve", lambda e: e.tensor_scalar(out=vmask[:], in0=vals[:], scalar1=0.0, scalar2=None, op0=ALU.is_gt, op1=ALU.add, accum_out=nrow[:]),
                 reads=[b_s], writes=[b_s])
            s.op("dve", lambda e: e.tensor_tensor(out=vals[:], in0=vals[:], in1=vmask[:], op=ALU.mult), reads=[b_s], writes=[b_s])
            idx8 = idxs[:].bitcast(U8).rearrange("p (n two) -> p n two", two=2)
            dig = T(f"dig{l}", [128, 4, NSL], BF16, ps)
            tmpf = T(f"tmpf{l}", [128, NSL], F32, ps)
            s.op("dve", lambda e: e.tensor_copy(out=tmpf[:].rearrange("p (n o) -> p n o", o=1), in_=idx8[:, :, 1:2]), reads=[b_s], writes=[b_s])
            s.op("dve", lambda e: e.scalar_tensor_tensor(out=dig[:, 0, :], in0=tmpf[:], scalar=rowoff[:, 0:1], in1=vmask[:], op0=ALU.add, op1=ALU.mult),
                 reads=[b_s, b_c], writes=[b_s])
            s.op("dve", lambda e: e.tensor_copy(out=tmpf[:].rearrange("p (n o) -> p n o", o=1), in_=idx8[:, :, 0:1]), reads=[b_s], writes=[b_s])
            s.op("dve", lambda e: e.tensor_tensor(out=dig[:, 1, :], in0=tmpf[:], in1=vmask[:], op=ALU.mult), reads=[b_s], writes=[b_s])
            s.op("dve", lambda e: e.tensor_copy(out=dig[:, 2, :], in_=vals[:]), reads=[b_s], writes=[b_s])
            s.op("dve", lambda e: e.tensor_tensor(out=dig[:, 3, :], in0=vals[:], in1=dig[:, 2, :], op=ALU.subtract), reads=[b_s], writes=[b_s])
            s.op("pe", lambda e: e.matmul(banks[2][:, 0:1], lhsT=trimat[:], rhs=nrow[:], start=True, stop=True), reads=[b_s, b_c], writes=[bbuf[2]])
            s.op("dve", lambda e: e.tensor_copy(out=off_sb[:], in_=banks[2][:, 0:1]), reads=[bbuf[2]], writes=[b_s])
            s.op("dve", lambda e: e.tensor_scalar(out=diag[:], in0=ident_f[:], scalar1=off_sb[:, 0:1], scalar2=None, op0=ALU.mult), reads=[b_s], writes=[b_s])
            s.op("pe", lambda e: e.matmul(banks[3][:, 0:128], lhsT=ones_f[:], rhs=diag[:], start=True, stop=True), reads=[b_s], writes=[bbuf[3]])
            s.op("act", lambda e: e.copy(out=offB[:], in_=banks[3][:, 0:128]), reads=[bbuf[3]], writes=[b_s])
            tb2 = banks[0][:].bitcast(BF16)
            for ic in range(NIC):
                for k in range(4):
                    col = (ic * 4 + k) * 128
                    s.op("pe", lambda e: e.transpose(out=tb2[:, col:col + 128], in_=dig[:, k, ic * 128:(ic + 1) * 128], identity=ident_bf[:]),
                         reads=[b_s], writes=[bbuf[0]])
                s.op("dve", lambda e: e.tensor_copy(out=RT[:, ic, :, :].rearrange("p r k -> p k r"),
                                                    in_=tb2[:, ic * 512:(ic + 1) * 512].rearrange("p (k r) -> p k r", k=4)), reads=[bbuf[0]], writes=[b_s])
            s.barrier()
            sels = Rot([(T(f"selt{l}_{i}", [128, 512], BF16, ps), Buf()) for i in range(4)])
            for row in range(128):
                e_ = row // 8
                for ic in range(NIC):
                    sel_t, b_sel = sels.next()
                    s.op("dve", lambda e: e.tensor_scalar(out=sel_t[:], in0=dmat[:, ic, :], scalar1=offB[:, row:row + 1], scalar2=None, op0=ALU.is_equal),
                         writes=[b_sel])
                    first = (row % 8 == 0 and ic == 0)
                    last = (row % 8 == 7 and ic == NIC - 1)
                    for jc in range(4):
                        s.op("pe", lambda e: e.matmul(banks[4 + jc][:, e_ * 4:e_ * 4 + 4], lhsT=sel_t[:, jc * 128:(jc + 1) * 128], rhs=RT[:, ic, row, :],
                                                      start=first, stop=last), reads=[b_sel], writes=[bbuf[4 + jc]], inc=(jc == 3))
            rs_sb = T(f"rs_sb{l}", [128, 4, NE, 4], F32, ps)
            b_rs = Buf()
            for jc in range(4):
                s.op("act", lambda e: e.copy(out=rs_sb[:, jc, :, :], in_=banks[4 + jc][:, 0:4 * NE].rearrange("p (e t) -> p e t", t=4)),
                     reads=[bbuf[4 + jc]], writes=[b_rs])
            dg = lambda k: rs_sb[:, :, :, k:k + 1].rearrange("p j e o -> p j (e o)")
            s.op("dve", lambda e: e.scalar_tensor_tensor(out=idsT[:].rearrange("p (e j) -> p j e", j=4), in0=dg(0), scalar=256.0, in1=dg(1),
                                                         op0=ALU.mult, op1=ALU.add), reads=[b_rs])
            s.op("dve", lambda e: e.tensor_tensor(out=gT[:].rearrange("p (e j) -> p j e", j=4), in0=dg(2), in1=dg(3), op=ALU.add), reads=[b_rs])
            if debug:
                s.barrier()
                s.dma("sp", lambda e: e.dma_start(out=aff_dbg, in_=affT[:]))
                s.dma("sp", lambda e: e.dma_start(out=sel_dbg[:, 0:64], in_=idsT[:].bitcast(F32)))
                s.dma("sp", lambda e: e.dma_start(out=sel_dbg[:, 64:128], in_=gT[:]))
            s.barrier()
        if stop_after == "p4":
            s.barrier()
            wst.close()
            sel.close()
            break

        with ExitStack() as ps:
            xss = Rot([(T(f"xs{l}_{i}", [128, D], BF16, ps), Buf()) for i in range(8)])
            xsTs = [(T(f"xsT{l}_{i}", [128, 8, 512], BF16, ps), Buf()) for i in range(2)]
            hidTs = Rot([(T(f"hidT{l}_{i}", [128, 8, 512], BF16, ps), Buf()) for i in range(2)])
            sgs = Rot([(T(f"sg{l}_{i}", [128, 512], F32, ps), Buf()) for i in range(2)])
            ys = Rot([(T(f"y{l}_{i}", [128, D], F32, ps), Buf()) for i in range(2)])
            tpk = Rot([(banks[0], bbuf[0]), (banks[1], bbuf[1])])
            gub = Rot([(banks[2], bbuf[2], banks[3], bbuf[3]), (banks[4], bbuf[4], banks[5], bbuf[5])])
            ybk = Rot([(banks[6], bbuf[6]), (banks[7], bbuf[7])])
            hreg = [[Buf(f"hreg{q}_{p}") for p in range(ROWS_PER_E)] for q in range(2)]
            NCH = NE * SLOT_CHUNKS

            def rows_of(ch):
                e_, half = ch // SLOT_CHUNKS, ch % SLOT_CHUNKS
                return [e_ * ROWS_PER_E + half * 4 + i for i in range(4)]

            gathered = {}

            def issue_gathers(ch):
                lst_ = []
                for r in rows_of(ch):
                    xs, b_xs = xss.next()
                    s.dma("pool", lambda e: e.indirect_dma_start(out=xs[:], out_offset=None, in_=xn2_d,
                                                                 in_offset=bass.IndirectOffsetOnAxis(ap=idsT[:, r:r + 1], axis=0)),
                          writes=[b_xs])
                    lst_.append((xs, b_xs))
                gathered[ch] = lst_

            def do_transposes(ch):
                xsT, b_xsT = xsTs[ch % 2]
                for i, (xs, b_xs) in enumerate(gathered.pop(ch)):
                    pt, b_pt = tpk.next()
                    ptv = pt[:].bitcast(BF16)
                    for c in range(8):
                        s.op("pe", lambda e: e.transpose(out=ptv[:, c * 128:(c + 1) * 128], in_=xs[:, c * 128:(c + 1) * 128], identity=ident_bf[:]),
                             reads=[b_xs], writes=[b_pt], inc=(c == 7))
                    s.op("dve", lambda e: e.tensor_copy(out=xsT[:, :, i * 128:(i + 1) * 128], in_=ptv.rearrange("p (c t) -> p c t", c=8)),
                         reads=[b_pt], writes=[b_xsT])

            issue_gathers(0)
            do_transposes(0)
            for ch in range(NCH):
                e_ = ch // SLOT_CHUNKS
                Wg, Wu, Wd, b_W = Ws[e_ % 2]
                wsteps = load_expert_steps(e_ + 1) if (ch % SLOT_CHUNKS == 0 and e_ + 1 < NE) else []
                if ch + 1 < NCH:
                    issue_gathers(ch + 1)
                xsT, b_xsT = xsTs[ch % 2]
                hidT, b_hid = hidTs.next()
                for f in range(8):
                    for _ in range(3):
                        if wsteps:
                            wsteps.pop(0)()
                    gb, bgb, ub, bub = gub.next()
                    for c in range(8):
                        s.op("pe", lambda e: e.matmul(gb[:, :], lhsT=Wg[:, c, f * 128:(f + 1) * 128], rhs=xsT[:, c, :], start=(c == 0), stop=(c == 7)),
                             reads=[b_W, b_xsT], writes=[bgb], inc=(c == 7))
                    for c in range(8):
                        s.op("pe", lambda e: e.matmul(ub[:, :], lhsT=Wu[:, c, f * 128:(f + 1) * 128], rhs=xsT[:, c, :], start=(c == 0), stop=(c == 7)),
                             reads=[b_W, b_xsT], writes=[bub], inc=(c == 7))
                    sg, b_sg = sgs.next()
                    s.op("act", lambda e: e.activation(out=sg[:], in_=gb[:, :], func=AF.Silu), reads=[bgb], writes=[b_sg])
                    s.op("dve", lambda e: e.tensor_tensor(out=hidT[:, f, :], in0=ub[:, :], in1=sg[:], op=ALU.mult), reads=[bub, b_sg], writes=[b_hid])
                if ch + 1 < NCH:
                    do_transposes(ch + 1)
                for i, r in enumerate(rows_of(ch)):
                    part = r % ROWS_PER_E
                    y, b_y = ys.next()
                    for hc in range(2):
                        yb, byb = ybk.next()
                        for f in range(8):
                            s.op("pe", lambda e: e.matmul(yb[:, :], lhsT=hidT[:, f, i * 128:(i + 1) * 128], rhs=Wd[:, f, hc * 512:(hc + 1) * 512],
                                                          start=(f == 0), stop=(f == 7)),
                                 reads=[b_W, b_hid], writes=[byb], inc=(f == 7))
                        s.op("dve", lambda e: e.tensor_scalar(out=y[:, hc * 512:(hc + 1) * 512], in0=yb[:, :], scalar1=gT[:, r:r + 1], scalar2=None,
                                                              op0=ALU.mult), reads=[byb], writes=[b_y])
                    s.dma("pool", lambda e: e.indirect_dma_start(out=h_d, out_offset=bass.IndirectOffsetOnAxis(ap=idsT[:, r:r + 1], axis=0),
                                                                 in_=y[:], in_offset=None, compute_op=ALU.add),
                          reads=[b_y] + hreg[(e_ + 1) % 2], writes=[hreg[e_ % 2][part]])
            s.barrier()
        wst.close()
        sel.close()

    if stop_after is None:
        with ExitStack() as ps:
            g_bc = T("gfin", [128, D], F32, ps)
            b_g = Buf()
            s.dma("sp", lambda e: e.dma_start(out=g_bc[:], in_=final_norm_d[0:1, :].partition_broadcast(128)), writes=[b_g])
            hts = Rot([(T(f"htf_{i}", [128, D], F32, ps), Buf()) for i in range(3)])
            ots = Rot([(T(f"otf_{i}", [128, D], F32, ps), Buf()) for i in range(3)])
            junk = T("junkf", [128, D], BF16, ps)
            b_junk = Buf()
            sss = Rot([(T(f"ssf_{i}", [128, 1], F32, ps), Buf()) for i in range(2)])
            for tb in range(NTB):
                ht, b_ht = hts.next()
                ot, b_ot = ots.next()
                ss, b_ss = sss.next()
                s.dma("sp", lambda e: e.dma_start(out=ht[:], in_=h_d[tb * 128:(tb + 1) * 128, :]), writes=[b_ht])
                s.op("dve", lambda e: e.scalar_tensor_tensor(out=junk[:], in0=ht[:], scalar=1.0, in1=ht[:],
                                                             op0=ALU.mult, op1=ALU.mult, accum_out=ss[:]),
                     reads=[b_ht], writes=[b_junk, b_ss])
                rstd_from_ss(ss[:], ss[:], 1.0 / D, [b_ss], [b_ss])
                s.op("dve", lambda e: e.scalar_tensor_tensor(out=ot[:], in0=ht[:], scalar=ss[:, 0:1], in1=g_bc[:],
                                                             op0=ALU.mult, op1=ALU.mult), reads=[b_ht, b_ss, b_g], writes=[b_ot])
                s.dma("sp", lambda e: e.dma_start(out=out_d[tb * 128:(tb + 1) * 128, :], in_=ot[:]), reads=[b_ot])
    s.barrier()
    es.close()
    return nc


def _swap_halves(w, hd):
    sh = w.shape
    w4 = w.reshape(sh[:-1] + (sh[-1] // hd, 2, hd // 2))
    return np.ascontiguousarray(w4[..., ::-1, :]).reshape(sh)


def _rope_tables(dim):
    inv = (1.0 / (np.float32(10000.0) ** (np.arange(0, dim, 2, dtype=np.float32) / np.float32(dim)))).astype(np.float32)
    ang = np.arange(S, dtype=np.float32)[:, None] * inv[None, :]
    return np.cos(ang).astype(np.float32), np.sin(ang).astype(np.float32)


def _host_prep(inp):
    f = lambda a: np.ascontiguousarray(a, dtype=np.float32)
    w_in = np.asarray(inp["w_in"])
    shared = {}
    shared["attn_norm"] = f(inp["attn_norm"])
    shared["ffn_norm"] = f(inp["ffn_norm"])
    shared["final_norm"] = f(np.asarray(inp["final_norm"]).reshape(1, D))
    shared["w_na"] = f(w_in[:, :, 0:768])
    shared["w_lat"] = f(w_in[:, :, 768:1152])
    kr96 = w_in[:, :, 1088:1184]
    shared["w_kr"] = f(kr96)
    krp = np.array(kr96, copy=True)
    krp[:, :, 64:96] = _swap_halves(kr96[:, :, 64:96], 32)
    shared["w_krp"] = f(krp)
    cq = w_in[:, :, 1184:1568].reshape(L, D, 6, 64)
    order = [0, 3, 1, 4, 2, 5]
    cq_r = cq[:, :, order, :].reshape(L, D, 384)
    shared["w_cq"] = f(cq_r)
    shared["w_cqp"] = f(_swap_halves(cq_r, 64))
    ck = w_in[:, :, 1568:1696]
    shared["w_ck"] = f(ck)
    shared["w_ckp"] = f(_swap_halves(ck, 64))
    shared["w_cv"] = f(w_in[:, :, 1696:1824])
    rpb = np.asarray(inp["na_rpb"], dtype=np.float32)
    tiles = np.full((L, NV, 4, 128, 320), NEG, np.float32)
    p = np.arange(128)
    kc = p % 64
    krl = p // 64
    c = np.arange(64)
    cs_ = np.clip(c - 8, 0, 48)
    colvalid = (kc[:, None] >= cs_[None, :]) & (kc[:, None] <= cs_[None, :] + 15)
    dc = np.clip(kc[:, None] - c[None, :] + 15, 0, 30)
    for v, (d0, roff, nb) in enumerate(NA_KEYS):
        for b in range(nb):
            kr_rel = 2 * b + krl
            rowvalid = (kr_rel >= roff) & (kr_rel <= roff + 7)
            dr = np.clip(d0 + kr_rel + 7, 0, 14)
            vals = rpb[:, :, dr[:, None], dc]
            ok = (rowvalid[:, None] & colvalid)[None, None]
            tiles[:, v, :, :, b * 64:(b + 1) * 64] = np.where(ok, vals, np.float32(NEG))
    shared["rpb_tiles"] = tiles
    shared["mla_q_norm"] = f(inp["mla_q_norm"])
    shared["mla_kv_norm"] = f(inp["mla_kv_norm"])
    w_uq = np.asarray(inp["mla_w_uq"])
    shared["w_uq"] = f(w_uq)
    uq4 = np.array(w_uq.reshape(L, 256, 6, 96), copy=True)
    uq4[..., 64:96] = _swap_halves(uq4[..., 64:96], 32)
    shared["w_uqp"] = f(uq4.reshape(L, 256, 576))
    ukv = np.asarray(inp["mla_w_ukv"]).reshape(L, 128, 6, 128)
    shared["w_uk"] = f(ukv[..., 0:64].reshape(L, 128, 384))
    shared["w_uv"] = f(ukv[..., 64:128].reshape(L, 128, 384))
    shared["swa_sink"] = f(inp["swa_sink"])
    shared["group_norm"] = f(inp["group_norm"])
    shared["w_out"] = f(inp["w_out"])
    shared["w_router"] = f(inp["w_router"])
    shared["w_gate"] = f(inp["w_gate"])
    shared["w_up"] = f(inp["w_up"])
    shared["w_down"] = f(inp["w_down"])
    cos, sin = _rope_tables(64)
    cT = np.concatenate([cos.T, cos.T], 0)
    sT = np.concatenate([-sin.T, sin.T], 0)
    shared["rope_c_cos"] = f(np.concatenate([cT, cT], 0))
    shared["rope_c_sin"] = f(np.concatenate([sT, sT], 0))
    cos, sin = _rope_tables(32)
    shared["rope_b_cos"] = f(np.concatenate([cos.T, cos.T], 0))
    shared["rope_b_sin"] = f(np.concatenate([-sin.T, sin.T], 0))
    k = np.arange(128)[:, None]
    q = np.arange(512)[None, :]
    m = np.zeros((6, 128, 512), np.float32)
    for j in range(6):
        diff = q - k - (j - 1) * 128
        m[j] = np.where(np.abs(diff) <= 128, 1.0, 0.0)
    shared["swa_mask"] = m.astype(ml_dtypes.bfloat16)
    shared["ident_bf"] = np.eye(128, dtype=np.float32).astype(ml_dtypes.bfloat16)
    shared["ident_f"] = np.eye(128, dtype=np.float32)
    pp = np.arange(128)
    shared["gmat"] = (pp[:, None] // 8 == pp[None, :] // 8).astype(np.float32)
    shared["rowoff"] = ((pp % 8) * 2).astype(np.float32).reshape(128, 1)
    shared["trimat"] = ((pp[:, None] // 8 == pp[None, :] // 8) & (pp[:, None] < pp[None, :])).astype(np.float32)
    nic = ROUNDS * 8 // 128
    ii = np.arange(128)[:, None, None]
    icc = np.arange(nic)[None, :, None]
    jj = np.arange(512)[None, None, :]
    shared["dmat"] = (jj - ii - icc * 128).astype(np.int16)
    shared["invw"] = np.tile(np.array([[1 / 256, 1 / 384, 1 / 384]], np.float32), (128, 1))
    return shared


def kernel(**inputs):
    shared = _host_prep(inputs)
    x = np.asarray(inputs["x"], dtype=np.float32)
    nc = build()
    in_maps = []
    for c in range(NCORES):
        m = dict(shared)
        m["x"] = np.ascontiguousarray(x[c])
        in_maps.append(m)
    res = run_bass_kernel_spmd(nc, in_maps, core_ids=list(range(NCORES)))
    return np.stack([np.asarray(res.results[c]["out"], dtype=np.float32) for c in range(NCORES)], axis=0)
```
